# Optimizing a Trainium2 kernel written in Bass

```python
import math
import jax, jax.numpy as jnp
from jax import lax
import numpy as np

D_MODEL = 1024
BATCH = 2
SEQ = 8192
DEPTH = 1

N_DIFF_HEADS = 8
DIFF_HEAD_DIM = 64
DIFF_V_DIM = 2 * DIFF_HEAD_DIM
ATTN_WIDTH = N_DIFF_HEADS * 2 * DIFF_HEAD_DIM
CONV_WIDTH = 1024
CONV_K = 3
SPLIT_SIZES = (ATTN_WIDTH, ATTN_WIDTH, ATTN_WIDTH, CONV_WIDTH, CONV_WIDTH, CONV_WIDTH, D_MODEL, D_MODEL)
SPLIT_POINTS = tuple(int(v) for v in np.cumsum(SPLIT_SIZES)[:-1])
IN_WIDTH = int(sum(SPLIT_SIZES))
N_EXPERTS = 32
TOP_K = 4
D_FF = 1024
SWIGLU_LIMIT = 7.0
SWIGLU_ALPHA = 1.702
ROPE_THETA = 10000.0
RMS_EPS = 1e-6
SUBLN_EPS = 1e-5
Q_BLOCK = 128
MOE_BLOCK = 128

kernel_name = "hybrid_diffattn_shortconv_moe_encoder_block"


def rmsnorm(x, g, eps=RMS_EPS):
    xf = x.astype(jnp.float32)
    y = xf * lax.rsqrt(jnp.mean(xf * xf, axis=-1, keepdims=True) + eps)
    return (y * g.astype(jnp.float32)).astype(x.dtype)


def rope(t, seq_len):
    dh = t.shape[-1]
    inv_freq = ROPE_THETA ** (-jnp.arange(0, dh, 2, dtype=jnp.float32) / dh)
    pos = jnp.arange(seq_len, dtype=jnp.float32)
    ang = pos[:, None] * inv_freq[None, :]
    emb = jnp.concatenate([ang, ang], axis=-1)[None, :, None, None, :]
    cos, sin = jnp.cos(emb), jnp.sin(emb)
    tf = t.astype(jnp.float32)
    t1, t2 = jnp.split(tf, 2, axis=-1)
    rot = jnp.concatenate([-t2, t1], axis=-1)
    return (tf * cos + rot * sin).astype(t.dtype)


def diff_attention(q, k, v, lam):
    b, s, h, _, dh = q.shape
    nq = s // Q_BLOCK
    qb = q.reshape(b, nq, Q_BLOCK, h, 2, dh).transpose(1, 0, 2, 3, 4, 5)
    scale = 1.0 / math.sqrt(dh)

    def one_block(q_blk):
        sc = jnp.einsum('bqhcd,bkhcd->bhcqk', q_blk, k).astype(jnp.float32) * scale
        p = jax.nn.softmax(sc, axis=-1)
        a = p[:, :, 0] - lam * p[:, :, 1]
        return jnp.einsum('bhqk,bkhd->bqhd', a.astype(v.dtype), v)

    o = lax.map(one_block, qb)
    return o.transpose(1, 0, 2, 3, 4).reshape(b, s, h, v.shape[-1])


def short_conv(z, w):
    return lax.conv_general_dilated(
        z, w[:, None, :].astype(z.dtype), window_strides=(1,),
        padding=[(CONV_K // 2, CONV_K // 2)],
        dimension_numbers=('NWC', 'WIO', 'NWC'),
        feature_group_count=z.shape[-1])


def moe(h, w_router, b_router, w_gate_up, b_gate_up, w_down, b_down):
    n, d = h.shape
    logits = h.astype(jnp.float32) @ w_router.astype(jnp.float32) + b_router.astype(jnp.float32)
    top_vals, top_idx = lax.top_k(logits, TOP_K)
    top_w = jax.nn.softmax(top_vals, axis=-1)
    nk = n * TOP_K
    flat_e = top_idx.reshape(-1)
    flat_tok = jnp.broadcast_to(jnp.arange(n, dtype=jnp.int32)[:, None], (n, TOP_K)).reshape(-1)
    flat_w = top_w.reshape(-1)
    order = jnp.argsort(flat_e)
    sorted_e = flat_e[order]
    counts = jnp.bincount(flat_e, length=N_EXPERTS)
    padded = ((counts + MOE_BLOCK - 1) // MOE_BLOCK) * MOE_BLOCK
    ends_pad = jnp.cumsum(padded)
    start_pad = ends_pad - padded
    start = jnp.cumsum(counts) - counts
    dest = start_pad[sorted_e] + (jnp.arange(nk) - start[sorted_e])
    p_rows = nk + N_EXPERTS * MOE_BLOCK
    n_blocks = p_rows // MOE_BLOCK
    row_tok = jnp.full((p_rows,), n, dtype=jnp.int32).at[dest].set(flat_tok[order])
    row_w = jnp.zeros((p_rows,), jnp.float32).at[dest].set(flat_w[order])
    block_e = jnp.clip(jnp.searchsorted(ends_pad, jnp.arange(n_blocks) * MOE_BLOCK, side='right'),
                       0, N_EXPERTS - 1)
    h_pad = jnp.concatenate([h, jnp.zeros((1, d), h.dtype)], axis=0)
    xs = h_pad[row_tok].reshape(n_blocks, MOE_BLOCK, d)

    def expert_block(args):
        xb, e = args
        gu = xb @ w_gate_up[e] + b_gate_up[e]
        gate = jnp.minimum(gu[..., ::2], SWIGLU_LIMIT)
        up = jnp.clip(gu[..., 1::2], -SWIGLU_LIMIT, SWIGLU_LIMIT)
        glu = gate * jax.nn.sigmoid(gate * SWIGLU_ALPHA)
        return ((up + 1.0) * glu) @ w_down[e] + b_down[e]

    ys = lax.map(expert_block, (xs, block_e)).reshape(p_rows, d)
    ys = (ys.astype(jnp.float32) * row_w[:, None]).astype(h.dtype)
    out = jnp.zeros((n + 1, d), h.dtype).at[row_tok].add(ys)
    return out[:n]


def setup_inputs(seed: int = 0) -> dict:
    key = jax.random.key(seed)
    ks = jax.random.split(key, 32)
    f32 = jnp.float32
    L, D, H, dh = DEPTH, D_MODEL, N_DIFF_HEADS, DIFF_HEAD_DIM
    nrm = lambda k, shape, s: jax.random.normal(k, shape, f32) * s
    return {
        "x": nrm(ks[0], (BATCH, SEQ, D), 1.0),
        "c": nrm(ks[1], (BATCH, D), 1.0),
        "w_ada": nrm(ks[2], (L, D, 6 * D), 0.5 * D ** -0.5),
        "b_ada": nrm(ks[3], (L, 6 * D), 0.01),
        "norm1_w": 1.0 + nrm(ks[4], (L, D), 0.02),
        "w_in": nrm(ks[5], (L, D, IN_WIDTH), D ** -0.5),
        "q_norm_w": 1.0 + nrm(ks[6], (L, dh), 0.02),
        "k_norm_w": 1.0 + nrm(ks[7], (L, dh), 0.02),
        "lambda_q1": nrm(ks[8], (L, dh), 0.1),
        "lambda_k1": nrm(ks[9], (L, dh), 0.1),
        "lambda_q2": nrm(ks[10], (L, dh), 0.1),
        "lambda_k2": nrm(ks[11], (L, dh), 0.1),
        "subln_w": 1.0 + nrm(ks[12], (L, DIFF_V_DIM), 0.02),
        "w_attn_o": nrm(ks[13], (L, H * DIFF_V_DIM, D), (H * DIFF_V_DIM) ** -0.5),
        "conv_w": nrm(ks[14], (L, CONV_K, CONV_WIDTH), CONV_K ** -0.5),
        "w_conv_o": nrm(ks[15], (L, CONV_WIDTH, D), CONV_WIDTH ** -0.5),
        "w_out": nrm(ks[16], (L, D, D), D ** -0.5),
        "norm2_w": 1.0 + nrm(ks[17], (L, D), 0.02),
        "w_router": nrm(ks[18], (L, D, N_EXPERTS), D ** -0.5),
        "b_router": nrm(ks[19], (L, N_EXPERTS), 0.01),
        "w_gate_up": nrm(ks[20], (L, N_EXPERTS, D, 2 * D_FF), D ** -0.5),
        "b_gate_up": nrm(ks[21], (L, N_EXPERTS, 2 * D_FF), 0.01),
        "w_down": nrm(ks[22], (L, N_EXPERTS, D_FF, D), D_FF ** -0.5),
        "b_down": nrm(ks[23], (L, N_EXPERTS, D), 0.01),
    }


def reference(x, c, w_ada, b_ada, norm1_w, w_in, q_norm_w, k_norm_w, lambda_q1, lambda_k1,
              lambda_q2, lambda_k2, subln_w, w_attn_o, conv_w, w_conv_o, w_out, norm2_w,
              w_router, b_router, w_gate_up, b_gate_up, w_down, b_down):
    b, s, d = x.shape
    H, dh = N_DIFF_HEADS, DIFF_HEAD_DIM
    for l in range(DEPTH):
        lambda_init = 0.8 - 0.6 * math.exp(-0.3 * l)
        mod = jax.nn.silu(c) @ w_ada[l] + b_ada[l]
        sh1, sc1, g1, sh2, sc2, g2 = jnp.split(mod[:, None, :], 6, axis=-1)

        h = rmsnorm(x, norm1_w[l]) * (1.0 + sc1) + sh1
        proj = h @ w_in[l]
        q, k, v, cb, cc, cx, ga, gc = jnp.split(proj, SPLIT_POINTS, axis=-1)

        q = rope(rmsnorm(q.reshape(b, s, H, 2, dh), q_norm_w[l]), s)
        k = rope(rmsnorm(k.reshape(b, s, H, 2, dh), k_norm_w[l]), s)
        v = v.reshape(b, s, H, DIFF_V_DIM)
        lam = (jnp.exp(jnp.sum(lambda_q1[l].astype(jnp.float32) * lambda_k1[l].astype(jnp.float32)))
               - jnp.exp(jnp.sum(lambda_q2[l].astype(jnp.float32) * lambda_k2[l].astype(jnp.float32)))
               + lambda_init)
        o = diff_attention(q, k, v, lam)
        o = rmsnorm(o, subln_w[l], SUBLN_EPS) * (1.0 - lambda_init)
        y_attn = o.reshape(b, s, H * DIFF_V_DIM) @ w_attn_o[l]

        y_conv = (cb * short_conv(cc * cx, conv_w[l])) @ w_conv_o[l]

        m = jax.nn.sigmoid(ga) * y_attn + jax.nn.sigmoid(gc) * y_conv
        x = x + g1 * (m @ w_out[l])

        h2 = rmsnorm(x, norm2_w[l]) * (1.0 + sc2) + sh2
        y_moe = moe(h2.reshape(b * s, d), w_router[l], b_router[l], w_gate_up[l], b_gate_up[l],
                    w_down[l], b_down[l]).reshape(b, s, d)
        x = x + g2 * y_moe
    return x
```

```python
import types
import numpy as np
from contextlib import ExitStack
import concourse.bass as bass
import concourse.mybir as mybir
from concourse.bass_utils import run_bass_kernel_spmd

F32 = mybir.dt.float32
BF = mybir.dt.bfloat16
ALU = mybir.AluOpType
AF = mybir.ActivationFunctionType
AX = mybir.AxisListType

SEQ = 8192
D = 1024
TOWN = 2048
NE = 32
ENGS = ['pe', 'act', 'dve', 'pool', 'sp']
DEBUG = None


class Reg:
    __slots__ = ('name', 'w', 'rs', 'dsem', 'dcnt', 'excl')
    ALL = []

    def __init__(s, name, excl=False):
        s.name = name; s.w = None; s.rs = {}; s.dsem = None; s.dcnt = 0; s.excl = excl
        Reg.ALL.append(s)


class Op:
    __slots__ = ('eng', 'fn', 'deps', 'needs_inc', 'sigval', 'dreg')


def freeze(fn):
    if fn is None or fn.__closure__ is None:
        return fn
    cells = []
    for c in fn.__closure__:
        try:
            cells.append(types.CellType(c.cell_contents))
        except ValueError:
            cells.append(c)
    return types.FunctionType(fn.__code__, fn.__globals__, fn.__name__, fn.__defaults__, tuple(cells))


class Sched:
    def __init__(s):
        s.q = {e: [] for e in ENGS}
        s.dregs = []

    def add(s, eng, fn, r=(), w=(), dma=None):
        op = Op(); op.eng = eng; op.fn = freeze(fn); op.needs_inc = False; op.sigval = 0; op.dreg = dma
        w = list(w) + [g for g in r if g.excl]
        r = [g for g in r if not g.excl]
        deps = []
        for g in r:
            if g.w is not None:
                deps.append(g.w)
        for g in w:
            if g.w is not None:
                deps.append(g.w)
            deps.extend(g.rs.values())
        op.deps = [d for d in deps if not (d[0] == 'op' and d[1].eng == eng and eng == 'pe')]
        if dma is not None:
            if dma.dcnt == 0 and dma not in s.dregs:
                s.dregs.append(dma)
            dma.dcnt += 16
            tok = ('dma', dma, dma.dcnt); key = ('d', id(dma))
        else:
            tok = ('op', op); key = eng
        for g in r:
            g.rs[key] = tok
        for g in w:
            g.w = tok; g.rs = {}
        s.q[eng].append(op)
        return op

    def wait_regs(s, eng, regs):
        op = Op(); op.eng = eng; op.fn = None; op.needs_inc = False; op.sigval = 0; op.dreg = None
        deps = []
        for g in regs:
            if g.w is not None:
                deps.append(g.w)
            deps.extend(g.rs.values())
        op.deps = deps
        s.q[eng].append(op)

    def barrier(s):
        deps = []
        for e in ENGS:
            for op in reversed(s.q[e]):
                if op.fn is not None and op.dreg is None:
                    deps.append(('op', op))
                    break
        for g in s.dregs:
            if g.dcnt > 0:
                deps.append(('dma', g, g.dcnt))
        for e in ENGS:
            op = Op(); op.eng = e; op.fn = None; op.needs_inc = False; op.sigval = 0; op.dreg = None
            op.deps = [d for d in deps if not (d[0] == 'op' and d[1].eng == e and e == 'pe')]
            s.q[e].append(op)
        for g in Reg.ALL:
            g.w = None; g.rs = {}

    def prepare(s, nc, stack):
        for e in ENGS:
            for op in s.q[e]:
                for d in op.deps:
                    if d[0] == 'op':
                        d[1].needs_inc = True
        for e in ENGS:
            c = 0
            for op in s.q[e]:
                if op.needs_inc:
                    c += 1
                    op.sigval = c
        s.esem = {e: stack.enter_context(nc.semaphore("sem_" + e)) for e in ENGS}
        for i, g in enumerate(s.dregs):
            g.dsem = stack.enter_context(nc.semaphore("dsem%d_%s" % (i, g.name)))

    def run(s, e, eng):
        known = {}
        for op in s.q[e]:
            for d in op.deps:
                if d[0] == 'op':
                    sem = s.esem[d[1].eng]; val = d[1].sigval; k = d[1].eng
                else:
                    sem = d[1].dsem; val = d[2]; k = id(d[1])
                if known.get(k, 0) < val:
                    eng.wait_ge(sem, val)
                    known[k] = val
            if op.fn is None:
                continue
            ins = op.fn(eng)
            if op.dreg is not None:
                ins.then_inc(op.dreg.dsem, 16)
            elif op.needs_inc:
                ins.then_inc(s.esem[e], 1)


class Arena:
    def __init__(s, t, nbytes):
        s.t = t; s.off = 0; s.cap = nbytes

    def alloc(s, dtype, *free):
        n = 1
        for f in free:
            n *= f
        esz = 2 if dtype == BF else 4
        nbytes = (n * esz + 63) // 64 * 64
        assert s.off + nbytes <= s.cap, ("arena overflow", s.off, nbytes, s.cap)
        a = s.t[:, s.off // 4:(s.off + nbytes) // 4]
        s.off += nbytes
        if dtype != F32:
            a = a.bitcast(dtype)
        a = a[:, 0:n]
        if len(free) == 2:
            a = a.rearrange("p (a b) -> p a b", b=free[1])
        elif len(free) == 3:
            a = a.rearrange("p (a b c) -> p a b c", b=free[1], c=free[2])
        return a


def build():
    nc = bass.Bass("TRN2", target_bir_lowering=False)

    def din(name, shape, dt=F32):
        return nc.dram_tensor(name, list(shape), dt, kind="ExternalInput").ap()

    xs = din("xs", [SEQ, D]); xh = din("xh", [2, D]); hm = din("hm", [128, 2])
    rope = din("rope", [SEQ, 128]); cvec = din("cvec", [1, D]); ident = din("ident", [128, 128])
    w_ada = din("w_ada", [D, 6 * D]); b_ada = din("b_ada", [1, 6 * D])
    norm1_w = din("norm1_w", [1, D]); w_in = din("w_in", [D, 8 * D])
    q_norm_w = din("q_norm_w", [1, 64]); k_norm_w = din("k_norm_w", [1, 64])
    lq1 = din("lambda_q1", [1, 64]); lk1 = din("lambda_k1", [1, 64])
    lq2 = din("lambda_q2", [1, 64]); lk2 = din("lambda_k2", [1, 64])
    subln_w = din("subln_w", [1, 128]); w_attn_o = din("w_attn_o", [D, D])
    conv_w = din("conv_w", [3, D]); w_conv_o = din("w_conv_o", [D, D]); w_out = din("w_out", [D, D])
    norm2_w = din("norm2_w", [1, D]); w_router = din("w_router", [D, NE]); b_router = din("b_router", [1, NE])
    w_gate_up = din("w_gate_up", [NE, D, 2 * D]); b_gate_up = din("b_gate_up", [NE, 2 * D])
    w_down = din("w_down", [NE, D, D]); b_down = din("b_down", [NE, D])
    y = nc.dram_tensor("y", [TOWN, D], F32, kind="ExternalOutput").ap()
    KTd = nc.dram_tensor("ktd", [128, 8, SEQ], BF, kind="Internal").ap()
    Vd = nc.dram_tensor("vd", [8, SEQ, 128], BF, kind="Internal").ap()
    X1d = nc.dram_tensor("x1d", [TOWN, D], F32, kind="Internal").ap()
    dbg = None
    if DEBUG == 'attn':
        dbg = nc.dram_tensor("dbg", [128, 8 * TOWN], BF, kind="ExternalOutput").ap()
    elif DEBUG == 'x1':
        dbg = nc.dram_tensor("dbg", [TOWN, 32], F32, kind="ExternalOutput").ap()
    elif DEBUG == 'p4':
        dbg = nc.dram_tensor("dbg", [128, 4, 8 * 514], BF, kind="ExternalOutput").ap()

    S = Sched()
    stack = ExitStack()
    ARENA_BYTES = 188 * 1024
    arena_t = stack.enter_context(nc.sbuf_tensor("arena", [128, ARENA_BYTES // 4], F32))
    A = Arena(arena_t, ARENA_BYTES)
    PS = []
    PSr = []
    SXYs = []
    for i in range(2):
        sxy = stack.enter_context(nc.psum_tensor("sxy%d" % i, [128, 1024], F32))
        SXYs.append(sxy[:, :])
        PS.append(sxy[:, 0:512]); PS.append(sxy[:, 512:1024])
    for i in range(4, 8):
        t = stack.enter_context(nc.psum_tensor("ps%d" % i, [128, 512], F32))
        PS.append(t[:, :])
    for i in range(8):
        PSr.append(Reg("ps%d" % i, excl=True))
    PSB = [p.bitcast(BF) for p in PS]

    identF = A.alloc(F32, 128); identB = A.alloc(BF, 128)
    OT = A.alloc(BF, 8, TOWN)
    G2 = A.alloc(F32, D)
    WG = A.alloc(F32, 16, NE)
    P5_MARK = A.off
    SH1 = A.alloc(F32, D); G1 = A.alloc(F32, D); SH2 = A.alloc(F32, D)
    W1 = A.alloc(F32, D); W2 = A.alloc(F32, D)
    KNW = A.alloc(F32, 64); KNWS = A.alloc(F32, 64); QNW = A.alloc(F32, 64); QNWS = A.alloc(F32, 64)
    SUBW = A.alloc(F32, 128); NLAM = A.alloc(F32, 2)
    CW = A.alloc(F32, 3, 8); HM = A.alloc(F32, 2); BR = A.alloc(F32, NE); WRT = A.alloc(F32, 8, NE)
    PASS_MARK = A.off

    CONST = Reg("const")
    OTr = [Reg("ot%d" % g) for g in range(4)]
    WGr = Reg("wg")
    misc = Reg("misc")

    def cload(out, in_, **kw):
        S.add('sp', lambda e: e.dma_start(out=out, in_=in_, **kw), dma=CONST)

    def row_b(ap1d):
        return ap1d.partition_broadcast(128)

    SC1 = A.alloc(F32, D); SC2 = A.alloc(F32, D)
    cT = A.alloc(F32, 8); scT = A.alloc(F32, 8); scB = A.alloc(F32, 8, 128)
    LQ = [A.alloc(F32, 64) for _ in range(4)]
    LT = A.alloc(F32, 8)
    WA = [A.alloc(F32, 8, 512) for _ in range(2)]
    WAr = [Reg("wa0"), Reg("wa1")]

    cload(identF, ident)
    mod_tiles = [SH1, SC1, G1, SH2, SC2, G2]
    for i, t in enumerate(mod_tiles):
        cload(t, row_b(b_ada[0, i * D:(i + 1) * D]))
    cload(W1, row_b(norm1_w[0, :])); cload(W2, row_b(norm2_w[0, :]))
    cload(KNW, row_b(k_norm_w[0, :])); cload(QNW, row_b(q_norm_w[0, :]))
    cload(KNWS[:, 0:32], row_b(k_norm_w[0, 32:64])); cload(KNWS[:, 32:64], row_b(k_norm_w[0, 0:32]))
    cload(QNWS[:, 0:32], row_b(q_norm_w[0, 32:64])); cload(QNWS[:, 32:64], row_b(q_norm_w[0, 0:32]))
    cload(SUBW, row_b(subln_w[0, :]))
    for t, src in zip(LQ, (lq1, lk1, lq2, lk2)):
        cload(t, row_b(src[0, :]))
    cload(HM, hm); cload(BR, row_b(b_router[0, :]))
    cload(WRT, w_router.rearrange("(kc p) e -> p kc e", p=128))
    for k3 in range(3):
        cload(CW[:, k3, :], conv_w[k3, :].rearrange("(j p) -> p j", p=128), allow_slow_non_contiguous=True)
    cload(cT, cvec.rearrange("o (j p) -> p (o j)", p=128), allow_slow_non_contiguous=True)
    CONST.w = ('dma', CONST, CONST.dcnt)

    S.add('dve', lambda e: e.tensor_copy(out=identB, in_=identF), r=[CONST], w=[misc])
    S.add('pool', lambda e: e.memset(NLAM[:, 1:2], -0.5), w=[misc])
    S.add('act', lambda e: e.activation(out=scT, in_=cT, func=AF.Sigmoid), r=[CONST], w=[misc])
    S.add('dve', lambda e: e.tensor_tensor(out=scT, in0=scT, in1=cT, op=ALU.mult), r=[misc, CONST], w=[misc])
    S.add('dve', lambda e: e.tensor_copy(out=scB, in_=scT.unsqueeze(2).broadcast_to([128, 8, 128])),
          r=[misc], w=[misc])
    for i in range(2):
        S.add('dve', lambda e, i=i: e.tensor_tensor(out=LQ[2 * i], in0=LQ[2 * i], in1=LQ[2 * i + 1], op=ALU.mult),
              r=[CONST, misc], w=[misc])
        S.add('dve', lambda e, i=i: e.tensor_reduce(out=LT[:, i:i + 1], in_=LQ[2 * i], axis=AX.X, op=ALU.add),
              r=[misc], w=[misc])
    S.add('act', lambda e: e.activation(out=LT[:, 2:4], in_=LT[:, 0:2], func=AF.Exp), r=[misc], w=[misc])
    S.add('dve', lambda e: e.tensor_tensor(out=LT[:, 4:5], in0=LT[:, 2:3], in1=LT[:, 3:4], op=ALU.subtract),
          r=[misc], w=[misc])
    S.add('dve', lambda e: e.tensor_scalar(out=NLAM[:, 0:1], in0=LT[:, 4:5], scalar1=0.2, scalar2=-1.0,
                                           op0=ALU.add, op1=ALU.mult), r=[misc], w=[misc])
    S.add('dve', lambda e: e.tensor_scalar(out=SUBW, in0=SUBW, scalar1=0.8, scalar2=None, op0=ALU.mult),
          r=[CONST, misc], w=[misc])

    for cg in range(12):
        wa = WA[cg % 2]; war = WAr[cg % 2]
        S.add('sp', lambda e, wa=wa, cg=cg: e.dma_start(
            out=wa, in_=w_ada[:, cg * 512:(cg + 1) * 512].rearrange("(kc p) n -> p kc n", p=128)),
            w=[war], dma=war)
        bank = cg % 2
        for kc in range(8):
            S.add('pe', lambda e, wa=wa, kc=kc, bank=bank: e.matmul(
                PS[bank], lhsT=scB[:, kc, :], rhs=wa[:, kc, :], start=(kc == 0), stop=(kc == 7)),
                r=[misc, war], w=[PSr[bank]])
        dst = mod_tiles[cg // 2][:, (cg % 2) * 512:(cg % 2 + 1) * 512]
        S.add('dve', lambda e, dst=dst, bank=bank: e.tensor_tensor(out=dst, in0=PS[bank], in1=dst, op=ALU.add),
              r=[PSr[bank], CONST, misc], w=[misc])
    S.add('dve', lambda e: e.scalar_tensor_tensor(out=W1, in0=SC1, scalar=1.0, in1=W1, op0=ALU.add, op1=ALU.mult),
          r=[misc, CONST], w=[misc])
    S.add('dve', lambda e: e.scalar_tensor_tensor(out=W2, in0=SC2, scalar=1.0, in1=W2, op0=ALU.add, op1=ALU.mult),
          r=[misc, CONST], w=[misc])

    barrier = S.barrier

    nrm_r = Reg("nrm")

    def norm_tile(xt, xr, Wt, SHt, junk, ssq, tmp, outs, regs_out, npart=128, tag="n"):
        tr = nrm_r
        S.add('act', lambda e: e.activation(out=junk[0:npart], in_=xt[0:npart], func=AF.Square,
                                            accum_out=ssq[0:npart, 0:1]), r=[xr], w=[tr])
        S.add('act', lambda e: e.activation(out=ssq[0:npart, 1:2], in_=ssq[0:npart, 0:1], func=AF.Sqrt,
                                            scale=1.0 / D, bias=1e-6), r=[tr], w=[tr])
        S.add('dve', lambda e: e.reciprocal(out=ssq[0:npart, 2:3], in_=ssq[0:npart, 1:2]), r=[tr], w=[tr])
        S.add('dve', lambda e: e.scalar_tensor_tensor(out=tmp[0:npart], in0=xt[0:npart], scalar=ssq[0:npart, 2:3],
                                                      in1=Wt[0:npart], op0=ALU.mult, op1=ALU.mult),
              r=[tr, xr, misc], w=[tr])
        for o, ro in zip(outs, regs_out):
            S.add('dve', lambda e, o=o: e.tensor_tensor(out=o[0:npart], in0=tmp[0:npart], in1=SHt[0:npart],
                                                        op=ALU.add), r=[tr, misc, xr], w=[ro])

    barrier()
    A.off = PASS_MARK
    QT = A.alloc(BF, 8, TOWN); QTr = Reg("qt")
    WKV = A.alloc(BF, 8, 2048); WKVr = Reg("wkv")
    WQ = A.alloc(BF, 8, 1024); WQr = Reg("wq")
    XT = [A.alloc(F32, D) for _ in range(2)]; XTr = [Reg("xt0"), Reg("xt1")]
    RT = [A.alloc(F32, 128) for _ in range(2)]; RTr = [Reg("rt0"), Reg("rt1")]
    Hb2 = [A.alloc(BF, D) for _ in range(2)]; Hr2 = [Reg("h0"), Reg("h1")]
    HT2 = [A.alloc(BF, 8, 128) for _ in range(2)]; HTr2 = [Reg("ht0"), Reg("ht1")]
    JUNK = A.alloc(BF, D)
    SSQ = A.alloc(F32, 4)
    SQ = A.alloc(F32, D)
    T1 = A.alloc(F32, D); T2 = A.alloc(F32, D)
    S16 = A.alloc(F32, 48)
    AB = A.alloc(F32, 256)
    KB2 = [A.alloc(BF, D) for _ in range(2)]; KBr2 = [Reg("kb0"), Reg("kb1")]
    QB = A.alloc(BF, D); QBr = Reg("qb")
    KTt = [A.alloc(BF, 8, 128) for _ in range(2)]; KTtr = [Reg("ktt0"), Reg("ktt1")]
    Vt = [A.alloc(BF, D) for _ in range(2)]; Vtr = [Reg("vt0"), Reg("vt1")]
    p1r = Reg("p1")

    for half in range(2):
        S.add('pool', lambda e, half=half: e.dma_start(
            out=WKV[:, :, half * 1024:(half + 1) * 1024],
            in_=w_in[:, 1024 + half * 1024:2048 + half * 1024].rearrange("(kc p) n -> p kc n", p=128)),
            dma=WKVr)
    WKVr.w = ('dma', WKVr, WKVr.dcnt)
    S.add('pool', lambda e: e.dma_start(out=WQ, in_=w_in[:, 0:1024].rearrange("(kc p) n -> p kc n", p=128)),
          w=[WQr], dma=WQr)

    kpost_r = Reg("kpost")

    def qk_post(banks, A_, B_, outbf, outr, tag):
        tr = kpost_r
        for hh in range(2):
            S.add('act', lambda e, hh=hh: e.activation(out=SQ[:, hh * 512:(hh + 1) * 512], in_=PS[banks[hh]],
                                                       func=AF.Square), r=[PSr[banks[hh]]], w=[tr])
        S.add('dve', lambda e: e.tensor_reduce(out=S16[:, 0:16], in_=SQ.rearrange("p (g d) -> p g d", d=64),
                                               axis=AX.X, op=ALU.add), r=[tr], w=[tr])
        S.add('act', lambda e: e.activation(out=S16[:, 16:32], in_=S16[:, 0:16], func=AF.Sqrt,
                                            scale=1.0 / 64, bias=1e-6), r=[tr], w=[tr])
        S.add('dve', lambda e: e.reciprocal(out=S16[:, 32:48], in_=S16[:, 16:32]), r=[tr], w=[tr])
        for hh in range(2):
            pv = PS[banks[hh]].rearrange("p (g d) -> p g d", d=64)
            t1 = T1[:, hh * 512:(hh + 1) * 512].rearrange("p (g d) -> p g d", d=64)
            t2 = T2[:, hh * 512:(hh + 1) * 512].rearrange("p (g d) -> p g d", d=64)
            S.add('dve', lambda e, pv=pv, t1=t1: e.tensor_tensor(
                out=t1, in0=pv, in1=A_.unsqueeze(1).broadcast_to([128, 8, 64]), op=ALU.mult),
                r=[PSr[banks[hh]], p1r], w=[tr])
            S.add('dve', lambda e, pv=pv, t2=t2: e.tensor_tensor(
                out=t2[:, :, 0:32], in0=pv[:, :, 32:64], in1=B_[:, 0:32].unsqueeze(1).broadcast_to([128, 8, 32]),
                op=ALU.mult), r=[PSr[banks[hh]], p1r], w=[tr])
            S.add('dve', lambda e, pv=pv, t2=t2: e.tensor_tensor(
                out=t2[:, :, 32:64], in0=pv[:, :, 0:32], in1=B_[:, 32:64].unsqueeze(1).broadcast_to([128, 8, 32]),
                op=ALU.mult), r=[PSr[banks[hh]], p1r], w=[tr])
        S.add('pool', lambda e: e.tensor_tensor(out=T1, in0=T1, in1=T2, op=ALU.add), r=[tr], w=[tr])
        S.add('dve', lambda e: e.tensor_tensor(
            out=outbf.rearrange("p (g d) -> p g d", d=64), in0=T1.rearrange("p (g d) -> p g d", d=64),
            in1=S16[:, 32:48].unsqueeze(2).broadcast_to([128, 16, 64]), op=ALU.mult), r=[tr], w=[outr])

    NT = SEQ // 128
    NOWN = TOWN // 128

    def kbanks(j):
        return [1, 2] if (j < NOWN or j % 2 == 0) else [6, 7]

    def st_N(j):
        xt = XT[j % 2]; xr = XTr[j % 2]; rt = RT[j % 2]; rr = RTr[j % 2]
        S.add('sp', lambda e: e.dma_start(out=xt, in_=xs[j * 128:(j + 1) * 128, :]), w=[xr], dma=xr)
        S.add('sp', lambda e: e.dma_start(out=rt, in_=rope[j * 128:(j + 1) * 128, :]), w=[rr], dma=rr)
        norm_tile(xt, xr, W1, SH1, JUNK, SSQ, xt, [Hb2[j % 2]], [Hr2[j % 2]], tag="n1")

    def st_T(j):
        hb = Hb2[j % 2]; hr = Hr2[j % 2]; ht = HT2[j % 2]; htr = HTr2[j % 2]
        for kc in range(8):
            S.add('pe', lambda e: e.transpose(out=PSB[0][:, kc * 128:(kc + 1) * 128],
                                              in_=hb[:, kc * 128:(kc + 1) * 128], identity=identB),
                  r=[hr, misc], w=[PSr[0]])
        S.add('act', lambda e: e.copy(out=ht.rearrange("p a b -> p (a b)"), in_=PSB[0]), r=[PSr[0]], w=[htr])

    def st_M(j):
        ht = HT2[j % 2]; htr = HTr2[j % 2]
        kb_ = kbanks(j)
        banks = [kb_[0], kb_[1], 3, 4]
        for n in range(4):
            for kc in range(8):
                S.add('pe', lambda e: e.matmul(
                    PS[banks[n]], lhsT=ht[:, kc, :], rhs=WKV[:, kc, n * 512:(n + 1) * 512],
                    start=(kc == 0), stop=(kc == 7)), r=[htr, WKVr], w=[PSr[banks[n]]])
        if j < NOWN:
            for n in range(2):
                for kc in range(8):
                    S.add('pe', lambda e: e.matmul(
                        PS[6 + n], lhsT=ht[:, kc, :], rhs=WQ[:, kc, n * 512:(n + 1) * 512],
                        start=(kc == 0), stop=(kc == 7)), r=[htr, WQr], w=[PSr[6 + n]])

    def st_E(j):
        own = j < NOWN
        rt = RT[j % 2]; rr = RTr[j % 2]
        vt = Vt[j % 2]; vr = Vtr[j % 2]
        for n in range(2):
            S.add('act', lambda e: e.copy(out=vt[:, n * 512:(n + 1) * 512], in_=PS[3 + n]),
                  r=[PSr[3 + n]], w=[vr])
        S.add('sp', lambda e: e.dma_start(
            out=Vd[:, j * 128:(j + 1) * 128, :].rearrange("h t d -> t h d"),
            in_=vt.rearrange("p (h d) -> p h d", d=128)), r=[vr], dma=vr)
        S.add('dve', lambda e: e.tensor_tensor(out=AB[:, 0:64], in0=rt[:, 0:64], in1=KNW, op=ALU.mult),
              r=[rr, CONST], w=[p1r])
        S.add('dve', lambda e: e.tensor_tensor(out=AB[:, 64:128], in0=rt[:, 64:128], in1=KNWS, op=ALU.mult),
              r=[rr, CONST], w=[p1r])
        if own:
            S.add('dve', lambda e: e.tensor_tensor(out=AB[:, 128:192], in0=rt[:, 0:64], in1=QNW, op=ALU.mult),
                  r=[rr, CONST], w=[p1r])
            S.add('dve', lambda e: e.tensor_tensor(out=AB[:, 192:256], in0=rt[:, 64:128], in1=QNWS,
                                                   op=ALU.mult), r=[rr, CONST], w=[p1r])
        qk_post(kbanks(j), AB[:, 0:64], AB[:, 64:128], KB2[j % 2], KBr2[j % 2], "kpost")
        if own:
            qk_post([6, 7], AB[:, 128:192], AB[:, 192:256], QB, QBr, "qpost")

    def st_K(j):
        own = j < NOWN
        kb = KB2[j % 2]; kbr = KBr2[j % 2]
        ktt = KTt[j % 2]; ktr = KTtr[j % 2]
        for h in range(8):
            S.add('pe', lambda e: e.transpose(out=PSB[5][:, h * 128:(h + 1) * 128],
                                              in_=kb[:, h * 128:(h + 1) * 128], identity=identB),
                  r=[kbr, misc], w=[PSr[5]])
        S.add('act', lambda e: e.copy(out=ktt.rearrange("p a b -> p (a b)"), in_=PSB[5]),
              r=[PSr[5]], w=[ktr])
        S.add('sp', lambda e: e.dma_start(out=KTd[:, :, j * 128:(j + 1) * 128], in_=ktt),
              r=[ktr], dma=ktr)
        if own:
            qb_ = QB; qbr = QBr
            for h in range(8):
                S.add('pe', lambda e: e.transpose(out=PSB[5][:, h * 128:(h + 1) * 128],
                                                  in_=qb_[:, h * 128:(h + 1) * 128], identity=identB),
                      r=[qbr, misc], w=[PSr[5]])
            S.add('act', lambda e: e.copy(out=QT[:, :, j * 128:(j + 1) * 128],
                                          in_=PSB[5].rearrange("p (a b) -> p a b", b=128)),
                  r=[PSr[5]], w=[QTr])

    st_N(0); st_T(0); st_M(0)
    for j in range(NT):
        if j + 1 < NT:
            st_N(j + 1); st_T(j + 1)
        st_E(j)
        if j + 1 < NT:
            st_M(j + 1)
        st_K(j)

    barrier()
    A.off = PASS_MARK + 2 * 8 * TOWN
    KTb = [A.alloc(BF, SEQ) for _ in range(2)]; KTbr = [Reg("ktb0"), Reg("ktb1")]
    Vb = [A.alloc(BF, 64, 130) for _ in range(2)]; Vbr = [Reg("vb0"), Reg("vb1")]
    PT = [A.alloc(BF, 512) for _ in range(3)]; PTr = [Reg("pt%d" % i) for i in range(3)]
    RC = A.alloc(F32, 8)
    OS = [A.alloc(F32, 128) for _ in range(2)]
    ONB = [A.alloc(BF, 128) for _ in range(2)]
    JKF = A.alloc(F32, 128)
    ppr = [Reg("pp0"), Reg("pp1")]
    Sr = [Reg("s0", excl=True), Reg("s1", excl=True)]
    for b in range(2):
        S.add('pool', lambda e, b=b: e.memset(Vb[b][:, :, 128:129], 1.0), w=[Vbr[b]])

    for h in range(8):
        kt = KTb[h % 2]; ktr = KTbr[h % 2]; vb = Vb[h % 2]; vbr = Vbr[h % 2]
        S.add('sp', lambda e, kt=kt, h=h: e.dma_start(out=kt, in_=KTd[:, h, :]), w=[ktr], dma=ktr)
        S.add('sp', lambda e, vb=vb, h=h: e.dma_start(
            out=vb[:, :, 0:128], in_=Vd[h].rearrange("(kc p) d -> p kc d", p=128)), w=[vbr], dma=vbr)
        def emit_qk(i, kt=kt, ktr=ktr, h=h):
            qt, kc = divmod(i, 64); q0 = qt * 256; sb = i % 2
            for c in range(2):
                S.add('pe', lambda e: e.matmul(
                    PS[2 * sb + c][:, 0:256], lhsT=kt[c * 64:(c + 1) * 64, kc * 128:(kc + 1) * 128],
                    rhs=QT[c * 64:(c + 1) * 64, h, q0:q0 + 256], start=True, stop=True),
                    r=[ktr, QTr], w=[Sr[sb]])

        emit_qk(0)
        for it in range(512):
            qt, kc = divmod(it, 64); q0 = qt * 256
            sb = it % 2; pt = PT[it % 3]; ptr = PTr[it % 3]
            S.add('act', lambda e: e.activation(
                out=pt.rearrange("p (c n) -> p c n", n=256),
                in_=SXYs[sb].rearrange("p (c n) -> p c n", n=512)[:, :, 0:256],
                func=AF.Exp, scale=0.125), r=[Sr[sb]], w=[ptr])
            if it + 1 < 512:
                emit_qk(it + 1)
            for qb in range(2):
                for c in range(2):
                    bank = 4 + qb
                    S.add('pe', lambda e: e.matmul(
                        PS[bank][:, c * 256:c * 256 + 129], lhsT=pt[:, c * 256 + qb * 128:c * 256 + qb * 128 + 128],
                        rhs=vb[:, kc, 0:129], start=(kc == 0 and c == 0), stop=(kc == 63),
                        skip_group_check=True),
                        r=[ptr, vbr], w=[PSr[bank]])
            if kc != 63:
                continue
            for qb in range(2):
                bk = 4 + qb
                pr = ppr[qb]; os_ = OS[qb]; onb = ONB[qb]
                rc = RC[:, qb * 4:(qb + 1) * 4]
                S.add('dve', lambda e, bk=bk, rc=rc: e.reciprocal(out=rc[:, 0:1], in_=PS[bk][:, 128:129]),
                      r=[PSr[bk]], w=[pr])
                S.add('dve', lambda e, bk=bk, rc=rc: e.reciprocal(out=rc[:, 1:2], in_=PS[bk][:, 384:385]),
                      r=[PSr[bk]], w=[pr])
                S.add('dve', lambda e, rc=rc: e.tensor_tensor(out=rc[:, 1:2], in0=rc[:, 1:2], in1=NLAM[:, 0:1],
                                                              op=ALU.mult), r=[pr, misc], w=[pr])
                S.add('dve', lambda e, bk=bk, rc=rc, os_=os_: e.tensor_scalar(
                    out=os_, in0=PS[bk][:, 0:128], scalar1=rc[:, 0:1], scalar2=None, op0=ALU.mult),
                    r=[PSr[bk], pr], w=[pr])
                S.add('dve', lambda e, bk=bk, rc=rc, os_=os_: e.scalar_tensor_tensor(
                    out=os_, in0=PS[bk][:, 256:384], scalar=rc[:, 1:2], in1=os_, op0=ALU.mult, op1=ALU.add),
                    r=[PSr[bk], pr], w=[pr])
                S.add('dve', lambda e, os_=os_: e.tensor_tensor(out=JKF, in0=os_, in1=os_, op=ALU.mult),
                      r=[pr], w=[pr])
                S.add('dve', lambda e, rc=rc: e.tensor_reduce(out=rc[:, 2:3], in_=JKF, axis=AX.X, op=ALU.add),
                      r=[pr], w=[pr])
                S.add('dve', lambda e, rc=rc: e.tensor_scalar(out=rc[:, 2:3], in0=rc[:, 2:3], scalar1=1.0 / 128,
                                                              scalar2=1e-5, op0=ALU.mult, op1=ALU.add),
                      r=[pr], w=[pr])
                S.add('pool', lambda e, rc=rc: e.tensor_tensor(out=rc[:, 3:4], in0=rc[:, 2:3],
                                                               in1=NLAM[:, 1:2], op=ALU.pow), r=[pr, misc], w=[pr])
                S.add('dve', lambda e, os_=os_, rc=rc, onb=onb: e.scalar_tensor_tensor(
                    out=onb, in0=os_, scalar=rc[:, 3:4], in1=SUBW, op0=ALU.mult, op1=ALU.mult),
                    r=[pr, misc], w=[pr])
                S.add('pe', lambda e, onb=onb: e.transpose(out=PSB[7][:, 0:128], in_=onb, identity=identB),
                      r=[pr, misc], w=[PSr[7]])
                g = (q0 + qb * 128) // 512
                S.add('dve', lambda e, h=h, q0=q0, qb=qb: e.tensor_copy(
                    out=OT[:, h, q0 + qb * 128:q0 + qb * 128 + 128], in_=PSB[7][:, 0:128]),
                    r=[PSr[7]], w=[OTr[g]])

    if DEBUG == 'attn':
        barrier()
        S.add('sp', lambda e: e.dma_start(out=dbg, in_=OT.rearrange("p a b -> p (a b)")), r=OTr, dma=misc)
        S.wait_regs('sp', [misc])
        return finish(nc, S, stack)

    barrier()
    A.off = PASS_MARK
    RING = [A.alloc(BF, 8, 1024) for _ in range(3)]; RINGr = [Reg("ring%d" % i) for i in range(3)]
    ring_i = [0]

    def ring_load(src2d):
        i = ring_i[0] % len(RING); ring_i[0] += 1
        S.add('pool', lambda e, i=i: e.dma_start(out=RING[i], in_=src2d.rearrange("(kc p) n -> p kc n", p=128)),
              w=[RINGr[i]], dma=RINGr[i])
        return RING[i], RINGr[i]

    XG = [A.alloc(F32, D) for _ in range(2)]; XGr = [Reg("xg0"), Reg("xg1")]
    Hb = A.alloc(BF, D); Hr = Reg("h4")
    HTG = A.alloc(BF, 8, 514); HTGr = Reg("htg")
    JUNK = A.alloc(BF, D); SSQ = A.alloc(F32, 4); TMP = A.alloc(F32, D)
    CCs = A.alloc(F32, 512); CCr = Reg("ccs")
    ZP = A.alloc(BF, 8, 514); ZPr = Reg("zp")
    ZH = A.alloc(F32, 32)
    CT = A.alloc(F32, 512)
    U = A.alloc(BF, 8, 512); Ur = Reg("u")
    SG = A.alloc(BF, 512); SGr = Reg("sg")
    MC = A.alloc(BF, 8, 512); MCr = Reg("mc")
    M = U; Mr = Ur
    TMP2 = A.alloc(F32, D)
    X1 = A.alloc(F32, D); X1r = Reg("x1")
    H2 = A.alloc(F32, D); H2r = Reg("h2")
    H2B = A.alloc(BF, D); H2Br = Reg("h2b")
    H2T32 = TMP2.rearrange("p (a b) -> p a b", b=128); H2Tr = Reg("h2t32")
    LG = A.alloc(F32, NE); MX = A.alloc(F32, 8); EX = A.alloc(F32, NE); MK = A.alloc(F32, NE)
    SM = A.alloc(F32, 4)
    rr4 = Reg("r4")
    p4 = Reg("p4")

    COL = dict(cb=3072, cc=4096, cx=5120, ga=6144, gc=7168)
    for g in range(4):
        q0 = g * 512
        for tt in range(5):
            xt = XG[tt % 2]; xr = XGr[tt % 2]
            if tt < 4:
                npart = 128
                S.add('sp', lambda e, xt=xt, q0=q0, tt=tt: e.dma_start(
                    out=xt, in_=xs[q0 + tt * 128:q0 + (tt + 1) * 128, :]), w=[xr], dma=xr)
            else:
                npart = 2
                S.add('sp', lambda e, xt=xt, g=g, q0=q0: e.dma_start(
                    out=xt[0:1, :], in_=(xs[q0 - 1:q0, :] if g > 0 else xh[0:1, :])), w=[xr], dma=xr)
                S.add('sp', lambda e, xt=xt, g=g, q0=q0: e.dma_start(
                    out=xt[1:2, :], in_=(xs[q0 + 512:q0 + 513, :] if g < 3 else xh[1:2, :])), w=[xr], dma=xr)
            norm_tile(xt, xr, W1, SH1, JUNK, SSQ, TMP, [Hb], [Hr], npart=npart, tag="n4")
            for kc in range(8):
                S.add('pe', lambda e, kc=kc, npart=npart: e.transpose(
                    out=PSB[0][:, kc * 128:kc * 128 + npart], in_=Hb[0:npart, kc * 128:(kc + 1) * 128],
                    identity=identB[0:npart, 0:npart]), r=[Hr, misc], w=[PSr[0]])
            c0 = tt * 128
            S.add('act', lambda e, c0=c0, npart=npart: e.copy(
                out=HTG[:, :, c0:c0 + npart], in_=PSB[0].rearrange("p (a b) -> p a b", b=128)[:, :, 0:npart]),
                r=[PSr[0]], w=[HTGr])
        wcc, wccr = ring_load(w_in[:, COL['cc']:COL['cc'] + 1024])
        wcx, wcxr = ring_load(w_in[:, COL['cx']:COL['cx'] + 1024])
        for fc in range(8):
            for (wt, wr, bank) in ((wcc, wccr, 1), (wcx, wcxr, 2)):
                for kc in range(8):
                    S.add('pe', lambda e, wt=wt, bank=bank, kc=kc, fc=fc: e.matmul(
                        PS[bank], lhsT=wt[:, kc, fc * 128:(fc + 1) * 128], rhs=HTG[:, kc, 0:512],
                        start=(kc == 0), stop=(kc == 7)), r=[wr, HTGr], w=[PSr[bank]])
            for wi, (wt, wr) in enumerate(((wcc, wccr), (wcx, wcxr))):
                for kc in range(8):
                    S.add('pe', lambda e, wt=wt, kc=kc, fc=fc, wi=wi: e.matmul(
                        PS[3][:, (fc * 2 + wi) * 2:(fc * 2 + wi) * 2 + 2], lhsT=wt[:, kc, fc * 128:(fc + 1) * 128],
                        rhs=HTG[:, kc, 512:514], start=(kc == 0), stop=(kc == 7)), r=[wr, HTGr], w=[PSr[3]])
            S.add('act', lambda e: e.copy(out=CCs, in_=PS[1]), r=[PSr[1]], w=[CCr])
            S.add('dve', lambda e, fc=fc: e.tensor_tensor(out=ZP[:, fc, 1:513], in0=PS[2], in1=CCs, op=ALU.mult),
                  r=[PSr[2], CCr], w=[ZPr])
        S.add('act', lambda e: e.copy(out=ZH, in_=PS[3][:, 0:32]), r=[PSr[3]], w=[p4])
        zh4 = ZH.rearrange("p (f w c) -> p f w c", w=2, c=2)
        for ci, col in ((0, 0), (1, 513)):
            S.add('dve', lambda e, ci=ci, col=col: e.tensor_tensor(
                out=ZP[:, :, col:col + 1], in0=zh4[:, :, 0, ci:ci + 1], in1=zh4[:, :, 1, ci:ci + 1], op=ALU.mult),
                r=[p4], w=[ZPr])
        if g == 0:
            S.add('dve', lambda e: e.tensor_scalar(out=ZP[:, :, 0:1], in0=ZP[:, :, 0:1], scalar1=HM[:, 0:1],
                                                   scalar2=None, op0=ALU.mult), r=[CONST], w=[ZPr])
        if g == 3:
            S.add('dve', lambda e: e.tensor_scalar(out=ZP[:, :, 513:514], in0=ZP[:, :, 513:514],
                                                   scalar1=HM[:, 1:2], scalar2=None, op0=ALU.mult),
                  r=[CONST], w=[ZPr])
        wcb, wcbr = ring_load(w_in[:, COL['cb']:COL['cb'] + 1024])
        for fc in range(8):
            bank = 1 + fc % 2
            for kc in range(8):
                S.add('pe', lambda e, bank=bank, kc=kc, fc=fc: e.matmul(
                    PS[bank], lhsT=wcb[:, kc, fc * 128:(fc + 1) * 128], rhs=HTG[:, kc, 0:512],
                    start=(kc == 0), stop=(kc == 7)), r=[wcbr, HTGr], w=[PSr[bank]])
            S.add('dve', lambda e, fc=fc: e.tensor_scalar(out=CT, in0=ZP[:, fc, 0:512], scalar1=CW[:, 0, fc:fc + 1],
                                                          scalar2=None, op0=ALU.mult), r=[ZPr, CONST], w=[p4])
            S.add('dve', lambda e, fc=fc: e.scalar_tensor_tensor(out=CT, in0=ZP[:, fc, 1:513], scalar=CW[:, 1, fc:fc + 1],
                                                                 in1=CT, op0=ALU.mult, op1=ALU.add),
                  r=[ZPr, CONST, p4], w=[p4])
            S.add('dve', lambda e, fc=fc: e.scalar_tensor_tensor(out=CT, in0=ZP[:, fc, 2:514], scalar=CW[:, 2, fc:fc + 1],
                                                                 in1=CT, op0=ALU.mult, op1=ALU.add),
                  r=[ZPr, CONST, p4], w=[p4])
            S.add('dve', lambda e, fc=fc, bank=bank: e.tensor_tensor(out=U[:, fc, :], in0=PS[bank], in1=CT,
                                                                      op=ALU.mult), r=[PSr[bank], p4], w=[Ur])
        wco, wcor = ring_load(w_conv_o[:, :])
        wgc, wgcr = ring_load(w_in[:, COL['gc']:COL['gc'] + 1024])
        for dc in range(8):
            ba = 1 + 2 * (dc % 2); bb = ba + 1
            for kc in range(8):
                S.add('pe', lambda e, ba=ba, kc=kc, dc=dc: e.matmul(
                    PS[ba], lhsT=wco[:, kc, dc * 128:(dc + 1) * 128], rhs=U[:, kc, :],
                    start=(kc == 0), stop=(kc == 7)), r=[wcor, Ur], w=[PSr[ba]])
            for kc in range(8):
                S.add('pe', lambda e, bb=bb, kc=kc, dc=dc: e.matmul(
                    PS[bb], lhsT=wgc[:, kc, dc * 128:(dc + 1) * 128], rhs=HTG[:, kc, 0:512],
                    start=(kc == 0), stop=(kc == 7)), r=[wgcr, HTGr], w=[PSr[bb]])
            S.add('act', lambda e, bb=bb: e.activation(out=SG, in_=PS[bb], func=AF.Sigmoid), r=[PSr[bb]], w=[SGr])
            S.add('dve', lambda e, ba=ba, dc=dc: e.tensor_tensor(out=MC[:, dc, :], in0=PS[ba], in1=SG, op=ALU.mult),
                  r=[PSr[ba], SGr], w=[MCr])
        wao, waor = ring_load(w_attn_o[:, :])
        wga, wgar = ring_load(w_in[:, COL['ga']:COL['ga'] + 1024])
        for dc in range(8):
            ba = 1 + 2 * (dc % 2); bb = ba + 1
            for kc in range(8):
                S.add('pe', lambda e, ba=ba, kc=kc, dc=dc: e.matmul(
                    PS[ba], lhsT=wao[:, kc, dc * 128:(dc + 1) * 128], rhs=OT[:, kc, q0:q0 + 512],
                    start=(kc == 0), stop=(kc == 7)), r=[waor, OTr[g]], w=[PSr[ba]])
            for kc in range(8):
                S.add('pe', lambda e, bb=bb, kc=kc, dc=dc: e.matmul(
                    PS[bb], lhsT=wga[:, kc, dc * 128:(dc + 1) * 128], rhs=HTG[:, kc, 0:512],
                    start=(kc == 0), stop=(kc == 7)), r=[wgar, HTGr], w=[PSr[bb]])
            S.add('act', lambda e, bb=bb: e.activation(out=SG, in_=PS[bb], func=AF.Sigmoid), r=[PSr[bb]], w=[SGr])
            S.add('dve', lambda e, ba=ba: e.tensor_tensor(out=CT, in0=PS[ba], in1=SG, op=ALU.mult),
                  r=[PSr[ba], SGr], w=[p4])
            S.add('pool', lambda e, dc=dc: e.tensor_tensor(out=M[:, dc, :], in0=CT, in1=MC[:, dc, :], op=ALU.add),
                  r=[p4, MCr], w=[Mr])
        if DEBUG == 'p4' and g == 3:
            S.add('sp', lambda e: e.dma_start(out=dbg[:, 0, :], in_=ZP.rearrange("p a b -> p (a b)")), r=[ZPr], dma=misc)
            S.add('sp', lambda e: e.dma_start(out=dbg[:, 1, 0:4096], in_=MC.rearrange("p a b -> p (a b)")), r=[MCr], dma=misc)
            S.add('sp', lambda e: e.dma_start(out=dbg[:, 2, 0:4096], in_=M.rearrange("p a b -> p (a b)")), r=[Mr], dma=misc)
            S.add('sp', lambda e: e.dma_start(out=dbg[:, 3, :], in_=HTG.rearrange("p a b -> p (a b)")), r=[HTGr], dma=misc)
            S.wait_regs('sp', [misc])
            return finish(nc, S, stack)
        wo, wor = ring_load(w_out[:, :])
        for tt in range(4):
            tile_i = g * 4 + tt
            xt = XG[tt % 2]; xr = XGr[tt % 2]
            S.add('sp', lambda e, xt=xt, q0=q0, tt=tt: e.dma_start(
                out=xt, in_=xs[q0 + tt * 128:q0 + (tt + 1) * 128, :]), w=[xr], dma=xr)
            for half in range(2):
                bank = 5 + half
                for kc in range(8):
                    S.add('pe', lambda e, bank=bank, kc=kc, tt=tt, half=half: e.matmul(
                        PS[bank], lhsT=M[:, kc, tt * 128:(tt + 1) * 128], rhs=wo[:, kc, half * 512:(half + 1) * 512],
                        start=(kc == 0), stop=(kc == 7)), r=[Mr, wor], w=[PSr[bank]])
                S.add('dve', lambda e, bank=bank, half=half: e.tensor_tensor(
                    out=TMP2[:, half * 512:(half + 1) * 512], in0=PS[bank], in1=G1[:, half * 512:(half + 1) * 512],
                    op=ALU.mult), r=[PSr[bank], misc], w=[H2Tr])
            S.add('pool', lambda e, xt=xt: e.tensor_tensor(out=X1, in0=TMP2, in1=xt, op=ALU.add),
                  r=[H2Tr, xr], w=[X1r])
            x1dst = y if DEBUG == 'x1' else X1d
            S.add('sp', lambda e, tile_i=tile_i, x1dst=x1dst: e.dma_start(
                out=x1dst[tile_i * 128:(tile_i + 1) * 128, :], in_=X1), r=[X1r], dma=X1r)
            norm_tile(X1, X1r, W2, SH2, JUNK, SSQ, TMP, [H2, H2B], [H2r, H2Br], tag="n2")
            for kc in range(8):
                bank = 1 + kc // 4
                S.add('pe', lambda e, kc=kc, bank=bank: e.transpose(
                    out=PS[bank][:, (kc % 4) * 128:(kc % 4 + 1) * 128], in_=H2[:, kc * 128:(kc + 1) * 128],
                    identity=identF), r=[H2r, CONST], w=[PSr[bank]])
            for b2 in range(2):
                S.add('act', lambda e, b2=b2: e.copy(out=H2T32[:, b2 * 4:(b2 + 1) * 4, :].rearrange("p a b -> p (a b)"),
                                                     in_=PS[1 + b2]), r=[PSr[1 + b2]], w=[H2Tr])
            for kc in range(8):
                S.add('pe', lambda e, kc=kc: e.matmul(PS[3][:, 0:NE], lhsT=H2T32[:, kc, :], rhs=WRT[:, kc, :],
                                                      start=(kc == 0), stop=(kc == 7)),
                      r=[H2Tr, CONST], w=[PSr[3]])
            S.add('dve', lambda e: e.tensor_tensor(out=LG, in0=PS[3][:, 0:NE], in1=BR, op=ALU.add),
                  r=[PSr[3], CONST], w=[rr4])
            S.add('dve', lambda e: e.max(out=MX, in_=LG), r=[rr4], w=[rr4])
            S.add('dve', lambda e: e.tensor_scalar(out=MK, in0=LG, scalar1=MX[:, 3:4], scalar2=None, op0=ALU.is_ge),
                  r=[rr4], w=[rr4])
            S.add('dve', lambda e: e.tensor_scalar(out=SM[:, 0:1], in0=MX[:, 0:1], scalar1=-1.0, scalar2=None,
                                                   op0=ALU.mult), r=[rr4], w=[rr4])
            S.add('act', lambda e: e.activation(out=EX, in_=LG, func=AF.Exp, bias=SM[:, 0:1]), r=[rr4], w=[rr4])
            S.add('dve', lambda e: e.tensor_tensor(out=EX, in0=EX, in1=MK, op=ALU.mult), r=[rr4], w=[rr4])
            S.add('dve', lambda e: e.tensor_reduce(out=SM[:, 1:2], in_=EX, axis=AX.X, op=ALU.add), r=[rr4], w=[rr4])
            S.add('dve', lambda e: e.reciprocal(out=SM[:, 2:3], in_=SM[:, 1:2]), r=[rr4], w=[rr4])
            S.add('dve', lambda e, tile_i=tile_i: e.tensor_scalar(out=WG[:, tile_i, :], in0=EX, scalar1=SM[:, 2:3],
                                                                   scalar2=None, op0=ALU.mult), r=[rr4], w=[WGr])
            for kc in range(8):
                S.add('pe', lambda e, kc=kc: e.transpose(out=PSB[7][:, kc * 128:(kc + 1) * 128],
                                                         in_=H2B[:, kc * 128:(kc + 1) * 128], identity=identB),
                      r=[H2Br, misc], w=[PSr[7]])
            S.add('act', lambda e, q0=q0, tt=tt: e.copy(
                out=OT[:, :, q0 + tt * 128:q0 + (tt + 1) * 128], in_=PSB[7].rearrange("p (a b) -> p a b", b=128)),
                r=[PSr[7]], w=[OTr[g]])

    if DEBUG == 'x1':
        barrier()
        S.add('sp', lambda e: e.dma_start(out=dbg.rearrange("(t p) e -> p t e", p=128), in_=WG), r=[WGr], dma=misc)
        S.wait_regs('sp', [misc])
        return finish(nc, S, stack)

    barrier()
    A.off = P5_MARK
    H2T = OT
    ACC = A.alloc(F32, 16, D); ACCr = [Reg("acc%d" % i) for i in range(16)]
    BGg = A.alloc(F32, 8, NE); BGu = A.alloc(F32, 8, NE)
    LOOP_MARK = A.off
    BGrow = A.alloc(F32, 2048)
    BD = A.alloc(F32, D)
    WGT = A.alloc(F32, 16, 128)
    p5 = Reg("p5")
    S.add('sp', lambda e: e.dma_start(out=BGrow[0:NE, :], in_=b_gate_up), w=[p5], dma=p5)
    S.add('sp', lambda e: e.dma_start(out=BD[0:NE, :], in_=b_down), dma=p5)
    p5.w = ('dma', p5, p5.dcnt)
    bg3 = BGrow.rearrange("p (f m t) -> p f m t", m=128, t=2)
    for fc in range(8):
        S.add('pe', lambda e, fc=fc: e.transpose(out=PS[0][:, fc * NE:(fc + 1) * NE],
                                                 in_=bg3[0:NE, fc, :, 0],
                                                 identity=identF[0:NE, 0:NE]), r=[p5, CONST], w=[PSr[0]])
        S.add('pe', lambda e, fc=fc: e.transpose(out=PS[0][:, 256 + fc * NE:256 + (fc + 1) * NE],
                                                 in_=bg3[0:NE, fc, :, 1],
                                                 identity=identF[0:NE, 0:NE]), r=[p5, CONST], w=[PSr[0]])
    bgr = Reg("bg")
    S.add('dve', lambda e: e.tensor_copy(out=BGg.rearrange("p a b -> p (a b)"), in_=PS[0][:, 0:256]),
          r=[PSr[0]], w=[bgr])
    S.add('dve', lambda e: e.tensor_scalar(out=BGu.rearrange("p a b -> p (a b)"), in0=PS[0][:, 256:512],
                                           scalar1=1.0, scalar2=None, op0=ALU.add), r=[PSr[0]], w=[bgr])
    for ti in range(16):
        S.add('pe', lambda e, ti=ti: e.transpose(out=PS[1][0:NE, 0:128], in_=WG[:, ti, :], identity=identF),
              r=[WGr, CONST], w=[PSr[1]])
        S.add('dve', lambda e, ti=ti: e.tensor_copy(out=WGT[0:NE, ti, :], in_=PS[1][0:NE, 0:128]),
              r=[PSr[1]], w=[bgr])
        for half in range(2):
            S.add('pe', lambda e, ti=ti, half=half: e.matmul(
                PS[2 + half], lhsT=WGT[0:NE, ti, :], rhs=BD[0:NE, half * 512:(half + 1) * 512],
                start=True, stop=True), r=[bgr, p5], w=[PSr[2 + half]])
            S.add('act', lambda e, ti=ti, half=half: e.copy(out=ACC[:, ti, half * 512:(half + 1) * 512],
                                                            in_=PS[2 + half]), r=[PSr[2 + half]], w=[ACCr[ti]])
    barrier()
    A.off = LOOP_MARK
    RING5 = [A.alloc(BF, 8, 1024) for _ in range(3)]; RING5r = [Reg("r5_%d" % i) for i in range(3)]
    r5_i = [0]

    def ring5_load(src2d):
        i = r5_i[0] % 3; r5_i[0] += 1
        S.add('pool', lambda e, i=i: e.dma_start(out=RING5[i], in_=src2d.rearrange("(kc p) n -> p kc n", p=128)),
              w=[RING5r[i]], dma=RING5r[i])
        return RING5[i], RING5r[i]

    AT = [A.alloc(BF, 8, 512) for _ in range(2)]; ATr = [Reg("at0"), Reg("at1")]
    GS = [A.alloc(F32, 512) for _ in range(2)]; GSr = [Reg("gs0"), Reg("gs1")]
    SGM = [A.alloc(F32, 512) for _ in range(2)]
    U1 = [A.alloc(F32, 512) for _ in range(2)]

    it5 = [0]
    for ex in range(NE):
        wgA, wgAr = ring5_load(w_gate_up[ex, :, 0:1024])
        wgB, wgBr = ring5_load(w_gate_up[ex, :, 1024:2048])
        wd, wdr = ring5_load(w_down[ex, :, :])
        for tg in range(4):
            at = AT[tg % 2]; atr = ATr[tg % 2]
            for fc in range(8):
                wt, wr = (wgA, wgAr) if fc < 4 else (wgB, wgBr)
                wv = wt.rearrange("p k (f m t) -> p k f m t", m=128, t=2)
                k = it5[0] % 2; it5[0] += 1
                bg_, bu_ = 4 * k, 4 * k + 1
                for (bank, off) in ((bg_, 0), (bu_, 1)):
                    for kc in range(8):
                        S.add('pe', lambda e, bank=bank, off=off, wv=wv, kc=kc, fc=fc, tg=tg: e.matmul(
                            PS[bank], lhsT=wv[:, kc, fc % 4, :, off],
                            rhs=H2T[:, kc, tg * 512:(tg + 1) * 512], start=(kc == 0), stop=(kc == 7)),
                            r=[wr, OTr[tg]], w=[PSr[bank]])
                gs = GS[k]; gsr = GSr[k]; sg = SGM[k]; u1 = U1[k]
                S.add('dve', lambda e, gs=gs, bg_=bg_, fc=fc, ex=ex: e.tensor_scalar(
                    out=gs, in0=PS[bg_], scalar1=BGg[:, fc, ex:ex + 1], scalar2=7.0, op0=ALU.add, op1=ALU.min),
                    r=[PSr[bg_], bgr], w=[gsr])
                S.add('act', lambda e, gs=gs, sg=sg: e.activation(out=sg, in_=gs, func=AF.Sigmoid, scale=1.702),
                      r=[gsr], w=[gsr])
                S.add('dve', lambda e, u1=u1, bu_=bu_, fc=fc, ex=ex: e.tensor_scalar(
                    out=u1, in0=PS[bu_], scalar1=BGu[:, fc, ex:ex + 1], scalar2=8.0, op0=ALU.add, op1=ALU.min),
                    r=[PSr[bu_], bgr], w=[gsr])
                S.add('dve', lambda e, gs=gs, sg=sg: e.tensor_tensor(out=gs, in0=gs, in1=sg, op=ALU.mult),
                      r=[gsr], w=[gsr])
                S.add('dve', lambda e, u1=u1, gs=gs, at=at, fc=fc: e.scalar_tensor_tensor(
                    out=at[:, fc, :], in0=u1, scalar=-6.0, in1=gs, op0=ALU.max, op1=ALU.mult),
                    r=[gsr], w=[atr])
            for tt in range(4):
                ti = tg * 4 + tt
                for half in range(2):
                    bank = 2 + half
                    for fc in range(8):
                        S.add('pe', lambda e, bank=bank, fc=fc, at=at, tt=tt, half=half: e.matmul(
                            PS[bank], lhsT=at[:, fc, tt * 128:(tt + 1) * 128],
                            rhs=wd[:, fc, half * 512:(half + 1) * 512], start=(fc == 0), stop=(fc == 7)),
                            r=[atr, wdr], w=[PSr[bank]])
                    S.add('dve', lambda e, bank=bank, ti=ti, half=half, ex=ex: e.scalar_tensor_tensor(
                        out=ACC[:, ti, half * 512:(half + 1) * 512], in0=PS[bank], scalar=WG[:, ti, ex:ex + 1],
                        in1=ACC[:, ti, half * 512:(half + 1) * 512], op0=ALU.mult, op1=ALU.add),
                        r=[PSr[bank], WGr], w=[ACCr[ti]])

    barrier()
    A.off = LOOP_MARK
    XF = [A.alloc(F32, D) for _ in range(2)]; XFr = [Reg("xf0"), Reg("xf1")]
    for ti in range(16):
        xf = XF[ti % 2]; xfr = XFr[ti % 2]
        S.add('sp', lambda e, xf=xf, ti=ti: e.dma_start(out=xf, in_=X1d[ti * 128:(ti + 1) * 128, :]),
              w=[xfr], dma=xfr)
        S.add('dve', lambda e, ti=ti: e.tensor_tensor(out=ACC[:, ti, :], in0=ACC[:, ti, :], in1=G2, op=ALU.mult),
              r=[CONST, misc], w=[ACCr[ti]])
        S.add('pool', lambda e, ti=ti, xf=xf: e.tensor_tensor(out=xf, in0=ACC[:, ti, :], in1=xf, op=ALU.add),
              r=[ACCr[ti]], w=[xfr])
        S.add('sp', lambda e, xf=xf, ti=ti: e.dma_start(out=y[ti * 128:(ti + 1) * 128, :], in_=xf),
              r=[xfr], dma=xfr)
    S.wait_regs('sp', XFr)
    return finish(nc, S, stack)


def finish(nc, S, stack):
    S.prepare(nc, stack)
    with nc.Block() as block:
        @block.tensor
        def _(e):
            S.run('pe', e)

        @block.scalar
        def _(e):
            S.run('act', e)

        @block.vector
        def _(e):
            S.run('dve', e)

        @block.gpsimd
        def _(e):
            S.run('pool', e)

        @block.sync
        def _(e):
            S.run('sp', e)
    stack.close()
    return nc


def rope_table():
    inv = (np.float32(10000.0) ** (-(np.arange(0, 64, 2, dtype=np.float32) / np.float32(64)))).astype(np.float32)
    pos = np.arange(SEQ, dtype=np.float32)
    ang = (pos[:, None] * inv[None, :]).astype(np.float32)
    emb = np.concatenate([ang, ang], axis=-1)
    cos = np.cos(emb).astype(np.float32); sin = np.sin(emb).astype(np.float32)
    sin[:, 0:32] = -sin[:, 0:32]
    return np.concatenate([cos, sin], axis=1).astype(np.float32)


def make_in_maps(inp):
    x = np.asarray(inp['x'], dtype=np.float32)
    tab = rope_table()
    shared = {}
    for k in ('b_ada', 'norm1_w', 'q_norm_w', 'k_norm_w', 'lambda_q1', 'lambda_k1', 'lambda_q2', 'lambda_k2',
              'subln_w', 'norm2_w', 'b_router'):
        shared[k] = np.ascontiguousarray(np.asarray(inp[k], dtype=np.float32))
    for k in ('w_ada', 'w_in', 'w_attn_o', 'conv_w', 'w_conv_o', 'w_out', 'w_router', 'w_gate_up', 'b_gate_up',
              'w_down', 'b_down'):
        shared[k] = np.ascontiguousarray(np.asarray(inp[k], dtype=np.float32)[0])
    shared['ident'] = np.eye(128, dtype=np.float32)
    maps = []
    for core in range(8):
        b = core // 4; t0 = (core % 4) * TOWN
        m = dict(shared)
        m['xs'] = np.ascontiguousarray(np.roll(x[b], -t0, axis=0))
        m['rope'] = np.ascontiguousarray(np.roll(tab, -t0, axis=0))
        xhal = np.zeros((2, D), np.float32); msk = np.zeros((128, 2), np.float32)
        if t0 > 0:
            xhal[0] = x[b, t0 - 1]; msk[:, 0] = 1.0
        if t0 + TOWN < SEQ:
            xhal[1] = x[b, t0 + TOWN]; msk[:, 1] = 1.0
        m['xh'] = xhal; m['hm'] = msk
        m['cvec'] = np.ascontiguousarray(np.asarray(inp['c'], dtype=np.float32)[b:b + 1])
        maps.append(m)
    return maps


_NC = None


def kernel(**inp):
    global _NC
    if _NC is None:
        _NC = build()
    maps = make_in_maps(inp)
    res = run_bass_kernel_spmd(_NC, maps, core_ids=list(range(8)))
    out = np.zeros((2, SEQ, D), np.float32)
    for core in range(8):
        b = core // 4; t0 = (core % 4) * TOWN
        out[b, t0:t0 + TOWN] = res.results[core]['y']
    return out
```

```python
import types
import numpy as np
from contextlib import ExitStack
import concourse.bass as bass
import concourse.mybir as mybir
from concourse.bass_utils import run_bass_kernel_spmd

F32 = mybir.dt.float32
BF = mybir.dt.bfloat16
ALU = mybir.AluOpType
AF = mybir.ActivationFunctionType
AX = mybir.AxisListType

SEQ = 8192
D = 1024
TOWN = 2048
NE = 32
ENGS = ['pe', 'act', 'dve', 'pool', 'sp']
DEBUG = None
MOE_GATHER = False
CAP = 384


class Reg:
    __slots__ = ('name', 'w', 'rs', 'dsem', 'dcnt', 'excl')
    ALL = []

    def __init__(s, name, excl=False):
        s.name = name; s.w = None; s.rs = {}; s.dsem = None; s.dcnt = 0; s.excl = excl
        Reg.ALL.append(s)


class Op:
    __slots__ = ('eng', 'fn', 'deps', 'needs_inc', 'sigval', 'dreg')


def freeze(fn):
    if fn is None or fn.__closure__ is None:
        return fn
    cells = []
    for c in fn.__closure__:
        try:
            cells.append(types.CellType(c.cell_contents))
        except ValueError:
            cells.append(c)
    return types.FunctionType(fn.__code__, fn.__globals__, fn.__name__, fn.__defaults__, tuple(cells))


class Sched:
    def __init__(s):
        s.q = {e: [] for e in ENGS}
        s.dregs = []

    def add(s, eng, fn, r=(), w=(), dma=None):
        op = Op(); op.eng = eng; op.fn = freeze(fn); op.needs_inc = False; op.sigval = 0; op.dreg = dma
        w = list(w) + [g for g in r if g.excl]
        r = [g for g in r if not g.excl]
        deps = []
        for g in r:
            if g.w is not None:
                deps.append(g.w)
        for g in w:
            if g.w is not None:
                deps.append(g.w)
            deps.extend(g.rs.values())
        op.deps = [d for d in deps if not (d[0] == 'op' and d[1].eng == eng and eng == 'pe')]
        if dma is not None:
            if dma.dcnt == 0 and dma not in s.dregs:
                s.dregs.append(dma)
            dma.dcnt += 16
            tok = ('dma', dma, dma.dcnt); key = ('d', id(dma))
        else:
            tok = ('op', op); key = eng
        for g in r:
            g.rs[key] = tok
        for g in w:
            g.w = tok; g.rs = {}
        s.q[eng].append(op)
        return op

    def wait_regs(s, eng, regs):
        op = Op(); op.eng = eng; op.fn = None; op.needs_inc = False; op.sigval = 0; op.dreg = None
        deps = []
        for g in regs:
            if g.w is not None:
                deps.append(g.w)
            deps.extend(g.rs.values())
        op.deps = deps
        s.q[eng].append(op)

    def barrier(s):
        deps = []
        for e in ENGS:
            for op in reversed(s.q[e]):
                if op.fn is not None and op.dreg is None:
                    deps.append(('op', op))
                    break
        for g in s.dregs:
            if g.dcnt > 0:
                deps.append(('dma', g, g.dcnt))
        for e in ENGS:
            op = Op(); op.eng = e; op.fn = None; op.needs_inc = False; op.sigval = 0; op.dreg = None
            op.deps = [d for d in deps if not (d[0] == 'op' and d[1].eng == e and e == 'pe')]
            s.q[e].append(op)
        for g in Reg.ALL:
            g.w = None; g.rs = {}

    def prepare(s, nc, stack):
        for e in ENGS:
            for op in s.q[e]:
                for d in op.deps:
                    if d[0] == 'op':
                        d[1].needs_inc = True
        for e in ENGS:
            c = 0
            for op in s.q[e]:
                if op.needs_inc:
                    c += 1
                    op.sigval = c
        s.esem = {e: stack.enter_context(nc.semaphore("sem_" + e)) for e in ENGS}
        for i, g in enumerate(s.dregs):
            g.dsem = stack.enter_context(nc.semaphore("dsem%d_%s" % (i, g.name)))

    def run(s, e, eng):
        known = {}
        for op in s.q[e]:
            for d in op.deps:
                if d[0] == 'op':
                    sem = s.esem[d[1].eng]; val = d[1].sigval; k = d[1].eng
                else:
                    sem = d[1].dsem; val = d[2]; k = id(d[1])
                if known.get(k, 0) < val:
                    eng.wait_ge(sem, val)
                    known[k] = val
            if op.fn is None:
                continue
            ins = op.fn(eng)
            if op.dreg is not None:
                ins.then_inc(op.dreg.dsem, 16)
            elif op.needs_inc:
                ins.then_inc(s.esem[e], 1)


class Arena:
    def __init__(s, t, nbytes):
        s.t = t; s.off = 0; s.cap = nbytes

    def alloc(s, dtype, *free):
        n = 1
        for f in free:
            n *= f
        esz = 2 if dtype == BF else 4
        nbytes = (n * esz + 63) // 64 * 64
        assert s.off + nbytes <= s.cap, ("arena overflow", s.off, nbytes, s.cap)
        a = s.t[:, s.off // 4:(s.off + nbytes) // 4]
        s.off += nbytes
        if dtype != F32:
            a = a.bitcast(dtype)
        a = a[:, 0:n]
        if len(free) == 2:
            a = a.rearrange("p (a b) -> p a b", b=free[1])
        elif len(free) == 3:
            a = a.rearrange("p (a b c) -> p a b c", b=free[1], c=free[2])
        return a


def build():
    nc = bass.Bass("TRN2", target_bir_lowering=False)

    def din(name, shape, dt=F32):
        return nc.dram_tensor(name, list(shape), dt, kind="ExternalInput").ap()

    xs = din("xs", [SEQ, D]); xh = din("xh", [2, D]); hm = din("hm", [128, 2])
    rope = din("rope", [SEQ, 128]); cvec = din("cvec", [1, D]); ident = din("ident", [128, 128])
    w_ada = din("w_ada", [D, 6 * D]); b_ada = din("b_ada", [1, 6 * D])
    norm1_w = din("norm1_w", [1, D]); w_in = din("w_in", [D, 8 * D])
    q_norm_w = din("q_norm_w", [1, 64]); k_norm_w = din("k_norm_w", [1, 64])
    lq1 = din("lambda_q1", [1, 64]); lk1 = din("lambda_k1", [1, 64])
    lq2 = din("lambda_q2", [1, 64]); lk2 = din("lambda_k2", [1, 64])
    subln_w = din("subln_w", [1, 128]); w_attn_o = din("w_attn_o", [D, D])
    conv_w = din("conv_w", [3, D]); w_conv_o = din("w_conv_o", [D, D]); w_out = din("w_out", [D, D])
    norm2_w = din("norm2_w", [1, D]); w_router = din("w_router", [D, NE]); b_router = din("b_router", [1, NE])
    w_gate_up = din("w_gate_up", [NE, D, 2 * D]); b_gate_up = din("b_gate_up", [NE, 2 * D])
    w_down = din("w_down", [NE, D, D]); b_down = din("b_down", [NE, D])
    y = nc.dram_tensor("y", [TOWN, D], F32, kind="ExternalOutput").ap()
    KTd = nc.dram_tensor("ktd", [128, 8, SEQ], BF, kind="Internal").ap()
    Vd = nc.dram_tensor("vd", [8, SEQ, 128], BF, kind="Internal").ap()
    X1d = nc.dram_tensor("x1d", [TOWN, D], F32, kind="Internal").ap()
    H2d = nc.dram_tensor("h2d", [TOWN, D], BF, kind="Internal").ap()
    tri = din("tri", [128, 128]); iota = din("iota", [128, CAP])
    dbg = None
    if DEBUG == 'attn':
        dbg = nc.dram_tensor("dbg", [128, 8 * TOWN], BF, kind="ExternalOutput").ap()
    elif DEBUG == 'x1':
        dbg = nc.dram_tensor("dbg", [TOWN, 32], F32, kind="ExternalOutput").ap()
    elif DEBUG == 'p4':
        dbg = nc.dram_tensor("dbg", [128, 4, 8 * 514], BF, kind="ExternalOutput").ap()

    S = Sched()
    stack = ExitStack()
    ARENA_BYTES = 188 * 1024
    arena_t = stack.enter_context(nc.sbuf_tensor("arena", [128, ARENA_BYTES // 4], F32))
    A = Arena(arena_t, ARENA_BYTES)
    PS = []
    PSr = []
    SXYs = []
    for i in range(2):
        sxy = stack.enter_context(nc.psum_tensor("sxy%d" % i, [128, 1024], F32))
        SXYs.append(sxy[:, :])
        PS.append(sxy[:, 0:512]); PS.append(sxy[:, 512:1024])
    for i in range(4, 8):
        t = stack.enter_context(nc.psum_tensor("ps%d" % i, [128, 512], F32))
        PS.append(t[:, :])
    for i in range(8):
        PSr.append(Reg("ps%d" % i, excl=True))
    PSB = [p.bitcast(BF) for p in PS]

    identF = A.alloc(F32, 128); identB = A.alloc(BF, 128)
    OT = A.alloc(BF, 8, TOWN)
    G2 = A.alloc(F32, D)
    WG = A.alloc(F32, 16, NE)
    P5_MARK = A.off
    SH1 = A.alloc(F32, D); G1 = A.alloc(F32, D); SH2 = A.alloc(F32, D)
    W1 = A.alloc(F32, D); W2 = A.alloc(F32, D)
    KNW = A.alloc(F32, 64); KNWS = A.alloc(F32, 64); QNW = A.alloc(F32, 64); QNWS = A.alloc(F32, 64)
    SUBW = A.alloc(F32, 128); NLAM = A.alloc(F32, 2)
    CW = A.alloc(F32, 3, 8); HM = A.alloc(F32, 2); BR = A.alloc(F32, NE); WRT = A.alloc(F32, 8, NE)
    PASS_MARK = A.off

    CONST = Reg("const")
    OTr = [Reg("ot%d" % g) for g in range(4)]
    WGr = Reg("wg")
    misc = Reg("misc")

    def cload(out, in_, **kw):
        S.add('sp', lambda e: e.dma_start(out=out, in_=in_, **kw), dma=CONST)

    def row_b(ap1d):
        return ap1d.partition_broadcast(128)

    SC1 = A.alloc(F32, D); SC2 = A.alloc(F32, D)
    cT = A.alloc(F32, 8); scT = A.alloc(F32, 8); scB = A.alloc(F32, 8, 128)
    LQ = [A.alloc(F32, 64) for _ in range(4)]
    LT = A.alloc(F32, 8)
    WA = [A.alloc(F32, 8, 512) for _ in range(2)]
    WAr = [Reg("wa0"), Reg("wa1")]

    cload(identF, ident)
    mod_tiles = [SH1, SC1, G1, SH2, SC2, G2]
    for i, t in enumerate(mod_tiles):
        cload(t, row_b(b_ada[0, i * D:(i + 1) * D]))
    cload(W1, row_b(norm1_w[0, :])); cload(W2, row_b(norm2_w[0, :]))
    cload(KNW, row_b(k_norm_w[0, :])); cload(QNW, row_b(q_norm_w[0, :]))
    cload(KNWS[:, 0:32], row_b(k_norm_w[0, 32:64])); cload(KNWS[:, 32:64], row_b(k_norm_w[0, 0:32]))
    cload(QNWS[:, 0:32], row_b(q_norm_w[0, 32:64])); cload(QNWS[:, 32:64], row_b(q_norm_w[0, 0:32]))
    cload(SUBW, row_b(subln_w[0, :]))
    for t, src in zip(LQ, (lq1, lk1, lq2, lk2)):
        cload(t, row_b(src[0, :]))
    cload(HM, hm); cload(BR, row_b(b_router[0, :]))
    cload(WRT, w_router.rearrange("(kc p) e -> p kc e", p=128))
    for k3 in range(3):
        cload(CW[:, k3, :], conv_w[k3, :].rearrange("(j p) -> p j", p=128), allow_slow_non_contiguous=True)
    cload(cT, cvec.rearrange("o (j p) -> p (o j)", p=128), allow_slow_non_contiguous=True)
    CONST.w = ('dma', CONST, CONST.dcnt)

    S.add('dve', lambda e: e.tensor_copy(out=identB, in_=identF), r=[CONST], w=[misc])
    S.add('pool', lambda e: e.memset(NLAM[:, 1:2], -0.5), w=[misc])
    S.add('act', lambda e: e.activation(out=scT, in_=cT, func=AF.Sigmoid), r=[CONST], w=[misc])
    S.add('dve', lambda e: e.tensor_tensor(out=scT, in0=scT, in1=cT, op=ALU.mult), r=[misc, CONST], w=[misc])
    S.add('dve', lambda e: e.tensor_copy(out=scB, in_=scT.unsqueeze(2).broadcast_to([128, 8, 128])),
          r=[misc], w=[misc])
    for i in range(2):
        S.add('dve', lambda e, i=i: e.tensor_tensor(out=LQ[2 * i], in0=LQ[2 * i], in1=LQ[2 * i + 1], op=ALU.mult),
              r=[CONST, misc], w=[misc])
        S.add('dve', lambda e, i=i: e.tensor_reduce(out=LT[:, i:i + 1], in_=LQ[2 * i], axis=AX.X, op=ALU.add),
              r=[misc], w=[misc])
    S.add('act', lambda e: e.activation(out=LT[:, 2:4], in_=LT[:, 0:2], func=AF.Exp), r=[misc], w=[misc])
    S.add('dve', lambda e: e.tensor_tensor(out=LT[:, 4:5], in0=LT[:, 2:3], in1=LT[:, 3:4], op=ALU.subtract),
          r=[misc], w=[misc])
    S.add('dve', lambda e: e.tensor_scalar(out=NLAM[:, 0:1], in0=LT[:, 4:5], scalar1=0.2, scalar2=-1.0,
                                           op0=ALU.add, op1=ALU.mult), r=[misc], w=[misc])
    S.add('dve', lambda e: e.tensor_scalar(out=SUBW, in0=SUBW, scalar1=0.8, scalar2=None, op0=ALU.mult),
          r=[CONST, misc], w=[misc])

    for cg in range(12):
        wa = WA[cg % 2]; war = WAr[cg % 2]
        S.add('sp', lambda e, wa=wa, cg=cg: e.dma_start(
            out=wa, in_=w_ada[:, cg * 512:(cg + 1) * 512].rearrange("(kc p) n -> p kc n", p=128)),
            w=[war], dma=war)
        bank = cg % 2
        for kc in range(8):
            S.add('pe', lambda e, wa=wa, kc=kc, bank=bank: e.matmul(
                PS[bank], lhsT=scB[:, kc, :], rhs=wa[:, kc, :], start=(kc == 0), stop=(kc == 7)),
                r=[misc, war], w=[PSr[bank]])
        dst = mod_tiles[cg // 2][:, (cg % 2) * 512:(cg % 2 + 1) * 512]
        S.add('dve', lambda e, dst=dst, bank=bank: e.tensor_tensor(out=dst, in0=PS[bank], in1=dst, op=ALU.add),
              r=[PSr[bank], CONST, misc], w=[misc])
    S.add('dve', lambda e: e.scalar_tensor_tensor(out=W1, in0=SC1, scalar=1.0, in1=W1, op0=ALU.add, op1=ALU.mult),
          r=[misc, CONST], w=[misc])
    S.add('dve', lambda e: e.scalar_tensor_tensor(out=W2, in0=SC2, scalar=1.0, in1=W2, op0=ALU.add, op1=ALU.mult),
          r=[misc, CONST], w=[misc])

    barrier = S.barrier

    nrm_r = Reg("nrm")

    def norm_tile(xt, xr, Wt, SHt, junk, ssq, tmp, outs, regs_out, npart=128, tag="n"):
        tr = nrm_r
        S.add('act', lambda e: e.activation(out=junk[0:npart], in_=xt[0:npart], func=AF.Square,
                                            accum_out=ssq[0:npart, 0:1]), r=[xr], w=[tr])
        S.add('act', lambda e: e.activation(out=ssq[0:npart, 1:2], in_=ssq[0:npart, 0:1], func=AF.Sqrt,
                                            scale=1.0 / D, bias=1e-6), r=[tr], w=[tr])
        S.add('dve', lambda e: e.reciprocal(out=ssq[0:npart, 2:3], in_=ssq[0:npart, 1:2]), r=[tr], w=[tr])
        S.add('dve', lambda e: e.scalar_tensor_tensor(out=tmp[0:npart], in0=xt[0:npart], scalar=ssq[0:npart, 2:3],
                                                      in1=Wt[0:npart], op0=ALU.mult, op1=ALU.mult),
              r=[tr, xr, misc], w=[tr])
        for o, ro in zip(outs, regs_out):
            S.add('dve', lambda e, o=o: e.tensor_tensor(out=o[0:npart], in0=tmp[0:npart], in1=SHt[0:npart],
                                                        op=ALU.add), r=[tr, misc, xr], w=[ro])

    barrier()
    A.off = PASS_MARK
    QT = A.alloc(BF, 8, TOWN); QTr = Reg("qt")
    WKV = A.alloc(BF, 8, 2048); WKVr = Reg("wkv")
    WQ = A.alloc(BF, 8, 1024); WQr = Reg("wq")
    XT = [A.alloc(F32, D) for _ in range(2)]; XTr = [Reg("xt0"), Reg("xt1")]
    RT = [A.alloc(F32, 128) for _ in range(2)]; RTr = [Reg("rt0"), Reg("rt1")]
    Hb2 = [A.alloc(BF, D) for _ in range(2)]; Hr2 = [Reg("h0"), Reg("h1")]
    HT2 = [A.alloc(BF, 8, 128) for _ in range(2)]; HTr2 = [Reg("ht0"), Reg("ht1")]
    JUNK = A.alloc(BF, D)
    SSQ = A.alloc(F32, 4)
    SQ = A.alloc(F32, D)
    T1 = A.alloc(F32, D); T2 = A.alloc(F32, D)
    S16 = A.alloc(F32, 48)
    AB = A.alloc(F32, 256)
    KB2 = [A.alloc(BF, D) for _ in range(2)]; KBr2 = [Reg("kb0"), Reg("kb1")]
    QB = A.alloc(BF, D); QBr = Reg("qb")
    KTt = [A.alloc(BF, 8, 128) for _ in range(2)]; KTtr = [Reg("ktt0"), Reg("ktt1")]
    Vt = [A.alloc(BF, D) for _ in range(2)]; Vtr = [Reg("vt0"), Reg("vt1")]
    p1r = Reg("p1")

    for half in range(2):
        S.add('pool', lambda e, half=half: e.dma_start(
            out=WKV[:, :, half * 1024:(half + 1) * 1024],
            in_=w_in[:, 1024 + half * 1024:2048 + half * 1024].rearrange("(kc p) n -> p kc n", p=128)),
            dma=WKVr)
    WKVr.w = ('dma', WKVr, WKVr.dcnt)
    S.add('pool', lambda e: e.dma_start(out=WQ, in_=w_in[:, 0:1024].rearrange("(kc p) n -> p kc n", p=128)),
          w=[WQr], dma=WQr)

    kpost_r = Reg("kpost")

    def qk_post(banks, A_, B_, outbf, outr, tag):
        tr = kpost_r
        for hh in range(2):
            S.add('act', lambda e, hh=hh: e.activation(out=SQ[:, hh * 512:(hh + 1) * 512], in_=PS[banks[hh]],
                                                       func=AF.Square), r=[PSr[banks[hh]]], w=[tr])
        S.add('dve', lambda e: e.tensor_reduce(out=S16[:, 0:16], in_=SQ.rearrange("p (g d) -> p g d", d=64),
                                               axis=AX.X, op=ALU.add), r=[tr], w=[tr])
        S.add('act', lambda e: e.activation(out=S16[:, 16:32], in_=S16[:, 0:16], func=AF.Sqrt,
                                            scale=1.0 / 64, bias=1e-6), r=[tr], w=[tr])
        S.add('dve', lambda e: e.reciprocal(out=S16[:, 32:48], in_=S16[:, 16:32]), r=[tr], w=[tr])
        for hh in range(2):
            pv = PS[banks[hh]].rearrange("p (g d) -> p g d", d=64)
            t1 = T1[:, hh * 512:(hh + 1) * 512].rearrange("p (g d) -> p g d", d=64)
            t2 = T2[:, hh * 512:(hh + 1) * 512].rearrange("p (g d) -> p g d", d=64)
            S.add('dve', lambda e, pv=pv, t1=t1: e.tensor_tensor(
                out=t1, in0=pv, in1=A_.unsqueeze(1).broadcast_to([128, 8, 64]), op=ALU.mult),
                r=[PSr[banks[hh]], p1r], w=[tr])
            S.add('dve', lambda e, pv=pv, t2=t2: e.tensor_tensor(
                out=t2[:, :, 0:32], in0=pv[:, :, 32:64], in1=B_[:, 0:32].unsqueeze(1).broadcast_to([128, 8, 32]),
                op=ALU.mult), r=[PSr[banks[hh]], p1r], w=[tr])
            S.add('dve', lambda e, pv=pv, t2=t2: e.tensor_tensor(
                out=t2[:, :, 32:64], in0=pv[:, :, 0:32], in1=B_[:, 32:64].unsqueeze(1).broadcast_to([128, 8, 32]),
                op=ALU.mult), r=[PSr[banks[hh]], p1r], w=[tr])
        S.add('pool', lambda e: e.tensor_tensor(out=T1, in0=T1, in1=T2, op=ALU.add), r=[tr], w=[tr])
        S.add('dve', lambda e: e.tensor_tensor(
            out=outbf.rearrange("p (g d) -> p g d", d=64), in0=T1.rearrange("p (g d) -> p g d", d=64),
            in1=S16[:, 32:48].unsqueeze(2).broadcast_to([128, 16, 64]), op=ALU.mult), r=[tr], w=[outr])

    NT = SEQ // 128
    NOWN = TOWN // 128

    def kbanks(j):
        return [1, 2] if (j < NOWN or j % 2 == 0) else [6, 7]

    def st_N(j):
        xt = XT[j % 2]; xr = XTr[j % 2]; rt = RT[j % 2]; rr = RTr[j % 2]
        S.add('sp', lambda e: e.dma_start(out=xt, in_=xs[j * 128:(j + 1) * 128, :]), w=[xr], dma=xr)
        S.add('sp', lambda e: e.dma_start(out=rt, in_=rope[j * 128:(j + 1) * 128, :]), w=[rr], dma=rr)
        norm_tile(xt, xr, W1, SH1, JUNK, SSQ, xt, [Hb2[j % 2]], [Hr2[j % 2]], tag="n1")

    def st_T(j):
        hb = Hb2[j % 2]; hr = Hr2[j % 2]; ht = HT2[j % 2]; htr = HTr2[j % 2]
        for kc in range(8):
            S.add('pe', lambda e: e.transpose(out=PSB[0][:, kc * 128:(kc + 1) * 128],
                                              in_=hb[:, kc * 128:(kc + 1) * 128], identity=identB),
                  r=[hr, misc], w=[PSr[0]])
        S.add('act', lambda e: e.copy(out=ht.rearrange("p a b -> p (a b)"), in_=PSB[0]), r=[PSr[0]], w=[htr])

    def st_M(j):
        ht = HT2[j % 2]; htr = HTr2[j % 2]
        kb_ = kbanks(j)
        banks = [kb_[0], kb_[1], 3, 4]
        for n in range(4):
            for kc in range(8):
                S.add('pe', lambda e: e.matmul(
                    PS[banks[n]], lhsT=ht[:, kc, :], rhs=WKV[:, kc, n * 512:(n + 1) * 512],
                    start=(kc == 0), stop=(kc == 7)), r=[htr, WKVr], w=[PSr[banks[n]]])
        if j < NOWN:
            for n in range(2):
                for kc in range(8):
                    S.add('pe', lambda e: e.matmul(
                        PS[6 + n], lhsT=ht[:, kc, :], rhs=WQ[:, kc, n * 512:(n + 1) * 512],
                        start=(kc == 0), stop=(kc == 7)), r=[htr, WQr], w=[PSr[6 + n]])

    def st_E(j):
        own = j < NOWN
        rt = RT[j % 2]; rr = RTr[j % 2]
        vt = Vt[j % 2]; vr = Vtr[j % 2]
        for n in range(2):
            S.add('act', lambda e: e.copy(out=vt[:, n * 512:(n + 1) * 512], in_=PS[3 + n]),
                  r=[PSr[3 + n]], w=[vr])
        S.add('sp', lambda e: e.dma_start(
            out=Vd[:, j * 128:(j + 1) * 128, :].rearrange("h t d -> t h d"),
            in_=vt.rearrange("p (h d) -> p h d", d=128)), r=[vr], dma=vr)
        S.add('dve', lambda e: e.tensor_tensor(out=AB[:, 0:64], in0=rt[:, 0:64], in1=KNW, op=ALU.mult),
              r=[rr, CONST], w=[p1r])
        S.add('dve', lambda e: e.tensor_tensor(out=AB[:, 64:128], in0=rt[:, 64:128], in1=KNWS, op=ALU.mult),
              r=[rr, CONST], w=[p1r])
        if own:
            S.add('dve', lambda e: e.tensor_tensor(out=AB[:, 128:192], in0=rt[:, 0:64], in1=QNW, op=ALU.mult),
                  r=[rr, CONST], w=[p1r])
            S.add('dve', lambda e: e.tensor_tensor(out=AB[:, 192:256], in0=rt[:, 64:128], in1=QNWS,
                                                   op=ALU.mult), r=[rr, CONST], w=[p1r])
        qk_post(kbanks(j), AB[:, 0:64], AB[:, 64:128], KB2[j % 2], KBr2[j % 2], "kpost")
        if own:
            qk_post([6, 7], AB[:, 128:192], AB[:, 192:256], QB, QBr, "qpost")

    def st_K(j):
        own = j < NOWN
        kb = KB2[j % 2]; kbr = KBr2[j % 2]
        ktt = KTt[j % 2]; ktr = KTtr[j % 2]
        for h in range(8):
            S.add('pe', lambda e: e.transpose(out=PSB[5][:, h * 128:(h + 1) * 128],
                                              in_=kb[:, h * 128:(h + 1) * 128], identity=identB),
                  r=[kbr, misc], w=[PSr[5]])
        S.add('act', lambda e: e.copy(out=ktt.rearrange("p a b -> p (a b)"), in_=PSB[5]),
              r=[PSr[5]], w=[ktr])
        S.add('sp', lambda e: e.dma_start(out=KTd[:, :, j * 128:(j + 1) * 128], in_=ktt),
              r=[ktr], dma=ktr)
        if own:
            qb_ = QB; qbr = QBr
            for h in range(8):
                S.add('pe', lambda e: e.transpose(out=PSB[5][:, h * 128:(h + 1) * 128],
                                                  in_=qb_[:, h * 128:(h + 1) * 128], identity=identB),
                      r=[qbr, misc], w=[PSr[5]])
            S.add('act', lambda e: e.copy(out=QT[:, :, j * 128:(j + 1) * 128],
                                          in_=PSB[5].rearrange("p (a b) -> p a b", b=128)),
                  r=[PSr[5]], w=[QTr])

    st_N(0); st_T(0); st_M(0)
    for j in range(NT):
        if j + 1 < NT:
            st_N(j + 1); st_T(j + 1)
        st_E(j)
        if j + 1 < NT:
            st_M(j + 1)
        st_K(j)

    barrier()
    A.off = PASS_MARK + 2 * 8 * TOWN
    KTb = [A.alloc(BF, SEQ) for _ in range(2)]; KTbr = [Reg("ktb0"), Reg("ktb1")]
    Vb = [A.alloc(BF, 64, 130) for _ in range(2)]; Vbr = [Reg("vb0"), Reg("vb1")]
    PT = [A.alloc(BF, 512) for _ in range(3)]; PTr = [Reg("pt%d" % i) for i in range(3)]
    RC = A.alloc(F32, 8)
    OS = [A.alloc(F32, 128) for _ in range(2)]
    ONB = [A.alloc(BF, 128) for _ in range(2)]
    JKF = A.alloc(F32, 128)
    ppr = [Reg("pp0"), Reg("pp1")]
    Sr = [Reg("s0", excl=True), Reg("s1", excl=True)]
    for b in range(2):
        S.add('pool', lambda e, b=b: e.memset(Vb[b][:, :, 128:129], 1.0), w=[Vbr[b]])

    for h in range(8):
        kt = KTb[h % 2]; ktr = KTbr[h % 2]; vb = Vb[h % 2]; vbr = Vbr[h % 2]
        S.add('sp', lambda e, kt=kt, h=h: e.dma_start(out=kt, in_=KTd[:, h, :]), w=[ktr], dma=ktr)
        S.add('sp', lambda e, vb=vb, h=h: e.dma_start(
            out=vb[:, :, 0:128], in_=Vd[h].rearrange("(kc p) d -> p kc d", p=128)), w=[vbr], dma=vbr)
        def emit_qk(i, kt=kt, ktr=ktr, h=h):
            qt, kc = divmod(i, 64); q0 = qt * 256; sb = i % 2
            for c in range(2):
                S.add('pe', lambda e: e.matmul(
                    PS[2 * sb + c][:, 0:256], lhsT=kt[c * 64:(c + 1) * 64, kc * 128:(kc + 1) * 128],
                    rhs=QT[c * 64:(c + 1) * 64, h, q0:q0 + 256], start=True, stop=True),
                    r=[ktr, QTr], w=[Sr[sb]])

        emit_qk(0)
        emit_qk(1)
        pending = []
        for it in range(512):
            qt, kc = divmod(it, 64); q0 = qt * 256
            sb = it % 2; pt = PT[it % 3]; ptr = PTr[it % 3]
            S.add('act', lambda e: e.activation(
                out=pt.rearrange("p (c n) -> p c n", n=256),
                in_=SXYs[sb].rearrange("p (c n) -> p c n", n=512)[:, :, 0:256],
                func=AF.Exp, scale=0.125), r=[Sr[sb]], w=[ptr])
            if it + 2 < 512:
                emit_qk(it + 2)
            for qb in range(2):
                for c in range(2):
                    bank = 4 + qb
                    S.add('pe', lambda e: e.matmul(
                        PS[bank][:, c * 256:c * 256 + 129], lhsT=pt[:, c * 256 + qb * 128:c * 256 + qb * 128 + 128],
                        rhs=vb[:, kc, 0:129], start=(kc == 0 and c == 0), stop=(kc == 63),
                        skip_group_check=True),
                        r=[ptr, vbr], w=[PSr[bank]])
            if pending and (kc == 12 or it == 511):
                for fn_ in pending:
                    fn_()
                pending = []
            if kc != 63:
                continue
            for qb in range(2):
                bk = 4 + qb
                pr = ppr[qb]; os_ = OS[qb]; onb = ONB[qb]
                rc = RC[:, qb * 4:(qb + 1) * 4]
                S.add('dve', lambda e, bk=bk, rc=rc: e.reciprocal(out=rc[:, 0:1], in_=PS[bk][:, 128:129]),
                      r=[PSr[bk]], w=[pr])
                S.add('dve', lambda e, bk=bk, rc=rc: e.reciprocal(out=rc[:, 1:2], in_=PS[bk][:, 384:385]),
                      r=[PSr[bk]], w=[pr])
                S.add('dve', lambda e, rc=rc: e.tensor_tensor(out=rc[:, 1:2], in0=rc[:, 1:2], in1=NLAM[:, 0:1],
                                                              op=ALU.mult), r=[pr, misc], w=[pr])
                S.add('dve', lambda e, bk=bk, rc=rc, os_=os_: e.tensor_scalar(
                    out=os_, in0=PS[bk][:, 0:128], scalar1=rc[:, 0:1], scalar2=None, op0=ALU.mult),
                    r=[PSr[bk], pr], w=[pr])
                S.add('dve', lambda e, bk=bk, rc=rc, os_=os_: e.scalar_tensor_tensor(
                    out=os_, in0=PS[bk][:, 256:384], scalar=rc[:, 1:2], in1=os_, op0=ALU.mult, op1=ALU.add),
                    r=[PSr[bk], pr], w=[pr])
                S.add('dve', lambda e, os_=os_: e.tensor_tensor(out=JKF, in0=os_, in1=os_, op=ALU.mult),
                      r=[pr], w=[pr])
                S.add('dve', lambda e, rc=rc: e.tensor_reduce(out=rc[:, 2:3], in_=JKF, axis=AX.X, op=ALU.add),
                      r=[pr], w=[pr])
                S.add('dve', lambda e, rc=rc: e.tensor_scalar(out=rc[:, 2:3], in0=rc[:, 2:3], scalar1=1.0 / 128,
                                                              scalar2=1e-5, op0=ALU.mult, op1=ALU.add),
                      r=[pr], w=[pr])
                S.add('pool', lambda e, rc=rc: e.tensor_tensor(out=rc[:, 3:4], in0=rc[:, 2:3],
                                                               in1=NLAM[:, 1:2], op=ALU.pow), r=[pr, misc], w=[pr])
                S.add('dve', lambda e, os_=os_, rc=rc, onb=onb: e.scalar_tensor_tensor(
                    out=onb, in0=os_, scalar=rc[:, 3:4], in1=SUBW, op0=ALU.mult, op1=ALU.mult),
                    r=[pr, misc], w=[pr])
                def fin_post(onb=onb, pr=pr, h=h, q0=q0, qb=qb):
                    S.add('pe', lambda e: e.transpose(out=PSB[7][:, 0:128], in_=onb, identity=identB),
                          r=[pr, misc], w=[PSr[7]])
                    g = (q0 + qb * 128) // 512
                    S.add('dve', lambda e: e.tensor_copy(
                        out=OT[:, h, q0 + qb * 128:q0 + qb * 128 + 128], in_=PSB[7][:, 0:128]),
                        r=[PSr[7]], w=[OTr[g]])
                pending.append(fin_post)
            if it == 511:
                for fn_ in pending:
                    fn_()
                pending = []

    if DEBUG == 'attn':
        barrier()
        S.add('sp', lambda e: e.dma_start(out=dbg, in_=OT.rearrange("p a b -> p (a b)")), r=OTr, dma=misc)
        S.wait_regs('sp', [misc])
        return finish(nc, S, stack)

    barrier()
    A.off = PASS_MARK
    RING = [A.alloc(BF, 8, 1024) for _ in range(3)]; RINGr = [Reg("ring%d" % i) for i in range(3)]
    ring_i = [0]

    def ring_load(src2d):
        i = ring_i[0] % len(RING); ring_i[0] += 1
        S.add('pool', lambda e, i=i: e.dma_start(out=RING[i], in_=src2d.rearrange("(kc p) n -> p kc n", p=128)),
              w=[RINGr[i]], dma=RINGr[i])
        return RING[i], RINGr[i]

    XG = [A.alloc(F32, D) for _ in range(2)]; XGr = [Reg("xg0"), Reg("xg1")]
    Hb = A.alloc(BF, D); Hr = Reg("h4")
    HTG = A.alloc(BF, 8, 514); HTGr = Reg("htg")
    JUNK = A.alloc(BF, D); SSQ = A.alloc(F32, 4); TMP = A.alloc(F32, D)
    CCs = A.alloc(F32, 512); CCr = Reg("ccs")
    ZP = A.alloc(BF, 8, 514); ZPr = Reg("zp")
    ZH = A.alloc(F32, 32)
    CT = A.alloc(F32, 512)
    U = A.alloc(BF, 8, 512); Ur = Reg("u")
    SG = A.alloc(BF, 512); SGr = Reg("sg")
    MC = A.alloc(BF, 8, 512); MCr = Reg("mc")
    M = U; Mr = Ur
    TMP2 = A.alloc(F32, D)
    X1 = A.alloc(F32, D); X1r = Reg("x1")
    H2 = A.alloc(F32, D); H2r = Reg("h2")
    H2B = A.alloc(BF, D); H2Br = Reg("h2b")
    H2T32 = TMP2.rearrange("p (a b) -> p a b", b=128); H2Tr = Reg("h2t32")
    LG = A.alloc(F32, NE); MX = A.alloc(F32, 8); EX = A.alloc(F32, NE); MK = A.alloc(F32, NE)
    SM = A.alloc(F32, 4)
    rr4 = Reg("r4")
    p4 = Reg("p4")

    COL = dict(cb=3072, cc=4096, cx=5120, ga=6144, gc=7168)
    for g in range(4):
        q0 = g * 512
        for tt in range(5):
            xt = XG[tt % 2]; xr = XGr[tt % 2]
            if tt < 4:
                npart = 128
                S.add('sp', lambda e, xt=xt, q0=q0, tt=tt: e.dma_start(
                    out=xt, in_=xs[q0 + tt * 128:q0 + (tt + 1) * 128, :]), w=[xr], dma=xr)
            else:
                npart = 2
                S.add('sp', lambda e, xt=xt, g=g, q0=q0: e.dma_start(
                    out=xt[0:1, :], in_=(xs[q0 - 1:q0, :] if g > 0 else xh[0:1, :])), w=[xr], dma=xr)
                S.add('sp', lambda e, xt=xt, g=g, q0=q0: e.dma_start(
                    out=xt[1:2, :], in_=(xs[q0 + 512:q0 + 513, :] if g < 3 else xh[1:2, :])), w=[xr], dma=xr)
            norm_tile(xt, xr, W1, SH1, JUNK, SSQ, TMP, [Hb], [Hr], npart=npart, tag="n4")
            for kc in range(8):
                S.add('pe', lambda e, kc=kc, npart=npart: e.transpose(
                    out=PSB[0][:, kc * 128:kc * 128 + npart], in_=Hb[0:npart, kc * 128:(kc + 1) * 128],
                    identity=identB[0:npart, 0:npart]), r=[Hr, misc], w=[PSr[0]])
            c0 = tt * 128
            S.add('act', lambda e, c0=c0, npart=npart: e.copy(
                out=HTG[:, :, c0:c0 + npart], in_=PSB[0].rearrange("p (a b) -> p a b", b=128)[:, :, 0:npart]),
                r=[PSr[0]], w=[HTGr])
        wcc, wccr = ring_load(w_in[:, COL['cc']:COL['cc'] + 1024])
        wcx, wcxr = ring_load(w_in[:, COL['cx']:COL['cx'] + 1024])
        for fc in range(8):
            for (wt, wr, bank) in ((wcc, wccr, 1), (wcx, wcxr, 2)):
                for kc in range(8):
                    S.add('pe', lambda e, wt=wt, bank=bank, kc=kc, fc=fc: e.matmul(
                        PS[bank], lhsT=wt[:, kc, fc * 128:(fc + 1) * 128], rhs=HTG[:, kc, 0:512],
                        start=(kc == 0), stop=(kc == 7)), r=[wr, HTGr], w=[PSr[bank]])
            for wi, (wt, wr) in enumerate(((wcc, wccr), (wcx, wcxr))):
                for kc in range(8):
                    S.add('pe', lambda e, wt=wt, kc=kc, fc=fc, wi=wi: e.matmul(
                        PS[3][:, (fc * 2 + wi) * 2:(fc * 2 + wi) * 2 + 2], lhsT=wt[:, kc, fc * 128:(fc + 1) * 128],
                        rhs=HTG[:, kc, 512:514], start=(kc == 0), stop=(kc == 7)), r=[wr, HTGr], w=[PSr[3]])
            S.add('act', lambda e: e.copy(out=CCs, in_=PS[1]), r=[PSr[1]], w=[CCr])
            S.add('dve', lambda e, fc=fc: e.tensor_tensor(out=ZP[:, fc, 1:513], in0=PS[2], in1=CCs, op=ALU.mult),
                  r=[PSr[2], CCr], w=[ZPr])
        S.add('act', lambda e: e.copy(out=ZH, in_=PS[3][:, 0:32]), r=[PSr[3]], w=[p4])
        zh4 = ZH.rearrange("p (f w c) -> p f w c", w=2, c=2)
        for ci, col in ((0, 0), (1, 513)):
            S.add('dve', lambda e, ci=ci, col=col: e.tensor_tensor(
                out=ZP[:, :, col:col + 1], in0=zh4[:, :, 0, ci:ci + 1], in1=zh4[:, :, 1, ci:ci + 1], op=ALU.mult),
                r=[p4], w=[ZPr])
        if g == 0:
            S.add('dve', lambda e: e.tensor_scalar(out=ZP[:, :, 0:1], in0=ZP[:, :, 0:1], scalar1=HM[:, 0:1],
                                                   scalar2=None, op0=ALU.mult), r=[CONST], w=[ZPr])
        if g == 3:
            S.add('dve', lambda e: e.tensor_scalar(out=ZP[:, :, 513:514], in0=ZP[:, :, 513:514],
                                                   scalar1=HM[:, 1:2], scalar2=None, op0=ALU.mult),
                  r=[CONST], w=[ZPr])
        wcb, wcbr = ring_load(w_in[:, COL['cb']:COL['cb'] + 1024])
        for fc in range(8):
            bank = 1 + fc % 2
            for kc in range(8):
                S.add('pe', lambda e, bank=bank, kc=kc, fc=fc: e.matmul(
                    PS[bank], lhsT=wcb[:, kc, fc * 128:(fc + 1) * 128], rhs=HTG[:, kc, 0:512],
                    start=(kc == 0), stop=(kc == 7)), r=[wcbr, HTGr], w=[PSr[bank]])
            S.add('dve', lambda e, fc=fc: e.tensor_scalar(out=CT, in0=ZP[:, fc, 0:512], scalar1=CW[:, 0, fc:fc + 1],
                                                          scalar2=None, op0=ALU.mult), r=[ZPr, CONST], w=[p4])
            S.add('dve', lambda e, fc=fc: e.scalar_tensor_tensor(out=CT, in0=ZP[:, fc, 1:513], scalar=CW[:, 1, fc:fc + 1],
                                                                 in1=CT, op0=ALU.mult, op1=ALU.add),
                  r=[ZPr, CONST, p4], w=[p4])
            S.add('dve', lambda e, fc=fc: e.scalar_tensor_tensor(out=CT, in0=ZP[:, fc, 2:514], scalar=CW[:, 2, fc:fc + 1],
                                                                 in1=CT, op0=ALU.mult, op1=ALU.add),
                  r=[ZPr, CONST, p4], w=[p4])
            S.add('dve', lambda e, fc=fc, bank=bank: e.tensor_tensor(out=U[:, fc, :], in0=PS[bank], in1=CT,
                                                                      op=ALU.mult), r=[PSr[bank], p4], w=[Ur])
        wco, wcor = ring_load(w_conv_o[:, :])
        wgc, wgcr = ring_load(w_in[:, COL['gc']:COL['gc'] + 1024])
        for dc in range(8):
            ba = 1 + 2 * (dc % 2); bb = ba + 1
            for kc in range(8):
                S.add('pe', lambda e, ba=ba, kc=kc, dc=dc: e.matmul(
                    PS[ba], lhsT=wco[:, kc, dc * 128:(dc + 1) * 128], rhs=U[:, kc, :],
                    start=(kc == 0), stop=(kc == 7)), r=[wcor, Ur], w=[PSr[ba]])
            for kc in range(8):
                S.add('pe', lambda e, bb=bb, kc=kc, dc=dc: e.matmul(
                    PS[bb], lhsT=wgc[:, kc, dc * 128:(dc + 1) * 128], rhs=HTG[:, kc, 0:512],
                    start=(kc == 0), stop=(kc == 7)), r=[wgcr, HTGr], w=[PSr[bb]])
            S.add('act', lambda e, bb=bb: e.activation(out=SG, in_=PS[bb], func=AF.Sigmoid), r=[PSr[bb]], w=[SGr])
            S.add('dve', lambda e, ba=ba, dc=dc: e.tensor_tensor(out=MC[:, dc, :], in0=PS[ba], in1=SG, op=ALU.mult),
                  r=[PSr[ba], SGr], w=[MCr])
        wao, waor = ring_load(w_attn_o[:, :])
        wga, wgar = ring_load(w_in[:, COL['ga']:COL['ga'] + 1024])
        for dc in range(8):
            ba = 1 + 2 * (dc % 2); bb = ba + 1
            for kc in range(8):
                S.add('pe', lambda e, ba=ba, kc=kc, dc=dc: e.matmul(
                    PS[ba], lhsT=wao[:, kc, dc * 128:(dc + 1) * 128], rhs=OT[:, kc, q0:q0 + 512],
                    start=(kc == 0), stop=(kc == 7)), r=[waor, OTr[g]], w=[PSr[ba]])
            for kc in range(8):
                S.add('pe', lambda e, bb=bb, kc=kc, dc=dc: e.matmul(
                    PS[bb], lhsT=wga[:, kc, dc * 128:(dc + 1) * 128], rhs=HTG[:, kc, 0:512],
                    start=(kc == 0), stop=(kc == 7)), r=[wgar, HTGr], w=[PSr[bb]])
            S.add('act', lambda e, bb=bb: e.activation(out=SG, in_=PS[bb], func=AF.Sigmoid), r=[PSr[bb]], w=[SGr])
            S.add('dve', lambda e, ba=ba: e.tensor_tensor(out=CT, in0=PS[ba], in1=SG, op=ALU.mult),
                  r=[PSr[ba], SGr], w=[p4])
            S.add('pool', lambda e, dc=dc: e.tensor_tensor(out=M[:, dc, :], in0=CT, in1=MC[:, dc, :], op=ALU.add),
                  r=[p4, MCr], w=[Mr])
        if DEBUG == 'p4' and g == 3:
            S.add('sp', lambda e: e.dma_start(out=dbg[:, 0, :], in_=ZP.rearrange("p a b -> p (a b)")), r=[ZPr], dma=misc)
            S.add('sp', lambda e: e.dma_start(out=dbg[:, 1, 0:4096], in_=MC.rearrange("p a b -> p (a b)")), r=[MCr], dma=misc)
            S.add('sp', lambda e: e.dma_start(out=dbg[:, 2, 0:4096], in_=M.rearrange("p a b -> p (a b)")), r=[Mr], dma=misc)
            S.add('sp', lambda e: e.dma_start(out=dbg[:, 3, :], in_=HTG.rearrange("p a b -> p (a b)")), r=[HTGr], dma=misc)
            S.wait_regs('sp', [misc])
            return finish(nc, S, stack)
        wo, wor = ring_load(w_out[:, :])
        for tt in range(4):
            tile_i = g * 4 + tt
            xt = XG[tt % 2]; xr = XGr[tt % 2]
            S.add('sp', lambda e, xt=xt, q0=q0, tt=tt: e.dma_start(
                out=xt, in_=xs[q0 + tt * 128:q0 + (tt + 1) * 128, :]), w=[xr], dma=xr)
            for half in range(2):
                bank = 5 + half
                for kc in range(8):
                    S.add('pe', lambda e, bank=bank, kc=kc, tt=tt, half=half: e.matmul(
                        PS[bank], lhsT=M[:, kc, tt * 128:(tt + 1) * 128], rhs=wo[:, kc, half * 512:(half + 1) * 512],
                        start=(kc == 0), stop=(kc == 7)), r=[Mr, wor], w=[PSr[bank]])
                S.add('dve', lambda e, bank=bank, half=half: e.tensor_tensor(
                    out=TMP2[:, half * 512:(half + 1) * 512], in0=PS[bank], in1=G1[:, half * 512:(half + 1) * 512],
                    op=ALU.mult), r=[PSr[bank], misc], w=[H2Tr])
            S.add('pool', lambda e, xt=xt: e.tensor_tensor(out=X1, in0=TMP2, in1=xt, op=ALU.add),
                  r=[H2Tr, xr], w=[X1r])
            x1dst = y if DEBUG == 'x1' else X1d
            S.add('sp', lambda e, tile_i=tile_i, x1dst=x1dst: e.dma_start(
                out=x1dst[tile_i * 128:(tile_i + 1) * 128, :], in_=X1), r=[X1r], dma=X1r)
            norm_tile(X1, X1r, W2, SH2, JUNK, SSQ, TMP, [H2, H2B], [H2r, H2Br], tag="n2")
            for kc in range(8):
                bank = 1 + kc // 4
                S.add('pe', lambda e, kc=kc, bank=bank: e.transpose(
                    out=PS[bank][:, (kc % 4) * 128:(kc % 4 + 1) * 128], in_=H2[:, kc * 128:(kc + 1) * 128],
                    identity=identF), r=[H2r, CONST], w=[PSr[bank]])
            for b2 in range(2):
                S.add('act', lambda e, b2=b2: e.copy(out=H2T32[:, b2 * 4:(b2 + 1) * 4, :].rearrange("p a b -> p (a b)"),
                                                     in_=PS[1 + b2]), r=[PSr[1 + b2]], w=[H2Tr])
            for kc in range(8):
                S.add('pe', lambda e, kc=kc: e.matmul(PS[3][:, 0:NE], lhsT=H2T32[:, kc, :], rhs=WRT[:, kc, :],
                                                      start=(kc == 0), stop=(kc == 7)),
                      r=[H2Tr, CONST], w=[PSr[3]])
            S.add('dve', lambda e: e.tensor_tensor(out=LG, in0=PS[3][:, 0:NE], in1=BR, op=ALU.add),
                  r=[PSr[3], CONST], w=[rr4])
            S.add('dve', lambda e: e.max(out=MX, in_=LG), r=[rr4], w=[rr4])
            S.add('dve', lambda e: e.tensor_scalar(out=MK, in0=LG, scalar1=MX[:, 3:4], scalar2=None, op0=ALU.is_ge),
                  r=[rr4], w=[rr4])
            if MOE_GATHER:
                S.add('sp', lambda e, tile_i=tile_i: e.dma_start(out=H2d[tile_i * 128:(tile_i + 1) * 128, :],
                                                                 in_=H2B), r=[H2Br], dma=H2Br)
            S.add('dve', lambda e: e.tensor_scalar(out=SM[:, 0:1], in0=MX[:, 0:1], scalar1=-1.0, scalar2=None,
                                                   op0=ALU.mult), r=[rr4], w=[rr4])
            S.add('act', lambda e: e.activation(out=EX, in_=LG, func=AF.Exp, bias=SM[:, 0:1]), r=[rr4], w=[rr4])
            S.add('dve', lambda e: e.tensor_tensor(out=EX, in0=EX, in1=MK, op=ALU.mult), r=[rr4], w=[rr4])
            S.add('dve', lambda e: e.tensor_reduce(out=SM[:, 1:2], in_=EX, axis=AX.X, op=ALU.add), r=[rr4], w=[rr4])
            S.add('dve', lambda e: e.reciprocal(out=SM[:, 2:3], in_=SM[:, 1:2]), r=[rr4], w=[rr4])
            S.add('dve', lambda e, tile_i=tile_i: e.tensor_scalar(out=WG[:, tile_i, :], in0=EX, scalar1=SM[:, 2:3],
                                                                   scalar2=None, op0=ALU.mult), r=[rr4], w=[WGr])
            for kc in range(8):
                S.add('pe', lambda e, kc=kc: e.transpose(out=PSB[7][:, kc * 128:(kc + 1) * 128],
                                                         in_=H2B[:, kc * 128:(kc + 1) * 128], identity=identB),
                      r=[H2Br, misc], w=[PSr[7]])
            S.add('act', lambda e, q0=q0, tt=tt: e.copy(
                out=OT[:, :, q0 + tt * 128:q0 + (tt + 1) * 128], in_=PSB[7].rearrange("p (a b) -> p a b", b=128)),
                r=[PSr[7]], w=[OTr[g]])

    if DEBUG == 'x1':
        barrier()
        S.add('sp', lambda e: e.dma_start(out=dbg.rearrange("(t p) e -> p t e", p=128), in_=WG), r=[WGr], dma=misc)
        S.wait_regs('sp', [misc])
        return finish(nc, S, stack)

    barrier()
    A.off = P5_MARK
    H2T = OT
    ACC = A.alloc(F32, 16, D); ACCr = [Reg("acc%d" % i) for i in range(16)]
    BGg = A.alloc(F32, 8, NE); BGu = A.alloc(F32, 8, NE)
    RK = A.alloc(F32, 16, NE); rkr = Reg("rk")
    IOTA = A.alloc(F32, CAP)
    H2TOK = OT.rearrange("p a b -> p (a b)").rearrange("p (t d) -> p t d", d=D)
    LOOP_MARK = A.off
    BGrow = A.alloc(F32, 2048)
    BD = A.alloc(F32, D)
    WGT = A.alloc(F32, 16, 128)
    p5 = Reg("p5")
    S.add('sp', lambda e: e.dma_start(out=BGrow[0:NE, :], in_=b_gate_up), w=[p5], dma=p5)
    S.add('sp', lambda e: e.dma_start(out=BD[0:NE, :], in_=b_down), dma=p5)
    p5.w = ('dma', p5, p5.dcnt)
    bg3 = BGrow.rearrange("p (f m t) -> p f m t", m=128, t=2)
    for fc in range(8):
        S.add('pe', lambda e, fc=fc: e.transpose(out=PS[0][:, fc * NE:(fc + 1) * NE],
                                                 in_=bg3[0:NE, fc, :, 0],
                                                 identity=identF[0:NE, 0:NE]), r=[p5, CONST], w=[PSr[0]])
        S.add('pe', lambda e, fc=fc: e.transpose(out=PS[0][:, 256 + fc * NE:256 + (fc + 1) * NE],
                                                 in_=bg3[0:NE, fc, :, 1],
                                                 identity=identF[0:NE, 0:NE]), r=[p5, CONST], w=[PSr[0]])
    bgr = Reg("bg")
    S.add('dve', lambda e: e.tensor_copy(out=BGg.rearrange("p a b -> p (a b)"), in_=PS[0][:, 0:256]),
          r=[PSr[0]], w=[bgr])
    S.add('dve', lambda e: e.tensor_scalar(out=BGu.rearrange("p a b -> p (a b)"), in0=PS[0][:, 256:512],
                                           scalar1=1.0, scalar2=None, op0=ALU.add), r=[PSr[0]], w=[bgr])
    for ti in range(16):
        S.add('pe', lambda e, ti=ti: e.transpose(out=PS[1][0:NE, 0:128], in_=WG[:, ti, :], identity=identF),
              r=[WGr, CONST], w=[PSr[1]])
        S.add('dve', lambda e, ti=ti: e.tensor_copy(out=WGT[0:NE, ti, :], in_=PS[1][0:NE, 0:128]),
              r=[PSr[1]], w=[bgr])
        for half in range(2):
            S.add('pe', lambda e, ti=ti, half=half: e.matmul(
                PS[2 + half], lhsT=WGT[0:NE, ti, :], rhs=BD[0:NE, half * 512:(half + 1) * 512],
                start=True, stop=True), r=[bgr, p5], w=[PSr[2 + half]])
            S.add('act', lambda e, ti=ti, half=half: e.copy(out=ACC[:, ti, half * 512:(half + 1) * 512],
                                                            in_=PS[2 + half]), r=[PSr[2 + half]], w=[ACCr[ti]])
    def moe_dense():
        barrier()
        A.off = LOOP_MARK
        RING5 = [A.alloc(BF, 8, 1024) for _ in range(3)]; RING5r = [Reg("r5_%d" % i) for i in range(3)]
        r5_i = [0]

        def ring5_load(src2d):
            i = r5_i[0] % 3; r5_i[0] += 1
            S.add('pool', lambda e, i=i: e.dma_start(out=RING5[i], in_=src2d.rearrange("(kc p) n -> p kc n", p=128)),
                  w=[RING5r[i]], dma=RING5r[i])
            return RING5[i], RING5r[i]

        AT = [A.alloc(BF, 8, 512) for _ in range(2)]; ATr = [Reg("at0"), Reg("at1")]
        GS = [A.alloc(F32, 512) for _ in range(2)]; GSr = [Reg("gs0"), Reg("gs1")]
        SGM = [A.alloc(F32, 512) for _ in range(2)]
        U1 = [A.alloc(F32, 512) for _ in range(2)]

        it5 = [0]
        for ex in range(NE):
            wgA, wgAr = ring5_load(w_gate_up[ex, :, 0:1024])
            wgB, wgBr = ring5_load(w_gate_up[ex, :, 1024:2048])
            wd, wdr = ring5_load(w_down[ex, :, :])
            for tg in range(4):
                at = AT[tg % 2]; atr = ATr[tg % 2]
                for fc in range(8):
                    wt, wr = (wgA, wgAr) if fc < 4 else (wgB, wgBr)
                    wv = wt.rearrange("p k (f m t) -> p k f m t", m=128, t=2)
                    k = it5[0] % 2; it5[0] += 1
                    bg_, bu_ = 4 * k, 4 * k + 1
                    for (bank, off) in ((bg_, 0), (bu_, 1)):
                        for kc in range(8):
                            S.add('pe', lambda e, bank=bank, off=off, wv=wv, kc=kc, fc=fc, tg=tg: e.matmul(
                                PS[bank], lhsT=wv[:, kc, fc % 4, :, off],
                                rhs=H2T[:, kc, tg * 512:(tg + 1) * 512], start=(kc == 0), stop=(kc == 7)),
                                r=[wr, OTr[tg]], w=[PSr[bank]])
                    gs = GS[k]; gsr = GSr[k]; sg = SGM[k]; u1 = U1[k]
                    S.add('dve', lambda e, gs=gs, bg_=bg_, fc=fc, ex=ex: e.tensor_scalar(
                        out=gs, in0=PS[bg_], scalar1=BGg[:, fc, ex:ex + 1], scalar2=7.0, op0=ALU.add, op1=ALU.min),
                        r=[PSr[bg_], bgr], w=[gsr])
                    S.add('act', lambda e, gs=gs, sg=sg: e.activation(out=sg, in_=gs, func=AF.Sigmoid, scale=1.702),
                          r=[gsr], w=[gsr])
                    S.add('dve', lambda e, u1=u1, bu_=bu_, fc=fc, ex=ex: e.tensor_scalar(
                        out=u1, in0=PS[bu_], scalar1=BGu[:, fc, ex:ex + 1], scalar2=8.0, op0=ALU.add, op1=ALU.min),
                        r=[PSr[bu_], bgr], w=[gsr])
                    S.add('dve', lambda e, gs=gs, sg=sg: e.tensor_tensor(out=gs, in0=gs, in1=sg, op=ALU.mult),
                          r=[gsr], w=[gsr])
                    S.add('dve', lambda e, u1=u1, gs=gs, at=at, fc=fc: e.scalar_tensor_tensor(
                        out=at[:, fc, :], in0=u1, scalar=-6.0, in1=gs, op0=ALU.max, op1=ALU.mult),
                        r=[gsr], w=[atr])
                for tt in range(4):
                    ti = tg * 4 + tt
                    for half in range(2):
                        bank = 2 + half
                        for fc in range(8):
                            S.add('pe', lambda e, bank=bank, fc=fc, at=at, tt=tt, half=half: e.matmul(
                                PS[bank], lhsT=at[:, fc, tt * 128:(tt + 1) * 128],
                                rhs=wd[:, fc, half * 512:(half + 1) * 512], start=(fc == 0), stop=(fc == 7)),
                                r=[atr, wdr], w=[PSr[bank]])
                        S.add('dve', lambda e, bank=bank, ti=ti, half=half, ex=ex: e.scalar_tensor_tensor(
                            out=ACC[:, ti, half * 512:(half + 1) * 512], in0=PS[bank], scalar=WG[:, ti, ex:ex + 1],
                            in1=ACC[:, ti, half * 512:(half + 1) * 512], op0=ALU.mult, op1=ALU.add),
                            r=[PSr[bank], WGr], w=[ACCr[ti]])


    if MOE_GATHER:
        TRIF = A.alloc(F32, 128); TRIB = A.alloc(BF, 128); ONESB = A.alloc(BF, 128)
        MKA = A.alloc(BF, 16, NE)
        S.add('dve', lambda e: e.tensor_scalar(out=MKA.rearrange("p a b -> p (a b)"),
                                               in0=WG.rearrange("p a b -> p (a b)"), scalar1=0.0, scalar2=None,
                                               op0=ALU.is_gt), r=[WGr], w=[bgr])
        S.add('sp', lambda e: e.dma_start(out=TRIF, in_=tri), w=[p5], dma=p5)
        S.add('sp', lambda e: e.dma_start(out=IOTA, in_=iota), w=[p5], dma=p5)
        S.add('sp', lambda e: e.dma_start(out=H2TOK, in_=H2d.rearrange("(t p) d -> p t d", p=128)),
              w=OTr + [p5], dma=p5)
        S.add('dve', lambda e: e.tensor_copy(out=TRIB, in_=TRIF), r=[p5], w=[bgr])
        S.add('pool', lambda e: e.memset(ONESB, 1.0), w=[bgr])
        for ti in range(16):
            b = 4 + ti % 2
            for tj in range(ti):
                S.add('pe', lambda e: e.matmul(PS[b][:, 0:NE], lhsT=ONESB, rhs=MKA[:, tj, :],
                                               start=(tj == 0), stop=False), r=[bgr, WGr], w=[PSr[b]])
            S.add('pe', lambda e: e.matmul(PS[b][:, 0:NE], lhsT=TRIB, rhs=MKA[:, ti, :],
                                           start=(ti == 0), stop=True), r=[bgr, WGr], w=[PSr[b]])
            S.add('dve', lambda e: e.scalar_tensor_tensor(out=RK[:, ti, :], in0=PS[b][:, 0:NE], scalar=1.0,
                                                          in1=MKA[:, ti, :], op0=ALU.add, op1=ALU.mult),
                  r=[PSr[b], WGr], w=[rkr])
            S.add('dve', lambda e: e.tensor_scalar(out=RK[:, ti, :], in0=RK[:, ti, :], scalar1=-1.0, scalar2=None,
                                                   op0=ALU.add), r=[rkr], w=[rkr])
        barrier()
        A.off = LOOP_MARK
        NR = 4
        RING5 = [A.alloc(BF, 8, 512) for _ in range(NR)]; RING5r = [Reg("r5_%d" % i) for i in range(NR)]
        r5_i = [0]

        def ring5_load(src2d):
            i = r5_i[0] % NR; r5_i[0] += 1
            S.add('pool', lambda e: e.dma_start(out=RING5[i], in_=src2d.rearrange("(kc p) n -> p kc n", p=128)),
                  w=[RING5r[i]], dma=RING5r[i])
            return RING5[i], RING5r[i]

        SEL = A.alloc(BF, 16, CAP); SELr = Reg("sel")
        XET = A.alloc(BF, 8, CAP); XEr = Reg("xet")
        YE = XET.rearrange("p a b -> p (a b)").rearrange("p (c d) -> p c d", d=D)
        AT = A.alloc(BF, 8, CAP); ATr = Reg("at")
        ST = [A.alloc(BF, CAP) for _ in range(2)]; STr = [Reg("st0"), Reg("st1")]
        GS = [A.alloc(F32, CAP) for _ in range(2)]; GSr = [Reg("gs0"), Reg("gs1")]
        SGM = [A.alloc(F32, CAP) for _ in range(2)]
        U1 = [A.alloc(F32, CAP) for _ in range(2)]
        NCC = CAP // 128
        for ex in range(NE):
            for ti in range(16):
                S.add('dve', lambda e: e.tensor_scalar(out=SEL[:, ti, :], in0=IOTA, scalar1=RK[:, ti, ex:ex + 1],
                                                       scalar2=None, op0=ALU.is_equal), r=[rkr, p5], w=[SELr])
            for fc in range(8):
                b = fc % 2
                for ti in range(16):
                    S.add('pe', lambda e: e.matmul(PS[b][:, 0:CAP], lhsT=H2TOK[:, ti, fc * 128:(fc + 1) * 128],
                                                   rhs=SEL[:, ti, :], start=(ti == 0), stop=(ti == 15)),
                          r=[p5, SELr], w=[PSr[b]])
                S.add('act', lambda e: e.copy(out=XET[:, fc, :], in_=PS[b][:, 0:CAP]), r=[PSr[b]], w=[XEr])
            for fc in range(8):
                if fc % 2 == 0:
                    wt, wr = ring5_load(w_gate_up[ex, :, fc * 256:fc * 256 + 512])
                    wv = wt.rearrange("p k (f m t) -> p k f m t", m=128, t=2)
                k = fc % 2
                bg_, bu_ = 2 + 2 * k, 3 + 2 * k
                for (bank, off) in ((bg_, 0), (bu_, 1)):
                    for kc in range(8):
                        S.add('pe', lambda e: e.matmul(PS[bank][:, 0:CAP], lhsT=wv[:, kc, fc % 2, :, off],
                                                       rhs=XET[:, kc, :], start=(kc == 0), stop=(kc == 7)),
                              r=[wr, XEr], w=[PSr[bank]])
                gs = GS[k]; gsr = GSr[k]; sg = SGM[k]; u1 = U1[k]
                S.add('dve', lambda e: e.tensor_scalar(out=gs, in0=PS[bg_][:, 0:CAP], scalar1=BGg[:, fc, ex:ex + 1],
                                                       scalar2=7.0, op0=ALU.add, op1=ALU.min),
                      r=[PSr[bg_], bgr], w=[gsr])
                S.add('act', lambda e: e.activation(out=sg, in_=gs, func=AF.Sigmoid, scale=1.702),
                      r=[gsr], w=[gsr])
                S.add('dve', lambda e: e.tensor_scalar(out=u1, in0=PS[bu_][:, 0:CAP], scalar1=BGu[:, fc, ex:ex + 1],
                                                       scalar2=8.0, op0=ALU.add, op1=ALU.min),
                      r=[PSr[bu_], bgr], w=[gsr])
                S.add('dve', lambda e: e.tensor_tensor(out=gs, in0=gs, in1=sg, op=ALU.mult), r=[gsr], w=[gsr])
                S.add('dve', lambda e: e.scalar_tensor_tensor(out=AT[:, fc, :], in0=u1, scalar=-6.0, in1=gs,
                                                              op0=ALU.max, op1=ALU.mult), r=[gsr], w=[ATr])
            for half in range(2):
                wd, wdr = ring5_load(w_down[ex, :, half * 512:(half + 1) * 512])
                for cc in range(NCC):
                    b = 6 + (half * NCC + cc) % 2
                    for fc in range(8):
                        S.add('pe', lambda e: e.matmul(PS[b], lhsT=AT[:, fc, cc * 128:(cc + 1) * 128],
                                                       rhs=wd[:, fc, :], start=(fc == 0), stop=(fc == 7)),
                              r=[ATr, wdr], w=[PSr[b]])
                    S.add('act', lambda e: e.copy(out=YE[:, cc, half * 512:(half + 1) * 512], in_=PS[b]),
                          r=[PSr[b]], w=[XEr])
            for ti in range(16):
                tb = ti % 2
                st = ST[tb]; str_ = STr[tb]
                for cc in range(NCC):
                    S.add('pe', lambda e: e.transpose(out=PSB[tb][:, cc * 128:(cc + 1) * 128],
                                                      in_=SEL[:, ti, cc * 128:(cc + 1) * 128], identity=identB),
                          r=[SELr, misc], w=[PSr[tb]])
                S.add('act', lambda e: e.copy(out=st, in_=PSB[tb][:, 0:CAP]), r=[PSr[tb]], w=[str_])
                for half in range(2):
                    bank = 2 + 2 * tb + half
                    for cc in range(NCC):
                        S.add('pe', lambda e: e.matmul(PS[bank], lhsT=st[:, cc * 128:(cc + 1) * 128],
                                                       rhs=YE[:, cc, half * 512:(half + 1) * 512],
                                                       start=(cc == 0), stop=(cc == NCC - 1)),
                              r=[str_, XEr], w=[PSr[bank]])
                    S.add('dve', lambda e: e.scalar_tensor_tensor(
                        out=ACC[:, ti, half * 512:(half + 1) * 512], in0=PS[bank], scalar=WG[:, ti, ex:ex + 1],
                        in1=ACC[:, ti, half * 512:(half + 1) * 512], op0=ALU.mult, op1=ALU.add),
                        r=[PSr[bank], WGr], w=[ACCr[ti]])
    else:
        moe_dense()

    barrier()
    A.off = LOOP_MARK
    XF = [A.alloc(F32, D) for _ in range(2)]; XFr = [Reg("xf0"), Reg("xf1")]
    for ti in range(16):
        xf = XF[ti % 2]; xfr = XFr[ti % 2]
        S.add('sp', lambda e, xf=xf, ti=ti: e.dma_start(out=xf, in_=X1d[ti * 128:(ti + 1) * 128, :]),
              w=[xfr], dma=xfr)
        S.add('dve', lambda e, ti=ti: e.tensor_tensor(out=ACC[:, ti, :], in0=ACC[:, ti, :], in1=G2, op=ALU.mult),
              r=[CONST, misc], w=[ACCr[ti]])
        S.add('pool', lambda e, ti=ti, xf=xf: e.tensor_tensor(out=xf, in0=ACC[:, ti, :], in1=xf, op=ALU.add),
              r=[ACCr[ti]], w=[xfr])
        S.add('sp', lambda e, xf=xf, ti=ti: e.dma_start(out=y[ti * 128:(ti + 1) * 128, :], in_=xf),
              r=[xfr], dma=xfr)
    S.wait_regs('sp', XFr)
    return finish(nc, S, stack)


def finish(nc, S, stack):
    S.prepare(nc, stack)
    with nc.Block() as block:
        @block.tensor
        def _(e):
            S.run('pe', e)

        @block.scalar
        def _(e):
            S.run('act', e)

        @block.vector
        def _(e):
            S.run('dve', e)

        @block.gpsimd
        def _(e):
            S.run('pool', e)

        @block.sync
        def _(e):
            S.run('sp', e)
    stack.close()
    return nc


def rope_table():
    inv = (np.float32(10000.0) ** (-(np.arange(0, 64, 2, dtype=np.float32) / np.float32(64)))).astype(np.float32)
    pos = np.arange(SEQ, dtype=np.float32)
    ang = (pos[:, None] * inv[None, :]).astype(np.float32)
    emb = np.concatenate([ang, ang], axis=-1)
    cos = np.cos(emb).astype(np.float32); sin = np.sin(emb).astype(np.float32)
    sin[:, 0:32] = -sin[:, 0:32]
    return np.concatenate([cos, sin], axis=1).astype(np.float32)


def make_in_maps(inp):
    x = np.asarray(inp['x'], dtype=np.float32)
    tab = rope_table()
    shared = {}
    for k in ('b_ada', 'norm1_w', 'q_norm_w', 'k_norm_w', 'lambda_q1', 'lambda_k1', 'lambda_q2', 'lambda_k2',
              'subln_w', 'norm2_w', 'b_router'):
        shared[k] = np.ascontiguousarray(np.asarray(inp[k], dtype=np.float32))
    for k in ('w_ada', 'w_in', 'w_attn_o', 'conv_w', 'w_conv_o', 'w_out', 'w_router', 'w_gate_up', 'b_gate_up',
              'w_down', 'b_down'):
        shared[k] = np.ascontiguousarray(np.asarray(inp[k], dtype=np.float32)[0])
    shared['ident'] = np.eye(128, dtype=np.float32)
    shared['tri'] = np.triu(np.ones((128, 128), np.float32), 1)
    shared['iota'] = np.ascontiguousarray(np.broadcast_to(np.arange(CAP, dtype=np.float32), (128, CAP)))
    maps = []
    for core in range(8):
        b = core // 4; t0 = (core % 4) * TOWN
        m = dict(shared)
        m['xs'] = np.ascontiguousarray(np.roll(x[b], -t0, axis=0))
        m['rope'] = np.ascontiguousarray(np.roll(tab, -t0, axis=0))
        xhal = np.zeros((2, D), np.float32); msk = np.zeros((128, 2), np.float32)
        if t0 > 0:
            xhal[0] = x[b, t0 - 1]; msk[:, 0] = 1.0
        if t0 + TOWN < SEQ:
            xhal[1] = x[b, t0 + TOWN]; msk[:, 1] = 1.0
        m['xh'] = xhal; m['hm'] = msk
        m['cvec'] = np.ascontiguousarray(np.asarray(inp['c'], dtype=np.float32)[b:b + 1])
        maps.append(m)
    return maps


_NC = None


def kernel(**inp):
    global _NC
    if _NC is None:
        _NC = build()
    maps = make_in_maps(inp)
    res = run_bass_kernel_spmd(_NC, maps, core_ids=list(range(8)))
    out = np.zeros((2, SEQ, D), np.float32)
    for core in range(8):
        b = core // 4; t0 = (core % 4) * TOWN
        out[b, t0:t0 + TOWN] = res.results[core]['y']
    return out
```

```python
import types
import numpy as np
from contextlib import ExitStack
import concourse.bass as bass
import concourse.mybir as mybir
from concourse.bass_utils import run_bass_kernel_spmd

F32 = mybir.dt.float32
BF = mybir.dt.bfloat16
ALU = mybir.AluOpType
AF = mybir.ActivationFunctionType
AX = mybir.AxisListType

SEQ = 8192
D = 1024
TOWN = 2048
NE = 32
ENGS = ['pe', 'act', 'dve', 'pool', 'sp']
DEBUG = None
MOE_GATHER = False
CAP = 384


class Reg:
    __slots__ = ('name', 'w', 'rs', 'dsem', 'dcnt', 'excl')
    ALL = []

    def __init__(s, name, excl=False):
        s.name = name; s.w = None; s.rs = {}; s.dsem = None; s.dcnt = 0; s.excl = excl
        Reg.ALL.append(s)


class Op:
    __slots__ = ('eng', 'fn', 'deps', 'needs_inc', 'sigval', 'dreg')


def freeze(fn):
    if fn is None or fn.__closure__ is None:
        return fn
    cells = []
    for c in fn.__closure__:
        try:
            cells.append(types.CellType(c.cell_contents))
        except ValueError:
            cells.append(c)
    return types.FunctionType(fn.__code__, fn.__globals__, fn.__name__, fn.__defaults__, tuple(cells))


class Sched:
    def __init__(s):
        s.q = {e: [] for e in ENGS}
        s.dregs = []

    def add(s, eng, fn, r=(), w=(), dma=None):
        op = Op(); op.eng = eng; op.fn = freeze(fn); op.needs_inc = False; op.sigval = 0; op.dreg = dma
        w = list(w) + [g for g in r if g.excl]
        r = [g for g in r if not g.excl]
        deps = []
        for g in r:
            if g.w is not None:
                deps.append(g.w)
        for g in w:
            if g.w is not None:
                deps.append(g.w)
            deps.extend(g.rs.values())
        op.deps = [d for d in deps if not (d[0] == 'op' and d[1].eng == eng and eng == 'pe')]
        if dma is not None:
            if dma.dcnt == 0 and dma not in s.dregs:
                s.dregs.append(dma)
            dma.dcnt += 16
            tok = ('dma', dma, dma.dcnt); key = ('d', id(dma))
        else:
            tok = ('op', op); key = eng
        for g in r:
            g.rs[key] = tok
        for g in w:
            g.w = tok; g.rs = {}
        s.q[eng].append(op)
        return op

    def wait_regs(s, eng, regs):
        op = Op(); op.eng = eng; op.fn = None; op.needs_inc = False; op.sigval = 0; op.dreg = None
        deps = []
        for g in regs:
            if g.w is not None:
                deps.append(g.w)
            deps.extend(g.rs.values())
        op.deps = deps
        s.q[eng].append(op)

    def barrier(s):
        deps = []
        for e in ENGS:
            for op in reversed(s.q[e]):
                if op.fn is not None and op.dreg is None:
                    deps.append(('op', op))
                    break
        for g in s.dregs:
            if g.dcnt > 0:
                deps.append(('dma', g, g.dcnt))
        for e in ENGS:
            op = Op(); op.eng = e; op.fn = None; op.needs_inc = False; op.sigval = 0; op.dreg = None
            op.deps = [d for d in deps if not (d[0] == 'op' and d[1].eng == e and e == 'pe')]
            s.q[e].append(op)
        for g in Reg.ALL:
            g.w = None; g.rs = {}

    def prepare(s, nc, stack):
        for e in ENGS:
            for op in s.q[e]:
                for d in op.deps:
                    if d[0] == 'op':
                        d[1].needs_inc = True
        for e in ENGS:
            c = 0
            for op in s.q[e]:
                if op.needs_inc:
                    c += 1
                    op.sigval = c
        s.esem = {e: stack.enter_context(nc.semaphore("sem_" + e)) for e in ENGS}
        for i, g in enumerate(s.dregs):
            g.dsem = stack.enter_context(nc.semaphore("dsem%d_%s" % (i, g.name)))

    def run(s, e, eng):
        known = {}
        for op in s.q[e]:
            for d in op.deps:
                if d[0] == 'op':
                    sem = s.esem[d[1].eng]; val = d[1].sigval; k = d[1].eng
                else:
                    sem = d[1].dsem; val = d[2]; k = id(d[1])
                if known.get(k, 0) < val:
                    eng.wait_ge(sem, val)
                    known[k] = val
            if op.fn is None:
                continue
            ins = op.fn(eng)
            if op.dreg is not None:
                ins.then_inc(op.dreg.dsem, 16)
            elif op.needs_inc:
                ins.then_inc(s.esem[e], 1)


class Arena:
    def __init__(s, t, nbytes):
        s.t = t; s.off = 0; s.cap = nbytes

    def alloc(s, dtype, *free):
        n = 1
        for f in free:
            n *= f
        esz = 2 if dtype == BF else 4
        nbytes = (n * esz + 63) // 64 * 64
        assert s.off + nbytes <= s.cap, ("arena overflow", s.off, nbytes, s.cap)
        a = s.t[:, s.off // 4:(s.off + nbytes) // 4]
        s.off += nbytes
        if dtype != F32:
            a = a.bitcast(dtype)
        a = a[:, 0:n]
        if len(free) == 2:
            a = a.rearrange("p (a b) -> p a b", b=free[1])
        elif len(free) == 3:
            a = a.rearrange("p (a b c) -> p a b c", b=free[1], c=free[2])
        return a


def build():
    nc = bass.Bass("TRN2", target_bir_lowering=False)

    def din(name, shape, dt=F32):
        return nc.dram_tensor(name, list(shape), dt, kind="ExternalInput").ap()

    xs = din("xs", [SEQ, D]); xh = din("xh", [2, D]); hm = din("hm", [128, 2])
    rope = din("rope", [SEQ, 128]); cvec = din("cvec", [1, D]); ident = din("ident", [128, 128])
    w_ada = din("w_ada", [D, 6 * D]); b_ada = din("b_ada", [1, 6 * D])
    norm1_w = din("norm1_w", [1, D]); w_in = din("w_in", [D, 8 * D])
    q_norm_w = din("q_norm_w", [1, 64]); k_norm_w = din("k_norm_w", [1, 64])
    lq1 = din("lambda_q1", [1, 64]); lk1 = din("lambda_k1", [1, 64])
    lq2 = din("lambda_q2", [1, 64]); lk2 = din("lambda_k2", [1, 64])
    subln_w = din("subln_w", [1, 128]); w_attn_o = din("w_attn_o", [D, D])
    conv_w = din("conv_w", [3, D]); w_conv_o = din("w_conv_o", [D, D]); w_out = din("w_out", [D, D])
    norm2_w = din("norm2_w", [1, D]); w_router = din("w_router", [D, NE]); b_router = din("b_router", [1, NE])
    w_gate_up = din("w_gate_up", [NE, D, 2 * D]); b_gate_up = din("b_gate_up", [NE, 2 * D])
    w_down = din("w_down", [NE, D, D]); b_down = din("b_down", [NE, D])
    y = nc.dram_tensor("y", [TOWN, D], F32, kind="ExternalOutput").ap()
    KTd = nc.dram_tensor("ktd", [128, 8, SEQ], BF, kind="Internal").ap()
    Vd = nc.dram_tensor("vd", [8, SEQ, 128], BF, kind="Internal").ap()
    X1d = nc.dram_tensor("x1d", [TOWN, D], F32, kind="Internal").ap()
    H2d = nc.dram_tensor("h2d", [TOWN, D], BF, kind="Internal").ap()
    tri = din("tri", [128, 128]); iota = din("iota", [128, CAP])
    dbg = None
    if DEBUG == 'attn':
        dbg = nc.dram_tensor("dbg", [128, 8 * TOWN], BF, kind="ExternalOutput").ap()
    elif DEBUG == 'x1':
        dbg = nc.dram_tensor("dbg", [TOWN, 32], F32, kind="ExternalOutput").ap()
    elif DEBUG == 'p4':
        dbg = nc.dram_tensor("dbg", [128, 4, 8 * 514], BF, kind="ExternalOutput").ap()

    S = Sched()
    stack = ExitStack()
    ARENA_BYTES = 188 * 1024
    arena_t = stack.enter_context(nc.sbuf_tensor("arena", [128, ARENA_BYTES // 4], F32))
    A = Arena(arena_t, ARENA_BYTES)
    PS = []
    PSr = []
    SXYs = []
    for i in range(2):
        sxy = stack.enter_context(nc.psum_tensor("sxy%d" % i, [128, 1024], F32))
        SXYs.append(sxy[:, :])
        PS.append(sxy[:, 0:512]); PS.append(sxy[:, 512:1024])
    for i in range(4, 8):
        t = stack.enter_context(nc.psum_tensor("ps%d" % i, [128, 512], F32))
        PS.append(t[:, :])
    for i in range(8):
        PSr.append(Reg("ps%d" % i, excl=True))
    PSB = [p.bitcast(BF) for p in PS]

    identF = A.alloc(F32, 128); identB = A.alloc(BF, 128)
    OT = A.alloc(BF, 8, TOWN)
    G2 = A.alloc(F32, D)
    WG = A.alloc(F32, 16, NE)
    P5_MARK = A.off
    SH1 = A.alloc(F32, D); G1 = A.alloc(F32, D); SH2 = A.alloc(F32, D)
    W1 = A.alloc(F32, D); W2 = A.alloc(F32, D)
    KNW = A.alloc(F32, 64); KNWS = A.alloc(F32, 64); QNW = A.alloc(F32, 64); QNWS = A.alloc(F32, 64)
    SUBW = A.alloc(F32, 128); NLAM = A.alloc(F32, 2)
    CW = A.alloc(F32, 3, 8); HM = A.alloc(F32, 2); BR = A.alloc(F32, NE); WRT = A.alloc(F32, 8, NE)
    PASS_MARK = A.off

    CONST = Reg("const")
    OTr = [Reg("ot%d" % g) for g in range(4)]
    WGr = Reg("wg")
    misc = Reg("misc")

    def cload(out, in_, **kw):
        S.add('sp', lambda e: e.dma_start(out=out, in_=in_, **kw), dma=CONST)

    def row_b(ap1d):
        return ap1d.partition_broadcast(128)

    SC1 = A.alloc(F32, D); SC2 = A.alloc(F32, D)
    cT = A.alloc(F32, 8); scT = A.alloc(F32, 8); scB = A.alloc(F32, 8, 128)
    LQ = [A.alloc(F32, 64) for _ in range(4)]
    LT = A.alloc(F32, 8)
    WA = [A.alloc(F32, 8, 512) for _ in range(2)]
    WAr = [Reg("wa0"), Reg("wa1")]

    cload(identF, ident)
    mod_tiles = [SH1, SC1, G1, SH2, SC2, G2]
    for i, t in enumerate(mod_tiles):
        cload(t, row_b(b_ada[0, i * D:(i + 1) * D]))
    cload(W1, row_b(norm1_w[0, :])); cload(W2, row_b(norm2_w[0, :]))
    cload(KNW, row_b(k_norm_w[0, :])); cload(QNW, row_b(q_norm_w[0, :]))
    cload(KNWS[:, 0:32], row_b(k_norm_w[0, 32:64])); cload(KNWS[:, 32:64], row_b(k_norm_w[0, 0:32]))
    cload(QNWS[:, 0:32], row_b(q_norm_w[0, 32:64])); cload(QNWS[:, 32:64], row_b(q_norm_w[0, 0:32]))
    cload(SUBW, row_b(subln_w[0, :]))
    for t, src in zip(LQ, (lq1, lk1, lq2, lk2)):
        cload(t, row_b(src[0, :]))
    cload(HM, hm); cload(BR, row_b(b_router[0, :]))
    cload(WRT, w_router.rearrange("(kc p) e -> p kc e", p=128))
    for k3 in range(3):
        cload(CW[:, k3, :], conv_w[k3, :].rearrange("(j p) -> p j", p=128), allow_slow_non_contiguous=True)
    cload(cT, cvec.rearrange("o (j p) -> p (o j)", p=128), allow_slow_non_contiguous=True)
    CONST.w = ('dma', CONST, CONST.dcnt)

    S.add('dve', lambda e: e.tensor_copy(out=identB, in_=identF), r=[CONST], w=[misc])
    S.add('pool', lambda e: e.memset(NLAM[:, 1:2], -0.5), w=[misc])
    S.add('act', lambda e: e.activation(out=scT, in_=cT, func=AF.Sigmoid), r=[CONST], w=[misc])
    S.add('dve', lambda e: e.tensor_tensor(out=scT, in0=scT, in1=cT, op=ALU.mult), r=[misc, CONST], w=[misc])
    S.add('dve', lambda e: e.tensor_copy(out=scB, in_=scT.unsqueeze(2).broadcast_to([128, 8, 128])),
          r=[misc], w=[misc])
    for i in range(2):
        S.add('dve', lambda e, i=i: e.tensor_tensor(out=LQ[2 * i], in0=LQ[2 * i], in1=LQ[2 * i + 1], op=ALU.mult),
              r=[CONST, misc], w=[misc])
        S.add('dve', lambda e, i=i: e.tensor_reduce(out=LT[:, i:i + 1], in_=LQ[2 * i], axis=AX.X, op=ALU.add),
              r=[misc], w=[misc])
    S.add('act', lambda e: e.activation(out=LT[:, 2:4], in_=LT[:, 0:2], func=AF.Exp), r=[misc], w=[misc])
    S.add('dve', lambda e: e.tensor_tensor(out=LT[:, 4:5], in0=LT[:, 2:3], in1=LT[:, 3:4], op=ALU.subtract),
          r=[misc], w=[misc])
    S.add('dve', lambda e: e.tensor_scalar(out=NLAM[:, 0:1], in0=LT[:, 4:5], scalar1=0.2, scalar2=-1.0,
                                           op0=ALU.add, op1=ALU.mult), r=[misc], w=[misc])
    S.add('dve', lambda e: e.tensor_scalar(out=SUBW, in0=SUBW, scalar1=0.8, scalar2=None, op0=ALU.mult),
          r=[CONST, misc], w=[misc])

    for cg in range(12):
        wa = WA[cg % 2]; war = WAr[cg % 2]
        S.add('sp', lambda e, wa=wa, cg=cg: e.dma_start(
            out=wa, in_=w_ada[:, cg * 512:(cg + 1) * 512].rearrange("(kc p) n -> p kc n", p=128)),
            w=[war], dma=war)
        bank = cg % 2
        for kc in range(8):
            S.add('pe', lambda e, wa=wa, kc=kc, bank=bank: e.matmul(
                PS[bank], lhsT=scB[:, kc, :], rhs=wa[:, kc, :], start=(kc == 0), stop=(kc == 7)),
                r=[misc, war], w=[PSr[bank]])
        dst = mod_tiles[cg // 2][:, (cg % 2) * 512:(cg % 2 + 1) * 512]
        S.add('dve', lambda e, dst=dst, bank=bank: e.tensor_tensor(out=dst, in0=PS[bank], in1=dst, op=ALU.add),
              r=[PSr[bank], CONST, misc], w=[misc])
    S.add('dve', lambda e: e.scalar_tensor_tensor(out=W1, in0=SC1, scalar=1.0, in1=W1, op0=ALU.add, op1=ALU.mult),
          r=[misc, CONST], w=[misc])
    S.add('dve', lambda e: e.scalar_tensor_tensor(out=W2, in0=SC2, scalar=1.0, in1=W2, op0=ALU.add, op1=ALU.mult),
          r=[misc, CONST], w=[misc])

    barrier = S.barrier

    nrm_r = Reg("nrm")

    def norm_tile(xt, xr, Wt, SHt, junk, ssq, tmp, outs, regs_out, npart=128, tag="n"):
        tr = nrm_r
        S.add('act', lambda e: e.activation(out=junk[0:npart], in_=xt[0:npart], func=AF.Square,
                                            accum_out=ssq[0:npart, 0:1]), r=[xr], w=[tr])
        S.add('act', lambda e: e.activation(out=ssq[0:npart, 1:2], in_=ssq[0:npart, 0:1], func=AF.Sqrt,
                                            scale=1.0 / D, bias=1e-6), r=[tr], w=[tr])
        S.add('dve', lambda e: e.reciprocal(out=ssq[0:npart, 2:3], in_=ssq[0:npart, 1:2]), r=[tr], w=[tr])
        S.add('dve', lambda e: e.scalar_tensor_tensor(out=tmp[0:npart], in0=xt[0:npart], scalar=ssq[0:npart, 2:3],
                                                      in1=Wt[0:npart], op0=ALU.mult, op1=ALU.mult),
              r=[tr, xr, misc], w=[tr])
        for o, ro in zip(outs, regs_out):
            S.add('dve', lambda e, o=o: e.tensor_tensor(out=o[0:npart], in0=tmp[0:npart], in1=SHt[0:npart],
                                                        op=ALU.add), r=[tr, misc, xr], w=[ro])

    barrier()
    A.off = PASS_MARK
    QT = A.alloc(BF, 8, TOWN); QTr = Reg("qt")
    WKV = A.alloc(BF, 8, 2048); WKVr = Reg("wkv")
    WQ = A.alloc(BF, 8, 1024); WQr = Reg("wq")
    XT = [A.alloc(F32, D) for _ in range(2)]; XTr = [Reg("xt0"), Reg("xt1")]
    RT = [A.alloc(F32, 128) for _ in range(2)]; RTr = [Reg("rt0"), Reg("rt1")]
    Hb2 = [A.alloc(BF, D) for _ in range(2)]; Hr2 = [Reg("h0"), Reg("h1")]
    HT2 = [A.alloc(BF, 8, 128) for _ in range(2)]; HTr2 = [Reg("ht0"), Reg("ht1")]
    JUNK = A.alloc(BF, D)
    SSQ = A.alloc(F32, 4)
    SQ = A.alloc(F32, D)
    T1 = A.alloc(F32, D); T2 = A.alloc(F32, D)
    S16 = A.alloc(F32, 48)
    AB = A.alloc(F32, 256)
    KB2 = [A.alloc(BF, D) for _ in range(2)]; KBr2 = [Reg("kb0"), Reg("kb1")]
    QB = A.alloc(BF, D); QBr = Reg("qb")
    KTt = [A.alloc(BF, 8, 128) for _ in range(2)]; KTtr = [Reg("ktt0"), Reg("ktt1")]
    Vt = [A.alloc(BF, D) for _ in range(2)]; Vtr = [Reg("vt0"), Reg("vt1")]
    p1r = Reg("p1")

    for half in range(2):
        S.add('pool', lambda e, half=half: e.dma_start(
            out=WKV[:, :, half * 1024:(half + 1) * 1024],
            in_=w_in[:, 1024 + half * 1024:2048 + half * 1024].rearrange("(kc p) n -> p kc n", p=128)),
            dma=WKVr)
    WKVr.w = ('dma', WKVr, WKVr.dcnt)
    S.add('pool', lambda e: e.dma_start(out=WQ, in_=w_in[:, 0:1024].rearrange("(kc p) n -> p kc n", p=128)),
          w=[WQr], dma=WQr)

    kpost_r = Reg("kpost")

    def qk_post(banks, A_, B_, outbf, outr, tag):
        tr = kpost_r
        for hh in range(2):
            S.add('act', lambda e, hh=hh: e.activation(out=SQ[:, hh * 512:(hh + 1) * 512], in_=PS[banks[hh]],
                                                       func=AF.Square), r=[PSr[banks[hh]]], w=[tr])
        S.add('dve', lambda e: e.tensor_reduce(out=S16[:, 0:16], in_=SQ.rearrange("p (g d) -> p g d", d=64),
                                               axis=AX.X, op=ALU.add), r=[tr], w=[tr])
        S.add('act', lambda e: e.activation(out=S16[:, 16:32], in_=S16[:, 0:16], func=AF.Sqrt,
                                            scale=1.0 / 64, bias=1e-6), r=[tr], w=[tr])
        S.add('dve', lambda e: e.reciprocal(out=S16[:, 32:48], in_=S16[:, 16:32]), r=[tr], w=[tr])
        for hh in range(2):
            pv = PS[banks[hh]].rearrange("p (g d) -> p g d", d=64)
            t1 = T1[:, hh * 512:(hh + 1) * 512].rearrange("p (g d) -> p g d", d=64)
            t2 = T2[:, hh * 512:(hh + 1) * 512].rearrange("p (g d) -> p g d", d=64)
            S.add('dve', lambda e, pv=pv, t1=t1: e.tensor_tensor(
                out=t1, in0=pv, in1=A_.unsqueeze(1).broadcast_to([128, 8, 64]), op=ALU.mult),
                r=[PSr[banks[hh]], p1r], w=[tr])
            S.add('dve', lambda e, pv=pv, t2=t2: e.tensor_tensor(
                out=t2[:, :, 0:32], in0=pv[:, :, 32:64], in1=B_[:, 0:32].unsqueeze(1).broadcast_to([128, 8, 32]),
                op=ALU.mult), r=[PSr[banks[hh]], p1r], w=[tr])
            S.add('dve', lambda e, pv=pv, t2=t2: e.tensor_tensor(
                out=t2[:, :, 32:64], in0=pv[:, :, 0:32], in1=B_[:, 32:64].unsqueeze(1).broadcast_to([128, 8, 32]),
                op=ALU.mult), r=[PSr[banks[hh]], p1r], w=[tr])
        S.add('pool', lambda e: e.tensor_tensor(out=T1, in0=T1, in1=T2, op=ALU.add), r=[tr], w=[tr])
        S.add('dve', lambda e: e.tensor_tensor(
            out=outbf.rearrange("p (g d) -> p g d", d=64), in0=T1.rearrange("p (g d) -> p g d", d=64),
            in1=S16[:, 32:48].unsqueeze(2).broadcast_to([128, 16, 64]), op=ALU.mult), r=[tr], w=[outr])

    NT = SEQ // 128
    NOWN = TOWN // 128

    def kbanks(j):
        return [1, 2] if (j < NOWN or j % 2 == 0) else [6, 7]

    def st_N(j):
        xt = XT[j % 2]; xr = XTr[j % 2]; rt = RT[j % 2]; rr = RTr[j % 2]
        S.add('sp', lambda e: e.dma_start(out=xt, in_=xs[j * 128:(j + 1) * 128, :]), w=[xr], dma=xr)
        S.add('sp', lambda e: e.dma_start(out=rt, in_=rope[j * 128:(j + 1) * 128, :]), w=[rr], dma=rr)
        norm_tile(xt, xr, W1, SH1, JUNK, SSQ, xt, [Hb2[j % 2]], [Hr2[j % 2]], tag="n1")

    def st_T(j):
        hb = Hb2[j % 2]; hr = Hr2[j % 2]; ht = HT2[j % 2]; htr = HTr2[j % 2]
        for kc in range(8):
            S.add('pe', lambda e: e.transpose(out=PSB[0][:, kc * 128:(kc + 1) * 128],
                                              in_=hb[:, kc * 128:(kc + 1) * 128], identity=identB),
                  r=[hr, misc], w=[PSr[0]])
        S.add('act', lambda e: e.copy(out=ht.rearrange("p a b -> p (a b)"), in_=PSB[0]), r=[PSr[0]], w=[htr])

    def st_M(j):
        ht = HT2[j % 2]; htr = HTr2[j % 2]
        kb_ = kbanks(j)
        banks = [kb_[0], kb_[1], 3, 4]
        for n in range(4):
            for kc in range(8):
                S.add('pe', lambda e: e.matmul(
                    PS[banks[n]], lhsT=ht[:, kc, :], rhs=WKV[:, kc, n * 512:(n + 1) * 512],
                    start=(kc == 0), stop=(kc == 7)), r=[htr, WKVr], w=[PSr[banks[n]]])
        if j < NOWN:
            for n in range(2):
                for kc in range(8):
                    S.add('pe', lambda e: e.matmul(
                        PS[6 + n], lhsT=ht[:, kc, :], rhs=WQ[:, kc, n * 512:(n + 1) * 512],
                        start=(kc == 0), stop=(kc == 7)), r=[htr, WQr], w=[PSr[6 + n]])

    def st_E(j):
        own = j < NOWN
        rt = RT[j % 2]; rr = RTr[j % 2]
        vt = Vt[j % 2]; vr = Vtr[j % 2]
        for n in range(2):
            S.add('act', lambda e: e.copy(out=vt[:, n * 512:(n + 1) * 512], in_=PS[3 + n]),
                  r=[PSr[3 + n]], w=[vr])
        S.add('sp', lambda e: e.dma_start(
            out=Vd[:, j * 128:(j + 1) * 128, :].rearrange("h t d -> t h d"),
            in_=vt.rearrange("p (h d) -> p h d", d=128)), r=[vr], dma=vr)
        S.add('dve', lambda e: e.tensor_tensor(out=AB[:, 0:64], in0=rt[:, 0:64], in1=KNW, op=ALU.mult),
              r=[rr, CONST], w=[p1r])
        S.add('dve', lambda e: e.tensor_tensor(out=AB[:, 64:128], in0=rt[:, 64:128], in1=KNWS, op=ALU.mult),
              r=[rr, CONST], w=[p1r])
        if own:
            S.add('dve', lambda e: e.tensor_tensor(out=AB[:, 128:192], in0=rt[:, 0:64], in1=QNW, op=ALU.mult),
                  r=[rr, CONST], w=[p1r])
            S.add('dve', lambda e: e.tensor_tensor(out=AB[:, 192:256], in0=rt[:, 64:128], in1=QNWS,
                                                   op=ALU.mult), r=[rr, CONST], w=[p1r])
        qk_post(kbanks(j), AB[:, 0:64], AB[:, 64:128], KB2[j % 2], KBr2[j % 2], "kpost")
        if own:
            qk_post([6, 7], AB[:, 128:192], AB[:, 192:256], QB, QBr, "qpost")

    def st_K(j):
        own = j < NOWN
        kb = KB2[j % 2]; kbr = KBr2[j % 2]
        ktt = KTt[j % 2]; ktr = KTtr[j % 2]
        for h in range(8):
            S.add('pe', lambda e: e.transpose(out=PSB[5][:, h * 128:(h + 1) * 128],
                                              in_=kb[:, h * 128:(h + 1) * 128], identity=identB),
                  r=[kbr, misc], w=[PSr[5]])
        S.add('act', lambda e: e.copy(out=ktt.rearrange("p a b -> p (a b)"), in_=PSB[5]),
              r=[PSr[5]], w=[ktr])
        S.add('sp', lambda e: e.dma_start(out=KTd[:, :, j * 128:(j + 1) * 128], in_=ktt),
              r=[ktr], dma=ktr)
        if own:
            qb_ = QB; qbr = QBr
            for h in range(8):
                S.add('pe', lambda e: e.transpose(out=PSB[5][:, h * 128:(h + 1) * 128],
                                                  in_=qb_[:, h * 128:(h + 1) * 128], identity=identB),
                      r=[qbr, misc], w=[PSr[5]])
            S.add('act', lambda e: e.copy(out=QT[:, :, j * 128:(j + 1) * 128],
                                          in_=PSB[5].rearrange("p (a b) -> p a b", b=128)),
                  r=[PSr[5]], w=[QTr])

    st_N(0); st_N(1); st_T(0); st_M(0)
    for j in range(NT):
        if j + 1 < NT:
            st_T(j + 1)
        st_E(j)
        if j + 1 < NT:
            st_M(j + 1)
        if j + 2 < NT:
            st_N(j + 2)
        st_K(j)

    barrier()
    A.off = PASS_MARK + 2 * 8 * TOWN
    KTb = [A.alloc(BF, SEQ) for _ in range(2)]; KTbr = [Reg("ktb0"), Reg("ktb1")]
    Vb = [A.alloc(BF, 64, 130) for _ in range(2)]; Vbr = [Reg("vb0"), Reg("vb1")]
    PT = [A.alloc(BF, 512) for _ in range(3)]; PTr = [Reg("pt%d" % i) for i in range(3)]
    RC = A.alloc(F32, 8)
    OS = [A.alloc(F32, 128) for _ in range(2)]
    ONB = [A.alloc(BF, 128) for _ in range(2)]
    JKF = A.alloc(F32, 128)
    ppr = [Reg("pp0"), Reg("pp1")]
    Sr = [Reg("s0", excl=True), Reg("s1", excl=True)]
    for b in range(2):
        S.add('pool', lambda e, b=b: e.memset(Vb[b][:, :, 128:129], 1.0), w=[Vbr[b]])

    for h in range(8):
        kt = KTb[h % 2]; ktr = KTbr[h % 2]; vb = Vb[h % 2]; vbr = Vbr[h % 2]
        S.add('sp', lambda e, kt=kt, h=h: e.dma_start(out=kt, in_=KTd[:, h, :]), w=[ktr], dma=ktr)
        S.add('sp', lambda e, vb=vb, h=h: e.dma_start(
            out=vb[:, :, 0:128], in_=Vd[h].rearrange("(kc p) d -> p kc d", p=128)), w=[vbr], dma=vbr)
        def emit_qk(i, kt=kt, ktr=ktr, h=h):
            qt, kc = divmod(i, 64); q0 = qt * 256; sb = i % 2
            for c in range(2):
                S.add('pe', lambda e: e.matmul(
                    PS[2 * sb + c][:, 0:256], lhsT=kt[c * 64:(c + 1) * 64, kc * 128:(kc + 1) * 128],
                    rhs=QT[c * 64:(c + 1) * 64, h, q0:q0 + 256], start=True, stop=True),
                    r=[ktr, QTr], w=[Sr[sb]])

        emit_qk(0)
        emit_qk(1)
        pending = []
        for it in range(512):
            qt, kc = divmod(it, 64); q0 = qt * 256
            sb = it % 2; pt = PT[it % 3]; ptr = PTr[it % 3]
            S.add('act', lambda e: e.activation(
                out=pt.rearrange("p (c n) -> p c n", n=256),
                in_=SXYs[sb].rearrange("p (c n) -> p c n", n=512)[:, :, 0:256],
                func=AF.Exp, scale=0.125), r=[Sr[sb]], w=[ptr])
            if it + 2 < 512:
                emit_qk(it + 2)
            for qb in range(2):
                for c in range(2):
                    bank = 4 + qb
                    S.add('pe', lambda e: e.matmul(
                        PS[bank][:, c * 256:c * 256 + 129], lhsT=pt[:, c * 256 + qb * 128:c * 256 + qb * 128 + 128],
                        rhs=vb[:, kc, 0:129], start=(kc == 0 and c == 0), stop=(kc == 63),
                        skip_group_check=True),
                        r=[ptr, vbr], w=[PSr[bank]])
            if pending and (kc == 12 or it == 511):
                for fn_ in pending:
                    fn_()
                pending = []
            if kc != 63:
                continue
            for qb in range(2):
                bk = 4 + qb
                pr = ppr[qb]; os_ = OS[qb]; onb = ONB[qb]
                rc = RC[:, qb * 4:(qb + 1) * 4]
                S.add('dve', lambda e, bk=bk, rc=rc: e.reciprocal(out=rc[:, 0:1], in_=PS[bk][:, 128:129]),
                      r=[PSr[bk]], w=[pr])
                S.add('dve', lambda e, bk=bk, rc=rc: e.reciprocal(out=rc[:, 1:2], in_=PS[bk][:, 384:385]),
                      r=[PSr[bk]], w=[pr])
                S.add('dve', lambda e, rc=rc: e.tensor_tensor(out=rc[:, 1:2], in0=rc[:, 1:2], in1=NLAM[:, 0:1],
                                                              op=ALU.mult), r=[pr, misc], w=[pr])
                S.add('dve', lambda e, bk=bk, rc=rc, os_=os_: e.tensor_scalar(
                    out=os_, in0=PS[bk][:, 0:128], scalar1=rc[:, 0:1], scalar2=None, op0=ALU.mult),
                    r=[PSr[bk], pr], w=[pr])
                S.add('dve', lambda e, bk=bk, rc=rc, os_=os_: e.scalar_tensor_tensor(
                    out=os_, in0=PS[bk][:, 256:384], scalar=rc[:, 1:2], in1=os_, op0=ALU.mult, op1=ALU.add),
                    r=[PSr[bk], pr], w=[pr])
                S.add('dve', lambda e, os_=os_: e.tensor_tensor(out=JKF, in0=os_, in1=os_, op=ALU.mult),
                      r=[pr], w=[pr])
                S.add('dve', lambda e, rc=rc: e.tensor_reduce(out=rc[:, 2:3], in_=JKF, axis=AX.X, op=ALU.add),
                      r=[pr], w=[pr])
                S.add('dve', lambda e, rc=rc: e.tensor_scalar(out=rc[:, 2:3], in0=rc[:, 2:3], scalar1=1.0 / 128,
                                                              scalar2=1e-5, op0=ALU.mult, op1=ALU.add),
                      r=[pr], w=[pr])
                S.add('pool', lambda e, rc=rc: e.tensor_tensor(out=rc[:, 3:4], in0=rc[:, 2:3],
                                                               in1=NLAM[:, 1:2], op=ALU.pow), r=[pr, misc], w=[pr])
                S.add('dve', lambda e, os_=os_, rc=rc, onb=onb: e.scalar_tensor_tensor(
                    out=onb, in0=os_, scalar=rc[:, 3:4], in1=SUBW, op0=ALU.mult, op1=ALU.mult),
                    r=[pr, misc], w=[pr])
                def fin_post(onb=onb, pr=pr, h=h, q0=q0, qb=qb):
                    S.add('pe', lambda e: e.transpose(out=PSB[7][:, 0:128], in_=onb, identity=identB),
                          r=[pr, misc], w=[PSr[7]])
                    g = (q0 + qb * 128) // 512
                    S.add('dve', lambda e: e.tensor_copy(
                        out=OT[:, h, q0 + qb * 128:q0 + qb * 128 + 128], in_=PSB[7][:, 0:128]),
                        r=[PSr[7]], w=[OTr[g]])
                pending.append(fin_post)
            if it == 511:
                for fn_ in pending:
                    fn_()
                pending = []

    if DEBUG == 'attn':
        barrier()
        S.add('sp', lambda e: e.dma_start(out=dbg, in_=OT.rearrange("p a b -> p (a b)")), r=OTr, dma=misc)
        S.wait_regs('sp', [misc])
        return finish(nc, S, stack)

    barrier()
    A.off = PASS_MARK
    RING = [A.alloc(BF, 8, 1024) for _ in range(3)]; RINGr = [Reg("ring%d" % i) for i in range(3)]
    ring_i = [0]

    def ring_load(src2d):
        i = ring_i[0] % len(RING); ring_i[0] += 1
        S.add('pool', lambda e, i=i: e.dma_start(out=RING[i], in_=src2d.rearrange("(kc p) n -> p kc n", p=128)),
              w=[RINGr[i]], dma=RINGr[i])
        return RING[i], RINGr[i]

    XG = [A.alloc(F32, D) for _ in range(2)]; XGr = [Reg("xg0"), Reg("xg1")]
    Hb = A.alloc(BF, D); Hr = Reg("h4")
    HTG = A.alloc(BF, 8, 514); HTGr = Reg("htg")
    JUNK = A.alloc(BF, D); SSQ = A.alloc(F32, 4); TMP = A.alloc(F32, D)
    CCs = A.alloc(F32, 512); CCr = Reg("ccs")
    ZP = A.alloc(BF, 8, 514); ZPr = Reg("zp")
    ZH = A.alloc(F32, 32)
    CT = A.alloc(F32, 512)
    U = A.alloc(BF, 8, 512); Ur = Reg("u")
    SG = A.alloc(BF, 512); SGr = Reg("sg")
    MC = A.alloc(BF, 8, 512); MCr = Reg("mc")
    M = U; Mr = Ur
    TMP2 = A.alloc(F32, D)
    X1 = A.alloc(F32, D); X1r = Reg("x1")
    H2 = A.alloc(F32, D); H2r = Reg("h2")
    H2B = A.alloc(BF, D); H2Br = Reg("h2b")
    H2T32 = TMP2.rearrange("p (a b) -> p a b", b=128); H2Tr = Reg("h2t32")
    LG = A.alloc(F32, NE); MX = A.alloc(F32, 8); EX = A.alloc(F32, NE); MK = A.alloc(F32, NE)
    SM = A.alloc(F32, 4)
    rr4 = Reg("r4")
    p4 = Reg("p4")

    COL = dict(cb=3072, cc=4096, cx=5120, ga=6144, gc=7168)
    for g in range(4):
        q0 = g * 512
        for tt in range(5):
            xt = XG[tt % 2]; xr = XGr[tt % 2]
            if tt < 4:
                npart = 128
                S.add('sp', lambda e, xt=xt, q0=q0, tt=tt: e.dma_start(
                    out=xt, in_=xs[q0 + tt * 128:q0 + (tt + 1) * 128, :]), w=[xr], dma=xr)
            else:
                npart = 2
                S.add('sp', lambda e, xt=xt, g=g, q0=q0: e.dma_start(
                    out=xt[0:1, :], in_=(xs[q0 - 1:q0, :] if g > 0 else xh[0:1, :])), w=[xr], dma=xr)
                S.add('sp', lambda e, xt=xt, g=g, q0=q0: e.dma_start(
                    out=xt[1:2, :], in_=(xs[q0 + 512:q0 + 513, :] if g < 3 else xh[1:2, :])), w=[xr], dma=xr)
            norm_tile(xt, xr, W1, SH1, JUNK, SSQ, TMP, [Hb], [Hr], npart=npart, tag="n4")
            for kc in range(8):
                S.add('pe', lambda e, kc=kc, npart=npart: e.transpose(
                    out=PSB[0][:, kc * 128:kc * 128 + npart], in_=Hb[0:npart, kc * 128:(kc + 1) * 128],
                    identity=identB[0:npart, 0:npart]), r=[Hr, misc], w=[PSr[0]])
            c0 = tt * 128
            S.add('act', lambda e, c0=c0, npart=npart: e.copy(
                out=HTG[:, :, c0:c0 + npart], in_=PSB[0].rearrange("p (a b) -> p a b", b=128)[:, :, 0:npart]),
                r=[PSr[0]], w=[HTGr])
        wcc, wccr = ring_load(w_in[:, COL['cc']:COL['cc'] + 1024])
        wcx, wcxr = ring_load(w_in[:, COL['cx']:COL['cx'] + 1024])
        for fc in range(8):
            for (wt, wr, bank) in ((wcc, wccr, 1), (wcx, wcxr, 2)):
                for kc in range(8):
                    S.add('pe', lambda e, wt=wt, bank=bank, kc=kc, fc=fc: e.matmul(
                        PS[bank], lhsT=wt[:, kc, fc * 128:(fc + 1) * 128], rhs=HTG[:, kc, 0:512],
                        start=(kc == 0), stop=(kc == 7)), r=[wr, HTGr], w=[PSr[bank]])
            for wi, (wt, wr) in enumerate(((wcc, wccr), (wcx, wcxr))):
                for kc in range(8):
                    S.add('pe', lambda e, wt=wt, kc=kc, fc=fc, wi=wi: e.matmul(
                        PS[3][:, (fc * 2 + wi) * 2:(fc * 2 + wi) * 2 + 2], lhsT=wt[:, kc, fc * 128:(fc + 1) * 128],
                        rhs=HTG[:, kc, 512:514], start=(kc == 0), stop=(kc == 7)), r=[wr, HTGr], w=[PSr[3]])
            S.add('act', lambda e: e.copy(out=CCs, in_=PS[1]), r=[PSr[1]], w=[CCr])
            S.add('dve', lambda e, fc=fc: e.tensor_tensor(out=ZP[:, fc, 1:513], in0=PS[2], in1=CCs, op=ALU.mult),
                  r=[PSr[2], CCr], w=[ZPr])
        S.add('act', lambda e: e.copy(out=ZH, in_=PS[3][:, 0:32]), r=[PSr[3]], w=[p4])
        zh4 = ZH.rearrange("p (f w c) -> p f w c", w=2, c=2)
        for ci, col in ((0, 0), (1, 513)):
            S.add('dve', lambda e, ci=ci, col=col: e.tensor_tensor(
                out=ZP[:, :, col:col + 1], in0=zh4[:, :, 0, ci:ci + 1], in1=zh4[:, :, 1, ci:ci + 1], op=ALU.mult),
                r=[p4], w=[ZPr])
        if g == 0:
            S.add('dve', lambda e: e.tensor_scalar(out=ZP[:, :, 0:1], in0=ZP[:, :, 0:1], scalar1=HM[:, 0:1],
                                                   scalar2=None, op0=ALU.mult), r=[CONST], w=[ZPr])
        if g == 3:
            S.add('dve', lambda e: e.tensor_scalar(out=ZP[:, :, 513:514], in0=ZP[:, :, 513:514],
                                                   scalar1=HM[:, 1:2], scalar2=None, op0=ALU.mult),
                  r=[CONST], w=[ZPr])
        wcb, wcbr = ring_load(w_in[:, COL['cb']:COL['cb'] + 1024])
        for fc in range(8):
            bank = 1 + fc % 2
            for kc in range(8):
                S.add('pe', lambda e, bank=bank, kc=kc, fc=fc: e.matmul(
                    PS[bank], lhsT=wcb[:, kc, fc * 128:(fc + 1) * 128], rhs=HTG[:, kc, 0:512],
                    start=(kc == 0), stop=(kc == 7)), r=[wcbr, HTGr], w=[PSr[bank]])
            S.add('dve', lambda e, fc=fc: e.tensor_scalar(out=CT, in0=ZP[:, fc, 0:512], scalar1=CW[:, 0, fc:fc + 1],
                                                          scalar2=None, op0=ALU.mult), r=[ZPr, CONST], w=[p4])
            S.add('dve', lambda e, fc=fc: e.scalar_tensor_tensor(out=CT, in0=ZP[:, fc, 1:513], scalar=CW[:, 1, fc:fc + 1],
                                                                 in1=CT, op0=ALU.mult, op1=ALU.add),
                  r=[ZPr, CONST, p4], w=[p4])
            S.add('dve', lambda e, fc=fc: e.scalar_tensor_tensor(out=CT, in0=ZP[:, fc, 2:514], scalar=CW[:, 2, fc:fc + 1],
                                                                 in1=CT, op0=ALU.mult, op1=ALU.add),
                  r=[ZPr, CONST, p4], w=[p4])
            S.add('dve', lambda e, fc=fc, bank=bank: e.tensor_tensor(out=U[:, fc, :], in0=PS[bank], in1=CT,
                                                                      op=ALU.mult), r=[PSr[bank], p4], w=[Ur])
        wco, wcor = ring_load(w_conv_o[:, :])
        wgc, wgcr = ring_load(w_in[:, COL['gc']:COL['gc'] + 1024])
        for dc in range(8):
            ba = 1 + 2 * (dc % 2); bb = ba + 1
            for kc in range(8):
                S.add('pe', lambda e, ba=ba, kc=kc, dc=dc: e.matmul(
                    PS[ba], lhsT=wco[:, kc, dc * 128:(dc + 1) * 128], rhs=U[:, kc, :],
                    start=(kc == 0), stop=(kc == 7)), r=[wcor, Ur], w=[PSr[ba]])
            for kc in range(8):
                S.add('pe', lambda e, bb=bb, kc=kc, dc=dc: e.matmul(
                    PS[bb], lhsT=wgc[:, kc, dc * 128:(dc + 1) * 128], rhs=HTG[:, kc, 0:512],
                    start=(kc == 0), stop=(kc == 7)), r=[wgcr, HTGr], w=[PSr[bb]])
            S.add('act', lambda e, bb=bb: e.activation(out=SG, in_=PS[bb], func=AF.Sigmoid), r=[PSr[bb]], w=[SGr])
            S.add('dve', lambda e, ba=ba, dc=dc: e.tensor_tensor(out=MC[:, dc, :], in0=PS[ba], in1=SG, op=ALU.mult),
                  r=[PSr[ba], SGr], w=[MCr])
        wao, waor = ring_load(w_attn_o[:, :])
        wga, wgar = ring_load(w_in[:, COL['ga']:COL['ga'] + 1024])
        for dc in range(8):
            ba = 1 + 2 * (dc % 2); bb = ba + 1
            for kc in range(8):
                S.add('pe', lambda e, ba=ba, kc=kc, dc=dc: e.matmul(
                    PS[ba], lhsT=wao[:, kc, dc * 128:(dc + 1) * 128], rhs=OT[:, kc, q0:q0 + 512],
                    start=(kc == 0), stop=(kc == 7)), r=[waor, OTr[g]], w=[PSr[ba]])
            for kc in range(8):
                S.add('pe', lambda e, bb=bb, kc=kc, dc=dc: e.matmul(
                    PS[bb], lhsT=wga[:, kc, dc * 128:(dc + 1) * 128], rhs=HTG[:, kc, 0:512],
                    start=(kc == 0), stop=(kc == 7)), r=[wgar, HTGr], w=[PSr[bb]])
            S.add('act', lambda e, bb=bb: e.activation(out=SG, in_=PS[bb], func=AF.Sigmoid), r=[PSr[bb]], w=[SGr])
            S.add('dve', lambda e, ba=ba: e.tensor_tensor(out=CT, in0=PS[ba], in1=SG, op=ALU.mult),
                  r=[PSr[ba], SGr], w=[p4])
            S.add('pool', lambda e, dc=dc: e.tensor_tensor(out=M[:, dc, :], in0=CT, in1=MC[:, dc, :], op=ALU.add),
                  r=[p4, MCr], w=[Mr])
        if DEBUG == 'p4' and g == 3:
            S.add('sp', lambda e: e.dma_start(out=dbg[:, 0, :], in_=ZP.rearrange("p a b -> p (a b)")), r=[ZPr], dma=misc)
            S.add('sp', lambda e: e.dma_start(out=dbg[:, 1, 0:4096], in_=MC.rearrange("p a b -> p (a b)")), r=[MCr], dma=misc)
            S.add('sp', lambda e: e.dma_start(out=dbg[:, 2, 0:4096], in_=M.rearrange("p a b -> p (a b)")), r=[Mr], dma=misc)
            S.add('sp', lambda e: e.dma_start(out=dbg[:, 3, :], in_=HTG.rearrange("p a b -> p (a b)")), r=[HTGr], dma=misc)
            S.wait_regs('sp', [misc])
            return finish(nc, S, stack)
        wo, wor = ring_load(w_out[:, :])
        for tt in range(4):
            tile_i = g * 4 + tt
            xt = XG[tt % 2]; xr = XGr[tt % 2]
            S.add('sp', lambda e, xt=xt, q0=q0, tt=tt: e.dma_start(
                out=xt, in_=xs[q0 + tt * 128:q0 + (tt + 1) * 128, :]), w=[xr], dma=xr)
            for half in range(2):
                bank = 5 + half
                for kc in range(8):
                    S.add('pe', lambda e, bank=bank, kc=kc, tt=tt, half=half: e.matmul(
                        PS[bank], lhsT=M[:, kc, tt * 128:(tt + 1) * 128], rhs=wo[:, kc, half * 512:(half + 1) * 512],
                        start=(kc == 0), stop=(kc == 7)), r=[Mr, wor], w=[PSr[bank]])
                S.add('dve', lambda e, bank=bank, half=half: e.tensor_tensor(
                    out=TMP2[:, half * 512:(half + 1) * 512], in0=PS[bank], in1=G1[:, half * 512:(half + 1) * 512],
                    op=ALU.mult), r=[PSr[bank], misc], w=[H2Tr])
            S.add('pool', lambda e, xt=xt: e.tensor_tensor(out=X1, in0=TMP2, in1=xt, op=ALU.add),
                  r=[H2Tr, xr], w=[X1r])
            x1dst = y if DEBUG == 'x1' else X1d
            S.add('sp', lambda e, tile_i=tile_i, x1dst=x1dst: e.dma_start(
                out=x1dst[tile_i * 128:(tile_i + 1) * 128, :], in_=X1), r=[X1r], dma=X1r)
            norm_tile(X1, X1r, W2, SH2, JUNK, SSQ, TMP, [H2, H2B], [H2r, H2Br], tag="n2")
            for kc in range(8):
                bank = 1 + kc // 4
                S.add('pe', lambda e, kc=kc, bank=bank: e.transpose(
                    out=PS[bank][:, (kc % 4) * 128:(kc % 4 + 1) * 128], in_=H2[:, kc * 128:(kc + 1) * 128],
                    identity=identF), r=[H2r, CONST], w=[PSr[bank]])
            for b2 in range(2):
                S.add('act', lambda e, b2=b2: e.copy(out=H2T32[:, b2 * 4:(b2 + 1) * 4, :].rearrange("p a b -> p (a b)"),
                                                     in_=PS[1 + b2]), r=[PSr[1 + b2]], w=[H2Tr])
            for kc in range(8):
                S.add('pe', lambda e, kc=kc: e.matmul(PS[3][:, 0:NE], lhsT=H2T32[:, kc, :], rhs=WRT[:, kc, :],
                                                      start=(kc == 0), stop=(kc == 7)),
                      r=[H2Tr, CONST], w=[PSr[3]])
            S.add('dve', lambda e: e.tensor_tensor(out=LG, in0=PS[3][:, 0:NE], in1=BR, op=ALU.add),
                  r=[PSr[3], CONST], w=[rr4])
            S.add('dve', lambda e: e.max(out=MX, in_=LG), r=[rr4], w=[rr4])
            S.add('dve', lambda e: e.tensor_scalar(out=MK, in0=LG, scalar1=MX[:, 3:4], scalar2=None, op0=ALU.is_ge),
                  r=[rr4], w=[rr4])
            if MOE_GATHER:
                S.add('sp', lambda e, tile_i=tile_i: e.dma_start(out=H2d[tile_i * 128:(tile_i + 1) * 128, :],
                                                                 in_=H2B), r=[H2Br], dma=H2Br)
            S.add('dve', lambda e: e.tensor_scalar(out=SM[:, 0:1], in0=MX[:, 0:1], scalar1=-1.0, scalar2=None,
                                                   op0=ALU.mult), r=[rr4], w=[rr4])
            S.add('act', lambda e: e.activation(out=EX, in_=LG, func=AF.Exp, bias=SM[:, 0:1]), r=[rr4], w=[rr4])
            S.add('dve', lambda e: e.tensor_tensor(out=EX, in0=EX, in1=MK, op=ALU.mult), r=[rr4], w=[rr4])
            S.add('dve', lambda e: e.tensor_reduce(out=SM[:, 1:2], in_=EX, axis=AX.X, op=ALU.add), r=[rr4], w=[rr4])
            S.add('dve', lambda e: e.reciprocal(out=SM[:, 2:3], in_=SM[:, 1:2]), r=[rr4], w=[rr4])
            S.add('dve', lambda e, tile_i=tile_i: e.tensor_scalar(out=WG[:, tile_i, :], in0=EX, scalar1=SM[:, 2:3],
                                                                   scalar2=None, op0=ALU.mult), r=[rr4], w=[WGr])
            for kc in range(8):
                S.add('pe', lambda e, kc=kc: e.transpose(out=PSB[7][:, kc * 128:(kc + 1) * 128],
                                                         in_=H2B[:, kc * 128:(kc + 1) * 128], identity=identB),
                      r=[H2Br, misc], w=[PSr[7]])
            S.add('act', lambda e, q0=q0, tt=tt: e.copy(
                out=OT[:, :, q0 + tt * 128:q0 + (tt + 1) * 128], in_=PSB[7].rearrange("p (a b) -> p a b", b=128)),
                r=[PSr[7]], w=[OTr[g]])

    if DEBUG == 'x1':
        barrier()
        S.add('sp', lambda e: e.dma_start(out=dbg.rearrange("(t p) e -> p t e", p=128), in_=WG), r=[WGr], dma=misc)
        S.wait_regs('sp', [misc])
        return finish(nc, S, stack)

    barrier()
    A.off = P5_MARK
    H2T = OT
    ACC = A.alloc(F32, 16, D); ACCr = [Reg("acc%d" % i) for i in range(16)]
    BGg = A.alloc(F32, 8, NE); BGu = A.alloc(F32, 8, NE)
    if MOE_GATHER:
        RK = A.alloc(F32, 16, NE); rkr = Reg("rk")
        IOTA = A.alloc(F32, CAP)
    H2TOK = OT.rearrange("p a b -> p (a b)").rearrange("p (t d) -> p t d", d=D)
    LOOP_MARK = A.off
    BGrow = A.alloc(F32, 2048)
    BD = A.alloc(F32, D)
    WGT = A.alloc(F32, 16, 128)
    p5 = Reg("p5")
    S.add('sp', lambda e: e.dma_start(out=BGrow[0:NE, :], in_=b_gate_up), w=[p5], dma=p5)
    S.add('sp', lambda e: e.dma_start(out=BD[0:NE, :], in_=b_down), dma=p5)
    p5.w = ('dma', p5, p5.dcnt)
    bg3 = BGrow.rearrange("p (f m t) -> p f m t", m=128, t=2)
    for fc in range(8):
        S.add('pe', lambda e, fc=fc: e.transpose(out=PS[0][:, fc * NE:(fc + 1) * NE],
                                                 in_=bg3[0:NE, fc, :, 0],
                                                 identity=identF[0:NE, 0:NE]), r=[p5, CONST], w=[PSr[0]])
        S.add('pe', lambda e, fc=fc: e.transpose(out=PS[0][:, 256 + fc * NE:256 + (fc + 1) * NE],
                                                 in_=bg3[0:NE, fc, :, 1],
                                                 identity=identF[0:NE, 0:NE]), r=[p5, CONST], w=[PSr[0]])
    bgr = Reg("bg")
    S.add('dve', lambda e: e.tensor_copy(out=BGg.rearrange("p a b -> p (a b)"), in_=PS[0][:, 0:256]),
          r=[PSr[0]], w=[bgr])
    S.add('dve', lambda e: e.tensor_scalar(out=BGu.rearrange("p a b -> p (a b)"), in0=PS[0][:, 256:512],
                                           scalar1=1.0, scalar2=None, op0=ALU.add), r=[PSr[0]], w=[bgr])
    for ti in range(16):
        S.add('pe', lambda e, ti=ti: e.transpose(out=PS[1][0:NE, 0:128], in_=WG[:, ti, :], identity=identF),
              r=[WGr, CONST], w=[PSr[1]])
        S.add('dve', lambda e, ti=ti: e.tensor_copy(out=WGT[0:NE, ti, :], in_=PS[1][0:NE, 0:128]),
              r=[PSr[1]], w=[bgr])
        for half in range(2):
            S.add('pe', lambda e, ti=ti, half=half: e.matmul(
                PS[2 + half], lhsT=WGT[0:NE, ti, :], rhs=BD[0:NE, half * 512:(half + 1) * 512],
                start=True, stop=True), r=[bgr, p5], w=[PSr[2 + half]])
            S.add('act', lambda e, ti=ti, half=half: e.copy(out=ACC[:, ti, half * 512:(half + 1) * 512],
                                                            in_=PS[2 + half]), r=[PSr[2 + half]], w=[ACCr[ti]])
    def moe_dense():
        barrier()
        A.off = LOOP_MARK
        RING5 = [A.alloc(BF, 8, 1024) for _ in range(4)]; RING5r = [Reg("r5_%d" % i) for i in range(4)]
        r5_i = [0]

        def ring5_load(src2d):
            i = r5_i[0] % 4; r5_i[0] += 1
            S.add('pool', lambda e, i=i: e.dma_start(out=RING5[i], in_=src2d.rearrange("(kc p) n -> p kc n", p=128)),
                  w=[RING5r[i]], dma=RING5r[i])
            return RING5[i], RING5r[i]

        AT = [A.alloc(BF, 8, 512)] * 2; ATr = [Reg("at0")] * 2
        GS = [A.alloc(F32, 512) for _ in range(2)]; GSr = [Reg("gs0"), Reg("gs1")]
        SGM = [A.alloc(BF, 512) for _ in range(2)]
        U1 = [A.alloc(F32, 512) for _ in range(2)]

        it5 = [0]
        for ex in range(NE):
            wgA, wgAr = ring5_load(w_gate_up[ex, :, 0:1024])
            wgB, wgBr = ring5_load(w_gate_up[ex, :, 1024:2048])
            wd, wdr = ring5_load(w_down[ex, :, :])
            for tg in range(4):
                at = AT[tg % 2]; atr = ATr[tg % 2]
                for fc in range(8):
                    wt, wr = (wgA, wgAr) if fc < 4 else (wgB, wgBr)
                    wv = wt.rearrange("p k (f m t) -> p k f m t", m=128, t=2)
                    k = it5[0] % 2; it5[0] += 1
                    bg_, bu_ = 4 * k, 4 * k + 1
                    for (bank, off) in ((bg_, 0), (bu_, 1)):
                        for kc in range(8):
                            S.add('pe', lambda e, bank=bank, off=off, wv=wv, kc=kc, fc=fc, tg=tg: e.matmul(
                                PS[bank], lhsT=wv[:, kc, fc % 4, :, off],
                                rhs=H2T[:, kc, tg * 512:(tg + 1) * 512], start=(kc == 0), stop=(kc == 7)),
                                r=[wr, OTr[tg]], w=[PSr[bank]])
                    gs = GS[k]; gsr = GSr[k]; sg = SGM[k]; u1 = U1[k]
                    S.add('dve', lambda e, gs=gs, bg_=bg_, fc=fc, ex=ex: e.tensor_scalar(
                        out=gs, in0=PS[bg_], scalar1=BGg[:, fc, ex:ex + 1], scalar2=7.0, op0=ALU.add, op1=ALU.min),
                        r=[PSr[bg_], bgr], w=[gsr])
                    S.add('act', lambda e, gs=gs, sg=sg: e.activation(out=sg, in_=gs, func=AF.Sigmoid, scale=1.702),
                          r=[gsr], w=[gsr])
                    S.add('dve', lambda e, u1=u1, bu_=bu_, fc=fc, ex=ex: e.tensor_scalar(
                        out=u1, in0=PS[bu_], scalar1=BGu[:, fc, ex:ex + 1], scalar2=8.0, op0=ALU.add, op1=ALU.min),
                        r=[PSr[bu_], bgr], w=[gsr])
                    S.add('dve', lambda e, gs=gs, sg=sg: e.tensor_tensor(out=gs, in0=gs, in1=sg, op=ALU.mult),
                          r=[gsr], w=[gsr])
                    S.add('dve', lambda e, u1=u1, gs=gs, at=at, fc=fc: e.scalar_tensor_tensor(
                        out=at[:, fc, :], in0=u1, scalar=-6.0, in1=gs, op0=ALU.max, op1=ALU.mult),
                        r=[gsr], w=[atr])
                for tt in range(4):
                    ti = tg * 4 + tt
                    for half in range(2):
                        bank = 2 + half
                        for fc in range(8):
                            S.add('pe', lambda e, bank=bank, fc=fc, at=at, tt=tt, half=half: e.matmul(
                                PS[bank], lhsT=at[:, fc, tt * 128:(tt + 1) * 128],
                                rhs=wd[:, fc, half * 512:(half + 1) * 512], start=(fc == 0), stop=(fc == 7)),
                                r=[atr, wdr], w=[PSr[bank]])
                        S.add('dve', lambda e, bank=bank, ti=ti, half=half, ex=ex: e.scalar_tensor_tensor(
                            out=ACC[:, ti, half * 512:(half + 1) * 512], in0=PS[bank], scalar=WG[:, ti, ex:ex + 1],
                            in1=ACC[:, ti, half * 512:(half + 1) * 512], op0=ALU.mult, op1=ALU.add),
                            r=[PSr[bank], WGr], w=[ACCr[ti]])


    if MOE_GATHER:
        TRIF = A.alloc(F32, 128); TRIB = A.alloc(BF, 128); ONESB = A.alloc(BF, 128)
        MKA = A.alloc(BF, 16, NE)
        S.add('dve', lambda e: e.tensor_scalar(out=MKA.rearrange("p a b -> p (a b)"),
                                               in0=WG.rearrange("p a b -> p (a b)"), scalar1=0.0, scalar2=None,
                                               op0=ALU.is_gt), r=[WGr], w=[bgr])
        S.add('sp', lambda e: e.dma_start(out=TRIF, in_=tri), w=[p5], dma=p5)
        S.add('sp', lambda e: e.dma_start(out=IOTA, in_=iota), w=[p5], dma=p5)
        S.add('sp', lambda e: e.dma_start(out=H2TOK, in_=H2d.rearrange("(t p) d -> p t d", p=128)),
              w=OTr + [p5], dma=p5)
        S.add('dve', lambda e: e.tensor_copy(out=TRIB, in_=TRIF), r=[p5], w=[bgr])
        S.add('pool', lambda e: e.memset(ONESB, 1.0), w=[bgr])
        for ti in range(16):
            b = 4 + ti % 2
            for tj in range(ti):
                S.add('pe', lambda e: e.matmul(PS[b][:, 0:NE], lhsT=ONESB, rhs=MKA[:, tj, :],
                                               start=(tj == 0), stop=False), r=[bgr, WGr], w=[PSr[b]])
            S.add('pe', lambda e: e.matmul(PS[b][:, 0:NE], lhsT=TRIB, rhs=MKA[:, ti, :],
                                           start=(ti == 0), stop=True), r=[bgr, WGr], w=[PSr[b]])
            S.add('dve', lambda e: e.scalar_tensor_tensor(out=RK[:, ti, :], in0=PS[b][:, 0:NE], scalar=1.0,
                                                          in1=MKA[:, ti, :], op0=ALU.add, op1=ALU.mult),
                  r=[PSr[b], WGr], w=[rkr])
            S.add('dve', lambda e: e.tensor_scalar(out=RK[:, ti, :], in0=RK[:, ti, :], scalar1=-1.0, scalar2=None,
                                                   op0=ALU.add), r=[rkr], w=[rkr])
        barrier()
        A.off = LOOP_MARK
        NR = 4
        RING5 = [A.alloc(BF, 8, 512) for _ in range(NR)]; RING5r = [Reg("r5_%d" % i) for i in range(NR)]
        r5_i = [0]

        def ring5_load(src2d):
            i = r5_i[0] % NR; r5_i[0] += 1
            S.add('pool', lambda e: e.dma_start(out=RING5[i], in_=src2d.rearrange("(kc p) n -> p kc n", p=128)),
                  w=[RING5r[i]], dma=RING5r[i])
            return RING5[i], RING5r[i]

        SEL = A.alloc(BF, 16, CAP); SELr = Reg("sel")
        XET = A.alloc(BF, 8, CAP); XEr = Reg("xet")
        YE = XET.rearrange("p a b -> p (a b)").rearrange("p (c d) -> p c d", d=D)
        AT = A.alloc(BF, 8, CAP); ATr = Reg("at")
        ST = [A.alloc(BF, CAP) for _ in range(2)]; STr = [Reg("st0"), Reg("st1")]
        GS = [A.alloc(F32, CAP) for _ in range(2)]; GSr = [Reg("gs0"), Reg("gs1")]
        SGM = [A.alloc(F32, CAP) for _ in range(2)]
        U1 = [A.alloc(F32, CAP) for _ in range(2)]
        NCC = CAP // 128
        for ex in range(NE):
            for ti in range(16):
                S.add('dve', lambda e: e.tensor_scalar(out=SEL[:, ti, :], in0=IOTA, scalar1=RK[:, ti, ex:ex + 1],
                                                       scalar2=None, op0=ALU.is_equal), r=[rkr, p5], w=[SELr])
            for fc in range(8):
                b = fc % 2
                for ti in range(16):
                    S.add('pe', lambda e: e.matmul(PS[b][:, 0:CAP], lhsT=H2TOK[:, ti, fc * 128:(fc + 1) * 128],
                                                   rhs=SEL[:, ti, :], start=(ti == 0), stop=(ti == 15)),
                          r=[p5, SELr], w=[PSr[b]])
                S.add('act', lambda e: e.copy(out=XET[:, fc, :], in_=PS[b][:, 0:CAP]), r=[PSr[b]], w=[XEr])
            for fc in range(8):
                if fc % 2 == 0:
                    wt, wr = ring5_load(w_gate_up[ex, :, fc * 256:fc * 256 + 512])
                    wv = wt.rearrange("p k (f m t) -> p k f m t", m=128, t=2)
                k = fc % 2
                bg_, bu_ = 2 + 2 * k, 3 + 2 * k
                for (bank, off) in ((bg_, 0), (bu_, 1)):
                    for kc in range(8):
                        S.add('pe', lambda e: e.matmul(PS[bank][:, 0:CAP], lhsT=wv[:, kc, fc % 2, :, off],
                                                       rhs=XET[:, kc, :], start=(kc == 0), stop=(kc == 7)),
                              r=[wr, XEr], w=[PSr[bank]])
                gs = GS[k]; gsr = GSr[k]; sg = SGM[k]; u1 = U1[k]
                S.add('dve', lambda e: e.tensor_scalar(out=gs, in0=PS[bg_][:, 0:CAP], scalar1=BGg[:, fc, ex:ex + 1],
                                                       scalar2=7.0, op0=ALU.add, op1=ALU.min),
                      r=[PSr[bg_], bgr], w=[gsr])
                S.add('act', lambda e: e.activation(out=sg, in_=gs, func=AF.Sigmoid, scale=1.702),
                      r=[gsr], w=[gsr])
                S.add('dve', lambda e: e.tensor_scalar(out=u1, in0=PS[bu_][:, 0:CAP], scalar1=BGu[:, fc, ex:ex + 1],
                                                       scalar2=8.0, op0=ALU.add, op1=ALU.min),
                      r=[PSr[bu_], bgr], w=[gsr])
                S.add('dve', lambda e: e.tensor_tensor(out=gs, in0=gs, in1=sg, op=ALU.mult), r=[gsr], w=[gsr])
                S.add('dve', lambda e: e.scalar_tensor_tensor(out=AT[:, fc, :], in0=u1, scalar=-6.0, in1=gs,
                                                              op0=ALU.max, op1=ALU.mult), r=[gsr], w=[ATr])
            for half in range(2):
                wd, wdr = ring5_load(w_down[ex, :, half * 512:(half + 1) * 512])
                for cc in range(NCC):
                    b = 6 + (half * NCC + cc) % 2
                    for fc in range(8):
                        S.add('pe', lambda e: e.matmul(PS[b], lhsT=AT[:, fc, cc * 128:(cc + 1) * 128],
                                                       rhs=wd[:, fc, :], start=(fc == 0), stop=(fc == 7)),
                              r=[ATr, wdr], w=[PSr[b]])
                    S.add('act', lambda e: e.copy(out=YE[:, cc, half * 512:(half + 1) * 512], in_=PS[b]),
                          r=[PSr[b]], w=[XEr])
            for ti in range(16):
                tb = ti % 2
                st = ST[tb]; str_ = STr[tb]
                for cc in range(NCC):
                    S.add('pe', lambda e: e.transpose(out=PSB[tb][:, cc * 128:(cc + 1) * 128],
                                                      in_=SEL[:, ti, cc * 128:(cc + 1) * 128], identity=identB),
                          r=[SELr, misc], w=[PSr[tb]])
                S.add('act', lambda e: e.copy(out=st, in_=PSB[tb][:, 0:CAP]), r=[PSr[tb]], w=[str_])
                for half in range(2):
                    bank = 2 + 2 * tb + half
                    for cc in range(NCC):
                        S.add('pe', lambda e: e.matmul(PS[bank], lhsT=st[:, cc * 128:(cc + 1) * 128],
                                                       rhs=YE[:, cc, half * 512:(half + 1) * 512],
                                                       start=(cc == 0), stop=(cc == NCC - 1)),
                              r=[str_, XEr], w=[PSr[bank]])
                    S.add('dve', lambda e: e.scalar_tensor_tensor(
                        out=ACC[:, ti, half * 512:(half + 1) * 512], in0=PS[bank], scalar=WG[:, ti, ex:ex + 1],
                        in1=ACC[:, ti, half * 512:(half + 1) * 512], op0=ALU.mult, op1=ALU.add),
                        r=[PSr[bank], WGr], w=[ACCr[ti]])
    else:
        moe_dense()

    barrier()
    A.off = LOOP_MARK
    XF = [A.alloc(F32, D) for _ in range(2)]; XFr = [Reg("xf0"), Reg("xf1")]
    for ti in range(16):
        xf = XF[ti % 2]; xfr = XFr[ti % 2]
        S.add('sp', lambda e, xf=xf, ti=ti: e.dma_start(out=xf, in_=X1d[ti * 128:(ti + 1) * 128, :]),
              w=[xfr], dma=xfr)
        S.add('dve', lambda e, ti=ti: e.tensor_tensor(out=ACC[:, ti, :], in0=ACC[:, ti, :], in1=G2, op=ALU.mult),
              r=[CONST, misc], w=[ACCr[ti]])
        S.add('pool', lambda e, ti=ti, xf=xf: e.tensor_tensor(out=xf, in0=ACC[:, ti, :], in1=xf, op=ALU.add),
              r=[ACCr[ti]], w=[xfr])
        S.add('sp', lambda e, xf=xf, ti=ti: e.dma_start(out=y[ti * 128:(ti + 1) * 128, :], in_=xf),
              r=[xfr], dma=xfr)
    S.wait_regs('sp', XFr)
    return finish(nc, S, stack)


def finish(nc, S, stack):
    S.prepare(nc, stack)
    with nc.Block() as block:
        @block.tensor
        def _(e):
            S.run('pe', e)

        @block.scalar
        def _(e):
            S.run('act', e)

        @block.vector
        def _(e):
            S.run('dve', e)

        @block.gpsimd
        def _(e):
            S.run('pool', e)

        @block.sync
        def _(e):
            S.run('sp', e)
    stack.close()
    return nc


def rope_table():
    inv = (np.float32(10000.0) ** (-(np.arange(0, 64, 2, dtype=np.float32) / np.float32(64)))).astype(np.float32)
    pos = np.arange(SEQ, dtype=np.float32)
    ang = (pos[:, None] * inv[None, :]).astype(np.float32)
    emb = np.concatenate([ang, ang], axis=-1)
    cos = np.cos(emb).astype(np.float32); sin = np.sin(emb).astype(np.float32)
    sin[:, 0:32] = -sin[:, 0:32]
    return np.concatenate([cos, sin], axis=1).astype(np.float32)


def make_in_maps(inp):
    x = np.asarray(inp['x'], dtype=np.float32)
    tab = rope_table()
    shared = {}
    for k in ('b_ada', 'norm1_w', 'q_norm_w', 'k_norm_w', 'lambda_q1', 'lambda_k1', 'lambda_q2', 'lambda_k2',
              'subln_w', 'norm2_w', 'b_router'):
        shared[k] = np.ascontiguousarray(np.asarray(inp[k], dtype=np.float32))
    for k in ('w_ada', 'w_in', 'w_attn_o', 'conv_w', 'w_conv_o', 'w_out', 'w_router', 'w_gate_up', 'b_gate_up',
              'w_down', 'b_down'):
        shared[k] = np.ascontiguousarray(np.asarray(inp[k], dtype=np.float32)[0])
    shared['ident'] = np.eye(128, dtype=np.float32)
    shared['tri'] = np.triu(np.ones((128, 128), np.float32), 1)
    shared['iota'] = np.ascontiguousarray(np.broadcast_to(np.arange(CAP, dtype=np.float32), (128, CAP)))
    maps = []
    for core in range(8):
        b = core // 4; t0 = (core % 4) * TOWN
        m = dict(shared)
        m['xs'] = np.ascontiguousarray(np.roll(x[b], -t0, axis=0))
        m['rope'] = np.ascontiguousarray(np.roll(tab, -t0, axis=0))
        xhal = np.zeros((2, D), np.float32); msk = np.zeros((128, 2), np.float32)
        if t0 > 0:
            xhal[0] = x[b, t0 - 1]; msk[:, 0] = 1.0
        if t0 + TOWN < SEQ:
            xhal[1] = x[b, t0 + TOWN]; msk[:, 1] = 1.0
        m['xh'] = xhal; m['hm'] = msk
        m['cvec'] = np.ascontiguousarray(np.asarray(inp['c'], dtype=np.float32)[b:b + 1])
        maps.append(m)
    return maps


_NC = None


def kernel(**inp):
    global _NC
    if _NC is None:
        _NC = build()
    maps = make_in_maps(inp)
    res = run_bass_kernel_spmd(_NC, maps, core_ids=list(range(8)))
    out = np.zeros((2, SEQ, D), np.float32)
    for core in range(8):
        b = core // 4; t0 = (core % 4) * TOWN
        out[b, t0:t0 + TOWN] = res.results[core]['y']
    return out
```

```python
import types
import numpy as np
from contextlib import ExitStack
import concourse.bass as bass
import concourse.mybir as mybir
from concourse.bass_utils import run_bass_kernel_spmd

F32 = mybir.dt.float32
BF = mybir.dt.bfloat16
ALU = mybir.AluOpType
AF = mybir.ActivationFunctionType
AX = mybir.AxisListType

SEQ = 8192
D = 1024
TOWN = 2048
NE = 32
ENGS = ['pe', 'act', 'dve', 'pool', 'sp']
DEBUG = None
MOE_GATHER = False
CAP = 384


class Reg:
    __slots__ = ('name', 'w', 'rs', 'dsem', 'dcnt', 'excl')
    ALL = []

    def __init__(s, name, excl=False):
        s.name = name; s.w = None; s.rs = {}; s.dsem = None; s.dcnt = 0; s.excl = excl
        Reg.ALL.append(s)


class Op:
    __slots__ = ('eng', 'fn', 'deps', 'needs_inc', 'sigval', 'dreg')


def freeze(fn):
    if fn is None or fn.__closure__ is None:
        return fn
    cells = []
    for c in fn.__closure__:
        try:
            cells.append(types.CellType(c.cell_contents))
        except ValueError:
            cells.append(c)
    return types.FunctionType(fn.__code__, fn.__globals__, fn.__name__, fn.__defaults__, tuple(cells))


class Sched:
    def __init__(s):
        s.q = {e: [] for e in ENGS}
        s.dregs = []

    def add(s, eng, fn, r=(), w=(), dma=None):
        op = Op(); op.eng = eng; op.fn = freeze(fn); op.needs_inc = False; op.sigval = 0; op.dreg = dma
        w = list(w) + [g for g in r if g.excl]
        r = [g for g in r if not g.excl]
        deps = []
        for g in r:
            if g.w is not None:
                deps.append(g.w)
        for g in w:
            if g.w is not None:
                deps.append(g.w)
            deps.extend(g.rs.values())
        op.deps = [d for d in deps if not (d[0] == 'op' and d[1].eng == eng and eng == 'pe')]
        if dma is not None:
            if dma.dcnt == 0 and dma not in s.dregs:
                s.dregs.append(dma)
            dma.dcnt += 16
            tok = ('dma', dma, dma.dcnt); key = ('d', id(dma))
        else:
            tok = ('op', op); key = eng
        for g in r:
            g.rs[key] = tok
        for g in w:
            g.w = tok; g.rs = {}
        s.q[eng].append(op)
        return op

    def wait_regs(s, eng, regs):
        op = Op(); op.eng = eng; op.fn = None; op.needs_inc = False; op.sigval = 0; op.dreg = None
        deps = []
        for g in regs:
            if g.w is not None:
                deps.append(g.w)
            deps.extend(g.rs.values())
        op.deps = deps
        s.q[eng].append(op)

    def barrier(s):
        deps = []
        for e in ENGS:
            for op in reversed(s.q[e]):
                if op.fn is not None and op.dreg is None:
                    deps.append(('op', op))
                    break
        for g in s.dregs:
            if g.dcnt > 0:
                deps.append(('dma', g, g.dcnt))
        for e in ENGS:
            op = Op(); op.eng = e; op.fn = None; op.needs_inc = False; op.sigval = 0; op.dreg = None
            op.deps = [d for d in deps if not (d[0] == 'op' and d[1].eng == e and e == 'pe')]
            s.q[e].append(op)
        for g in Reg.ALL:
            g.w = None; g.rs = {}

    def prepare(s, nc, stack):
        for e in ENGS:
            for op in s.q[e]:
                for d in op.deps:
                    if d[0] == 'op':
                        d[1].needs_inc = True
        for e in ENGS:
            c = 0
            for op in s.q[e]:
                if op.needs_inc:
                    c += 1
                    op.sigval = c
        s.esem = {e: stack.enter_context(nc.semaphore("sem_" + e)) for e in ENGS}
        for i, g in enumerate(s.dregs):
            g.dsem = stack.enter_context(nc.semaphore("dsem%d_%s" % (i, g.name)))

    def run(s, e, eng):
        known = {}
        for op in s.q[e]:
            for d in op.deps:
                if d[0] == 'op':
                    sem = s.esem[d[1].eng]; val = d[1].sigval; k = d[1].eng
                else:
                    sem = d[1].dsem; val = d[2]; k = id(d[1])
                if known.get(k, 0) < val:
                    eng.wait_ge(sem, val)
                    known[k] = val
            if op.fn is None:
                continue
            ins = op.fn(eng)
            if op.dreg is not None:
                ins.then_inc(op.dreg.dsem, 16)
            elif op.needs_inc:
                ins.then_inc(s.esem[e], 1)


class Arena:
    def __init__(s, t, nbytes):
        s.t = t; s.off = 0; s.cap = nbytes

    def alloc(s, dtype, *free):
        n = 1
        for f in free:
            n *= f
        esz = 2 if dtype == BF else 4
        nbytes = (n * esz + 63) // 64 * 64
        assert s.off + nbytes <= s.cap, ("arena overflow", s.off, nbytes, s.cap)
        a = s.t[:, s.off // 4:(s.off + nbytes) // 4]
        s.off += nbytes
        if dtype != F32:
            a = a.bitcast(dtype)
        a = a[:, 0:n]
        if len(free) == 2:
            a = a.rearrange("p (a b) -> p a b", b=free[1])
        elif len(free) == 3:
            a = a.rearrange("p (a b c) -> p a b c", b=free[1], c=free[2])
        return a


def build():
    nc = bass.Bass("TRN2", target_bir_lowering=False)

    def din(name, shape, dt=F32):
        return nc.dram_tensor(name, list(shape), dt, kind="ExternalInput").ap()

    xs = din("xs", [SEQ, D]); xh = din("xh", [2, D]); hm = din("hm", [128, 2])
    rope = din("rope", [SEQ, 128]); cvec = din("cvec", [1, D]); ident = din("ident", [128, 128])
    w_ada = din("w_ada", [D, 6 * D]); b_ada = din("b_ada", [1, 6 * D])
    norm1_w = din("norm1_w", [1, D]); w_in = din("w_in", [D, 8 * D])
    q_norm_w = din("q_norm_w", [1, 64]); k_norm_w = din("k_norm_w", [1, 64])
    lq1 = din("lambda_q1", [1, 64]); lk1 = din("lambda_k1", [1, 64])
    lq2 = din("lambda_q2", [1, 64]); lk2 = din("lambda_k2", [1, 64])
    subln_w = din("subln_w", [1, 128]); w_attn_o = din("w_attn_o", [D, D])
    conv_w = din("conv_w", [3, D]); w_conv_o = din("w_conv_o", [D, D]); w_out = din("w_out", [D, D])
    norm2_w = din("norm2_w", [1, D]); w_router = din("w_router", [D, NE]); b_router = din("b_router", [1, NE])
    w_gate_up = din("w_gate_up", [NE, D, 2 * D]); b_gate_up = din("b_gate_up", [NE, 2 * D])
    w_down = din("w_down", [NE, D, D]); b_down = din("b_down", [NE, D])
    y = nc.dram_tensor("y", [TOWN, D], F32, kind="ExternalOutput").ap()
    KTd = nc.dram_tensor("ktd", [128, 8, SEQ], BF, kind="Internal").ap()
    Vd = nc.dram_tensor("vd", [8, SEQ, 128], BF, kind="Internal").ap()
    X1d = nc.dram_tensor("x1d", [TOWN, D], F32, kind="Internal").ap()
    H2d = nc.dram_tensor("h2d", [TOWN, D], BF, kind="Internal").ap()
    tri = din("tri", [128, 128]); iota = din("iota", [128, CAP])
    dbg = None
    if DEBUG == 'attn':
        dbg = nc.dram_tensor("dbg", [128, 8 * TOWN], BF, kind="ExternalOutput").ap()
    elif DEBUG == 'x1':
        dbg = nc.dram_tensor("dbg", [TOWN, 32], F32, kind="ExternalOutput").ap()
    elif DEBUG == 'p4':
        dbg = nc.dram_tensor("dbg", [128, 4, 8 * 514], BF, kind="ExternalOutput").ap()

    S = Sched()
    stack = ExitStack()
    ARENA_BYTES = 188 * 1024
    arena_t = stack.enter_context(nc.sbuf_tensor("arena", [128, ARENA_BYTES // 4], F32))
    A = Arena(arena_t, ARENA_BYTES)
    PS = []
    PSr = []
    SXYs = []
    for i in range(2):
        sxy = stack.enter_context(nc.psum_tensor("sxy%d" % i, [128, 1024], F32))
        SXYs.append(sxy[:, :])
        PS.append(sxy[:, 0:512]); PS.append(sxy[:, 512:1024])
    for i in range(4, 8):
        t = stack.enter_context(nc.psum_tensor("ps%d" % i, [128, 512], F32))
        PS.append(t[:, :])
    for i in range(8):
        PSr.append(Reg("ps%d" % i, excl=True))
    PSB = [p.bitcast(BF) for p in PS]

    identF = A.alloc(F32, 128); identB = A.alloc(BF, 128)
    OT = A.alloc(BF, 8, TOWN)
    G2 = A.alloc(F32, D)
    WG = A.alloc(F32, 16, NE)
    P5_MARK = A.off
    SH1 = A.alloc(F32, D); G1 = A.alloc(F32, D); SH2 = A.alloc(F32, D)
    W1 = A.alloc(F32, D); W2 = A.alloc(F32, D)
    KNW = A.alloc(F32, 64); KNWS = A.alloc(F32, 64); QNW = A.alloc(F32, 64); QNWS = A.alloc(F32, 64)
    SUBW = A.alloc(F32, 128); NLAM = A.alloc(F32, 2)
    CW = A.alloc(F32, 3, 8); HM = A.alloc(F32, 2); BR = A.alloc(F32, NE); WRT = A.alloc(F32, 8, NE)
    PASS_MARK = A.off

    CONST = Reg("const")
    OTr = [Reg("ot%d" % g) for g in range(4)]
    WGr = Reg("wg")
    misc = Reg("misc")

    def cload(out, in_, **kw):
        S.add('sp', lambda e: e.dma_start(out=out, in_=in_, **kw), dma=CONST)

    def row_b(ap1d):
        return ap1d.partition_broadcast(128)

    SC1 = A.alloc(F32, D); SC2 = A.alloc(F32, D)
    cT = A.alloc(F32, 8); scT = A.alloc(F32, 8); scB = A.alloc(F32, 8, 128)
    LQ = [A.alloc(F32, 64) for _ in range(4)]
    LT = A.alloc(F32, 8)
    WA = [A.alloc(F32, 8, 512) for _ in range(2)]
    WAr = [Reg("wa0"), Reg("wa1")]

    cload(identF, ident)
    mod_tiles = [SH1, SC1, G1, SH2, SC2, G2]
    for i, t in enumerate(mod_tiles):
        cload(t, row_b(b_ada[0, i * D:(i + 1) * D]))
    cload(W1, row_b(norm1_w[0, :])); cload(W2, row_b(norm2_w[0, :]))
    cload(KNW, row_b(k_norm_w[0, :])); cload(QNW, row_b(q_norm_w[0, :]))
    cload(KNWS[:, 0:32], row_b(k_norm_w[0, 32:64])); cload(KNWS[:, 32:64], row_b(k_norm_w[0, 0:32]))
    cload(QNWS[:, 0:32], row_b(q_norm_w[0, 32:64])); cload(QNWS[:, 32:64], row_b(q_norm_w[0, 0:32]))
    cload(SUBW, row_b(subln_w[0, :]))
    for t, src in zip(LQ, (lq1, lk1, lq2, lk2)):
        cload(t, row_b(src[0, :]))
    cload(HM, hm); cload(BR, row_b(b_router[0, :]))
    cload(WRT, w_router.rearrange("(kc p) e -> p kc e", p=128))
    for k3 in range(3):
        cload(CW[:, k3, :], conv_w[k3, :].rearrange("(j p) -> p j", p=128), allow_slow_non_contiguous=True)
    cload(cT, cvec.rearrange("o (j p) -> p (o j)", p=128), allow_slow_non_contiguous=True)
    CONST.w = ('dma', CONST, CONST.dcnt)

    S.add('dve', lambda e: e.tensor_copy(out=identB, in_=identF), r=[CONST], w=[misc])
    S.add('pool', lambda e: e.memset(NLAM[:, 1:2], -0.5), w=[misc])
    S.add('act', lambda e: e.activation(out=scT, in_=cT, func=AF.Sigmoid), r=[CONST], w=[misc])
    S.add('dve', lambda e: e.tensor_tensor(out=scT, in0=scT, in1=cT, op=ALU.mult), r=[misc, CONST], w=[misc])
    S.add('dve', lambda e: e.tensor_copy(out=scB, in_=scT.unsqueeze(2).broadcast_to([128, 8, 128])),
          r=[misc], w=[misc])
    for i in range(2):
        S.add('dve', lambda e, i=i: e.tensor_tensor(out=LQ[2 * i], in0=LQ[2 * i], in1=LQ[2 * i + 1], op=ALU.mult),
              r=[CONST, misc], w=[misc])
        S.add('dve', lambda e, i=i: e.tensor_reduce(out=LT[:, i:i + 1], in_=LQ[2 * i], axis=AX.X, op=ALU.add),
              r=[misc], w=[misc])
    S.add('act', lambda e: e.activation(out=LT[:, 2:4], in_=LT[:, 0:2], func=AF.Exp), r=[misc], w=[misc])
    S.add('dve', lambda e: e.tensor_tensor(out=LT[:, 4:5], in0=LT[:, 2:3], in1=LT[:, 3:4], op=ALU.subtract),
          r=[misc], w=[misc])
    S.add('dve', lambda e: e.tensor_scalar(out=NLAM[:, 0:1], in0=LT[:, 4:5], scalar1=0.2, scalar2=-1.0,
                                           op0=ALU.add, op1=ALU.mult), r=[misc], w=[misc])
    S.add('dve', lambda e: e.tensor_scalar(out=SUBW, in0=SUBW, scalar1=0.8, scalar2=None, op0=ALU.mult),
          r=[CONST, misc], w=[misc])

    for cg in range(12):
        wa = WA[cg % 2]; war = WAr[cg % 2]
        S.add('sp', lambda e, wa=wa, cg=cg: e.dma_start(
            out=wa, in_=w_ada[:, cg * 512:(cg + 1) * 512].rearrange("(kc p) n -> p kc n", p=128)),
            w=[war], dma=war)
        bank = cg % 2
        for kc in range(8):
            S.add('pe', lambda e, wa=wa, kc=kc, bank=bank: e.matmul(
                PS[bank], lhsT=scB[:, kc, :], rhs=wa[:, kc, :], start=(kc == 0), stop=(kc == 7)),
                r=[misc, war], w=[PSr[bank]])
        dst = mod_tiles[cg // 2][:, (cg % 2) * 512:(cg % 2 + 1) * 512]
        S.add('dve', lambda e, dst=dst, bank=bank: e.tensor_tensor(out=dst, in0=PS[bank], in1=dst, op=ALU.add),
              r=[PSr[bank], CONST, misc], w=[misc])
    S.add('dve', lambda e: e.scalar_tensor_tensor(out=W1, in0=SC1, scalar=1.0, in1=W1, op0=ALU.add, op1=ALU.mult),
          r=[misc, CONST], w=[misc])
    S.add('dve', lambda e: e.scalar_tensor_tensor(out=W2, in0=SC2, scalar=1.0, in1=W2, op0=ALU.add, op1=ALU.mult),
          r=[misc, CONST], w=[misc])

    barrier = S.barrier

    nrm_r = Reg("nrm")

    def norm_tile(xt, xr, Wt, SHt, junk, ssq, tmp, outs, regs_out, npart=128, tag="n"):
        tr = nrm_r
        S.add('act', lambda e: e.activation(out=junk[0:npart], in_=xt[0:npart], func=AF.Square,
                                            accum_out=ssq[0:npart, 0:1]), r=[xr], w=[tr])
        S.add('act', lambda e: e.activation(out=ssq[0:npart, 1:2], in_=ssq[0:npart, 0:1], func=AF.Sqrt,
                                            scale=1.0 / D, bias=1e-6), r=[tr], w=[tr])
        S.add('dve', lambda e: e.reciprocal(out=ssq[0:npart, 2:3], in_=ssq[0:npart, 1:2]), r=[tr], w=[tr])
        S.add('dve', lambda e: e.scalar_tensor_tensor(out=tmp[0:npart], in0=xt[0:npart], scalar=ssq[0:npart, 2:3],
                                                      in1=Wt[0:npart], op0=ALU.mult, op1=ALU.mult),
              r=[tr, xr, misc], w=[tr])
        for o, ro in zip(outs, regs_out):
            S.add('dve', lambda e, o=o: e.tensor_tensor(out=o[0:npart], in0=tmp[0:npart], in1=SHt[0:npart],
                                                        op=ALU.add), r=[tr, misc, xr], w=[ro])

    barrier()
    A.off = PASS_MARK
    QT = A.alloc(BF, 8, TOWN); QTr = Reg("qt")
    WKV = A.alloc(BF, 8, 2048); WKVr = Reg("wkv")
    WQ = A.alloc(BF, 8, 1024); WQr = Reg("wq")
    XT = [A.alloc(F32, D) for _ in range(2)]; XTr = [Reg("xt0"), Reg("xt1")]
    RT = [A.alloc(F32, 128) for _ in range(2)]; RTr = [Reg("rt0"), Reg("rt1")]
    Hb2 = [A.alloc(BF, D) for _ in range(2)]; Hr2 = [Reg("h0"), Reg("h1")]
    HT2 = [A.alloc(BF, 8, 128) for _ in range(2)]; HTr2 = [Reg("ht0"), Reg("ht1")]
    JUNK = A.alloc(BF, D)
    SSQ = A.alloc(F32, 4)
    SQ = A.alloc(F32, D)
    T1 = A.alloc(F32, D); T2 = A.alloc(F32, D)
    S16 = A.alloc(F32, 48)
    AB = A.alloc(F32, 256)
    KB2 = [A.alloc(BF, D) for _ in range(2)]; KBr2 = [Reg("kb0"), Reg("kb1")]
    QB = A.alloc(BF, D); QBr = Reg("qb")
    KTt = [A.alloc(BF, 8, 128) for _ in range(2)]; KTtr = [Reg("ktt0"), Reg("ktt1")]
    Vt = [A.alloc(BF, D) for _ in range(2)]; Vtr = [Reg("vt0"), Reg("vt1")]
    p1r = Reg("p1")

    for half in range(2):
        S.add('pool', lambda e, half=half: e.dma_start(
            out=WKV[:, :, half * 1024:(half + 1) * 1024],
            in_=w_in[:, 1024 + half * 1024:2048 + half * 1024].rearrange("(kc p) n -> p kc n", p=128)),
            dma=WKVr)
    WKVr.w = ('dma', WKVr, WKVr.dcnt)
    S.add('pool', lambda e: e.dma_start(out=WQ, in_=w_in[:, 0:1024].rearrange("(kc p) n -> p kc n", p=128)),
          w=[WQr], dma=WQr)

    kpost_r = Reg("kpost")

    def qk_post(banks, A_, B_, outbf, outr, tag):
        tr = kpost_r
        for hh in range(2):
            S.add('act', lambda e, hh=hh: e.activation(out=SQ[:, hh * 512:(hh + 1) * 512], in_=PS[banks[hh]],
                                                       func=AF.Square), r=[PSr[banks[hh]]], w=[tr])
        S.add('dve', lambda e: e.tensor_reduce(out=S16[:, 0:16], in_=SQ.rearrange("p (g d) -> p g d", d=64),
                                               axis=AX.X, op=ALU.add), r=[tr], w=[tr])
        S.add('act', lambda e: e.activation(out=S16[:, 16:32], in_=S16[:, 0:16], func=AF.Sqrt,
                                            scale=1.0 / 64, bias=1e-6), r=[tr], w=[tr])
        S.add('dve', lambda e: e.reciprocal(out=S16[:, 32:48], in_=S16[:, 16:32]), r=[tr], w=[tr])
        for hh in range(2):
            pv = PS[banks[hh]].rearrange("p (g d) -> p g d", d=64)
            t1 = T1[:, hh * 512:(hh + 1) * 512].rearrange("p (g d) -> p g d", d=64)
            t2 = T2[:, hh * 512:(hh + 1) * 512].rearrange("p (g d) -> p g d", d=64)
            S.add('dve', lambda e, pv=pv, t1=t1: e.tensor_tensor(
                out=t1, in0=pv, in1=A_.unsqueeze(1).broadcast_to([128, 8, 64]), op=ALU.mult),
                r=[PSr[banks[hh]], p1r], w=[tr])
            S.add('dve', lambda e, pv=pv, t2=t2: e.tensor_tensor(
                out=t2[:, :, 0:32], in0=pv[:, :, 32:64], in1=B_[:, 0:32].unsqueeze(1).broadcast_to([128, 8, 32]),
                op=ALU.mult), r=[PSr[banks[hh]], p1r], w=[tr])
            S.add('dve', lambda e, pv=pv, t2=t2: e.tensor_tensor(
                out=t2[:, :, 32:64], in0=pv[:, :, 0:32], in1=B_[:, 32:64].unsqueeze(1).broadcast_to([128, 8, 32]),
                op=ALU.mult), r=[PSr[banks[hh]], p1r], w=[tr])
        S.add('pool', lambda e: e.tensor_tensor(out=T1, in0=T1, in1=T2, op=ALU.add), r=[tr], w=[tr])
        S.add('dve', lambda e: e.tensor_tensor(
            out=outbf.rearrange("p (g d) -> p g d", d=64), in0=T1.rearrange("p (g d) -> p g d", d=64),
            in1=S16[:, 32:48].unsqueeze(2).broadcast_to([128, 16, 64]), op=ALU.mult), r=[tr], w=[outr])

    NT = SEQ // 128
    NOWN = TOWN // 128

    def kbanks(j):
        return [1, 2] if (j < NOWN or j % 2 == 0) else [6, 7]

    def st_N(j):
        xt = XT[j % 2]; xr = XTr[j % 2]; rt = RT[j % 2]; rr = RTr[j % 2]
        S.add('sp', lambda e: e.dma_start(out=xt, in_=xs[j * 128:(j + 1) * 128, :]), w=[xr], dma=xr)
        S.add('sp', lambda e: e.dma_start(out=rt, in_=rope[j * 128:(j + 1) * 128, :]), w=[rr], dma=rr)
        norm_tile(xt, xr, W1, SH1, JUNK, SSQ, xt, [Hb2[j % 2]], [Hr2[j % 2]], tag="n1")

    def st_T(j):
        hb = Hb2[j % 2]; hr = Hr2[j % 2]; ht = HT2[j % 2]; htr = HTr2[j % 2]
        for kc in range(8):
            S.add('pe', lambda e: e.transpose(out=PSB[0][:, kc * 128:(kc + 1) * 128],
                                              in_=hb[:, kc * 128:(kc + 1) * 128], identity=identB),
                  r=[hr, misc], w=[PSr[0]])
        S.add('act', lambda e: e.copy(out=ht.rearrange("p a b -> p (a b)"), in_=PSB[0]), r=[PSr[0]], w=[htr])

    def st_M(j):
        ht = HT2[j % 2]; htr = HTr2[j % 2]
        kb_ = kbanks(j)
        banks = [kb_[0], kb_[1], 3, 4]
        for n in range(4):
            for kc in range(8):
                S.add('pe', lambda e: e.matmul(
                    PS[banks[n]], lhsT=ht[:, kc, :], rhs=WKV[:, kc, n * 512:(n + 1) * 512],
                    start=(kc == 0), stop=(kc == 7)), r=[htr, WKVr], w=[PSr[banks[n]]])
        if j < NOWN:
            for n in range(2):
                for kc in range(8):
                    S.add('pe', lambda e: e.matmul(
                        PS[6 + n], lhsT=ht[:, kc, :], rhs=WQ[:, kc, n * 512:(n + 1) * 512],
                        start=(kc == 0), stop=(kc == 7)), r=[htr, WQr], w=[PSr[6 + n]])

    def st_E(j):
        own = j < NOWN
        rt = RT[j % 2]; rr = RTr[j % 2]
        vt = Vt[j % 2]; vr = Vtr[j % 2]
        for n in range(2):
            S.add('act', lambda e: e.copy(out=vt[:, n * 512:(n + 1) * 512], in_=PS[3 + n]),
                  r=[PSr[3 + n]], w=[vr])
        S.add('sp', lambda e: e.dma_start(
            out=Vd[:, j * 128:(j + 1) * 128, :].rearrange("h t d -> t h d"),
            in_=vt.rearrange("p (h d) -> p h d", d=128)), r=[vr], dma=vr)
        S.add('dve', lambda e: e.tensor_tensor(out=AB[:, 0:64], in0=rt[:, 0:64], in1=KNW, op=ALU.mult),
              r=[rr, CONST], w=[p1r])
        S.add('dve', lambda e: e.tensor_tensor(out=AB[:, 64:128], in0=rt[:, 64:128], in1=KNWS, op=ALU.mult),
              r=[rr, CONST], w=[p1r])
        if own:
            S.add('dve', lambda e: e.tensor_tensor(out=AB[:, 128:192], in0=rt[:, 0:64], in1=QNW, op=ALU.mult),
                  r=[rr, CONST], w=[p1r])
            S.add('dve', lambda e: e.tensor_tensor(out=AB[:, 192:256], in0=rt[:, 64:128], in1=QNWS,
                                                   op=ALU.mult), r=[rr, CONST], w=[p1r])
        qk_post(kbanks(j), AB[:, 0:64], AB[:, 64:128], KB2[j % 2], KBr2[j % 2], "kpost")
        if own:
            qk_post([6, 7], AB[:, 128:192], AB[:, 192:256], QB, QBr, "qpost")

    def st_K(j):
        own = j < NOWN
        kb = KB2[j % 2]; kbr = KBr2[j % 2]
        ktt = KTt[j % 2]; ktr = KTtr[j % 2]
        for h in range(8):
            S.add('pe', lambda e: e.transpose(out=PSB[5][:, h * 128:(h + 1) * 128],
                                              in_=kb[:, h * 128:(h + 1) * 128], identity=identB),
                  r=[kbr, misc], w=[PSr[5]])
        S.add('act', lambda e: e.copy(out=ktt.rearrange("p a b -> p (a b)"), in_=PSB[5]),
              r=[PSr[5]], w=[ktr])
        S.add('sp', lambda e: e.dma_start(out=KTd[:, :, j * 128:(j + 1) * 128], in_=ktt),
              r=[ktr], dma=ktr)
        if own:
            qb_ = QB; qbr = QBr
            for h in range(8):
                S.add('pe', lambda e: e.transpose(out=PSB[5][:, h * 128:(h + 1) * 128],
                                                  in_=qb_[:, h * 128:(h + 1) * 128], identity=identB),
                      r=[qbr, misc], w=[PSr[5]])
            S.add('act', lambda e: e.copy(out=QT[:, :, j * 128:(j + 1) * 128],
                                          in_=PSB[5].rearrange("p (a b) -> p a b", b=128)),
                  r=[PSr[5]], w=[QTr])

    st_N(0); st_N(1); st_T(0); st_M(0)
    for j in range(NT):
        if j + 1 < NT:
            st_T(j + 1)
        st_E(j)
        if j + 1 < NT:
            st_M(j + 1)
        if j + 2 < NT:
            st_N(j + 2)
        st_K(j)

    barrier()
    A.off = PASS_MARK + 2 * 8 * TOWN
    KTb = [A.alloc(BF, SEQ) for _ in range(2)]; KTbr = [Reg("ktb0"), Reg("ktb1")]
    Vb = [A.alloc(BF, 64, 130) for _ in range(2)]; Vbr = [Reg("vb0"), Reg("vb1")]
    PT = [A.alloc(BF, 512) for _ in range(3)]; PTr = [Reg("pt%d" % i) for i in range(3)]
    RC = A.alloc(F32, 8)
    OS = [A.alloc(F32, 128) for _ in range(2)]
    ONB = [A.alloc(BF, 128) for _ in range(2)]
    JKF2 = [A.alloc(F32, 128) for _ in range(2)]
    ppr = [Reg("pp0"), Reg("pp1")]
    Sr = [Reg("s0", excl=True), Reg("s1", excl=True)]
    for b in range(2):
        S.add('pool', lambda e, b=b: e.memset(Vb[b][:, :, 128:129], 1.0), w=[Vbr[b]])

    for h in range(8):
        kt = KTb[h % 2]; ktr = KTbr[h % 2]; vb = Vb[h % 2]; vbr = Vbr[h % 2]
        S.add('sp', lambda e, kt=kt, h=h: e.dma_start(out=kt, in_=KTd[:, h, :]), w=[ktr], dma=ktr)
        S.add('sp', lambda e, vb=vb, h=h: e.dma_start(
            out=vb[:, :, 0:128], in_=Vd[h].rearrange("(kc p) d -> p kc d", p=128)), w=[vbr], dma=vbr)
        def emit_qk(i, kt=kt, ktr=ktr, h=h):
            qt, kc = divmod(i, 64); q0 = qt * 256; sb = i % 2
            for c in range(2):
                S.add('pe', lambda e: e.matmul(
                    PS[2 * sb + c][:, 0:256], lhsT=kt[c * 64:(c + 1) * 64, kc * 128:(kc + 1) * 128],
                    rhs=QT[c * 64:(c + 1) * 64, h, q0:q0 + 256], start=True, stop=True),
                    r=[ktr, QTr], w=[Sr[sb]])

        emit_qk(0)
        emit_qk(1)
        pending = []
        for it in range(512):
            qt, kc = divmod(it, 64); q0 = qt * 256
            sb = it % 2; pt = PT[it % 3]; ptr = PTr[it % 3]
            S.add('act', lambda e: e.activation(
                out=pt.rearrange("p (c n) -> p c n", n=256),
                in_=SXYs[sb].rearrange("p (c n) -> p c n", n=512)[:, :, 0:256],
                func=AF.Exp, scale=0.125), r=[Sr[sb]], w=[ptr])
            if it + 2 < 512:
                emit_qk(it + 2)
            for qb in range(2):
                for c in range(2):
                    bank = 4 + qb
                    S.add('pe', lambda e: e.matmul(
                        PS[bank][:, c * 256:c * 256 + 129], lhsT=pt[:, c * 256 + qb * 128:c * 256 + qb * 128 + 128],
                        rhs=vb[:, kc, 0:129], start=(kc == 0 and c == 0), stop=(kc == 63),
                        skip_group_check=True),
                        r=[ptr, vbr], w=[PSr[bank]])
            if pending and (kc == 12 or it == 511):
                for fn_ in pending:
                    fn_()
                pending = []
            if kc != 63:
                continue
            for qb in range(2):
                bk = 4 + qb
                pr = ppr[qb]; os_ = OS[qb]; onb = ONB[qb]
                rc = RC[:, qb * 4:(qb + 1) * 4]
                S.add('dve', lambda e, bk=bk, rc=rc: e.reciprocal(out=rc[:, 0:1], in_=PS[bk][:, 128:129]),
                      r=[PSr[bk]], w=[pr])
                S.add('dve', lambda e, bk=bk, rc=rc: e.reciprocal(out=rc[:, 1:2], in_=PS[bk][:, 384:385]),
                      r=[PSr[bk]], w=[pr])
                S.add('dve', lambda e, rc=rc: e.tensor_tensor(out=rc[:, 1:2], in0=rc[:, 1:2], in1=NLAM[:, 0:1],
                                                              op=ALU.mult), r=[pr, misc], w=[pr])
                S.add('dve', lambda e, bk=bk, rc=rc, os_=os_: e.tensor_scalar(
                    out=os_, in0=PS[bk][:, 0:128], scalar1=rc[:, 0:1], scalar2=None, op0=ALU.mult),
                    r=[PSr[bk], pr], w=[pr])
                S.add('dve', lambda e, bk=bk, rc=rc, os_=os_: e.scalar_tensor_tensor(
                    out=os_, in0=PS[bk][:, 256:384], scalar=rc[:, 1:2], in1=os_, op0=ALU.mult, op1=ALU.add),
                    r=[PSr[bk], pr], w=[pr])
            for qb in range(2):
                bk = 4 + qb
                pr = ppr[qb]; os_ = OS[qb]; onb = ONB[qb]
                rc = RC[:, qb * 4:(qb + 1) * 4]
                S.add('dve', lambda e, os_=os_: e.tensor_tensor(out=JKF2[qb], in0=os_, in1=os_, op=ALU.mult),
                      r=[pr], w=[pr])
                S.add('dve', lambda e, rc=rc: e.tensor_reduce(out=rc[:, 2:3], in_=JKF2[qb], axis=AX.X, op=ALU.add),
                      r=[pr], w=[pr])
                S.add('dve', lambda e, rc=rc: e.tensor_scalar(out=rc[:, 2:3], in0=rc[:, 2:3], scalar1=1.0 / 128,
                                                              scalar2=1e-5, op0=ALU.mult, op1=ALU.add),
                      r=[pr], w=[pr])
                S.add('pool', lambda e, rc=rc: e.tensor_tensor(out=rc[:, 3:4], in0=rc[:, 2:3],
                                                               in1=NLAM[:, 1:2], op=ALU.pow), r=[pr, misc], w=[pr])
                S.add('dve', lambda e, os_=os_, rc=rc, onb=onb: e.scalar_tensor_tensor(
                    out=onb, in0=os_, scalar=rc[:, 3:4], in1=SUBW, op0=ALU.mult, op1=ALU.mult),
                    r=[pr, misc], w=[pr])
                def fin_post(onb=onb, pr=pr, h=h, q0=q0, qb=qb):
                    S.add('pe', lambda e: e.transpose(out=PSB[7][:, 0:128], in_=onb, identity=identB),
                          r=[pr, misc], w=[PSr[7]])
                    g = (q0 + qb * 128) // 512
                    S.add('dve', lambda e: e.tensor_copy(
                        out=OT[:, h, q0 + qb * 128:q0 + qb * 128 + 128], in_=PSB[7][:, 0:128]),
                        r=[PSr[7]], w=[OTr[g]])
                pending.append(fin_post)
            if it == 511:
                for fn_ in pending:
                    fn_()
                pending = []

    if DEBUG == 'attn':
        barrier()
        S.add('sp', lambda e: e.dma_start(out=dbg, in_=OT.rearrange("p a b -> p (a b)")), r=OTr, dma=misc)
        S.wait_regs('sp', [misc])
        return finish(nc, S, stack)

    barrier()
    A.off = PASS_MARK
    RING = [A.alloc(BF, 8, 1024) for _ in range(3)]; RINGr = [Reg("ring%d" % i) for i in range(3)]
    ring_i = [0]

    def ring_load(src2d):
        i = ring_i[0] % len(RING); ring_i[0] += 1
        S.add('pool', lambda e, i=i: e.dma_start(out=RING[i], in_=src2d.rearrange("(kc p) n -> p kc n", p=128)),
              w=[RINGr[i]], dma=RINGr[i])
        return RING[i], RINGr[i]

    XG = [A.alloc(F32, D) for _ in range(2)]; XGr = [Reg("xg0"), Reg("xg1")]
    Hb = A.alloc(BF, D); Hr = Reg("h4")
    HTG = A.alloc(BF, 8, 514); HTGr = Reg("htg")
    JUNK = A.alloc(BF, D); SSQ = A.alloc(F32, 4); TMP = A.alloc(F32, D)
    CCs = A.alloc(F32, 512); CCr = Reg("ccs")
    ZP = A.alloc(BF, 8, 514); ZPr = Reg("zp")
    ZH = A.alloc(F32, 32)
    CT = A.alloc(F32, 512)
    U = A.alloc(BF, 8, 512); Ur = Reg("u")
    SG = A.alloc(BF, 512); SGr = Reg("sg")
    MC = A.alloc(BF, 8, 512); MCr = Reg("mc")
    M = U; Mr = Ur
    TMP2 = A.alloc(F32, D)
    X1 = A.alloc(F32, D); X1r = Reg("x1")
    H2 = A.alloc(F32, D); H2r = Reg("h2")
    H2B = A.alloc(BF, D); H2Br = Reg("h2b")
    H2T32 = TMP2.rearrange("p (a b) -> p a b", b=128); H2Tr = Reg("h2t32")
    LG = A.alloc(F32, NE); MX = A.alloc(F32, 8); EX = A.alloc(F32, NE); MK = A.alloc(F32, NE)
    SM = A.alloc(F32, 4)
    rr4 = Reg("r4")
    p4 = Reg("p4")

    COL = dict(cb=3072, cc=4096, cx=5120, ga=6144, gc=7168)
    for g in range(4):
        q0 = g * 512
        for tt in range(5):
            xt = XG[tt % 2]; xr = XGr[tt % 2]
            if tt < 4:
                npart = 128
                S.add('sp', lambda e, xt=xt, q0=q0, tt=tt: e.dma_start(
                    out=xt, in_=xs[q0 + tt * 128:q0 + (tt + 1) * 128, :]), w=[xr], dma=xr)
            else:
                npart = 2
                S.add('sp', lambda e, xt=xt, g=g, q0=q0: e.dma_start(
                    out=xt[0:1, :], in_=(xs[q0 - 1:q0, :] if g > 0 else xh[0:1, :])), w=[xr], dma=xr)
                S.add('sp', lambda e, xt=xt, g=g, q0=q0: e.dma_start(
                    out=xt[1:2, :], in_=(xs[q0 + 512:q0 + 513, :] if g < 3 else xh[1:2, :])), w=[xr], dma=xr)
            norm_tile(xt, xr, W1, SH1, JUNK, SSQ, TMP, [Hb], [Hr], npart=npart, tag="n4")
            for kc in range(8):
                S.add('pe', lambda e, kc=kc, npart=npart: e.transpose(
                    out=PSB[0][:, kc * 128:kc * 128 + npart], in_=Hb[0:npart, kc * 128:(kc + 1) * 128],
                    identity=identB[0:npart, 0:npart]), r=[Hr, misc], w=[PSr[0]])
            c0 = tt * 128
            S.add('act', lambda e, c0=c0, npart=npart: e.copy(
                out=HTG[:, :, c0:c0 + npart], in_=PSB[0].rearrange("p (a b) -> p a b", b=128)[:, :, 0:npart]),
                r=[PSr[0]], w=[HTGr])
        wcc, wccr = ring_load(w_in[:, COL['cc']:COL['cc'] + 1024])
        wcx, wcxr = ring_load(w_in[:, COL['cx']:COL['cx'] + 1024])
        for fc in range(8):
            for (wt, wr, bank) in ((wcc, wccr, 1), (wcx, wcxr, 2)):
                for kc in range(8):
                    S.add('pe', lambda e, wt=wt, bank=bank, kc=kc, fc=fc: e.matmul(
                        PS[bank], lhsT=wt[:, kc, fc * 128:(fc + 1) * 128], rhs=HTG[:, kc, 0:512],
                        start=(kc == 0), stop=(kc == 7)), r=[wr, HTGr], w=[PSr[bank]])
            for wi, (wt, wr) in enumerate(((wcc, wccr), (wcx, wcxr))):
                for kc in range(8):
                    S.add('pe', lambda e, wt=wt, kc=kc, fc=fc, wi=wi: e.matmul(
                        PS[3][:, (fc * 2 + wi) * 2:(fc * 2 + wi) * 2 + 2], lhsT=wt[:, kc, fc * 128:(fc + 1) * 128],
                        rhs=HTG[:, kc, 512:514], start=(kc == 0), stop=(kc == 7)), r=[wr, HTGr], w=[PSr[3]])
            S.add('act', lambda e: e.copy(out=CCs, in_=PS[1]), r=[PSr[1]], w=[CCr])
            S.add('dve', lambda e, fc=fc: e.tensor_tensor(out=ZP[:, fc, 1:513], in0=PS[2], in1=CCs, op=ALU.mult),
                  r=[PSr[2], CCr], w=[ZPr])
        S.add('act', lambda e: e.copy(out=ZH, in_=PS[3][:, 0:32]), r=[PSr[3]], w=[p4])
        zh4 = ZH.rearrange("p (f w c) -> p f w c", w=2, c=2)
        for ci, col in ((0, 0), (1, 513)):
            S.add('dve', lambda e, ci=ci, col=col: e.tensor_tensor(
                out=ZP[:, :, col:col + 1], in0=zh4[:, :, 0, ci:ci + 1], in1=zh4[:, :, 1, ci:ci + 1], op=ALU.mult),
                r=[p4], w=[ZPr])
        if g == 0:
            S.add('dve', lambda e: e.tensor_scalar(out=ZP[:, :, 0:1], in0=ZP[:, :, 0:1], scalar1=HM[:, 0:1],
                                                   scalar2=None, op0=ALU.mult), r=[CONST], w=[ZPr])
        if g == 3:
            S.add('dve', lambda e: e.tensor_scalar(out=ZP[:, :, 513:514], in0=ZP[:, :, 513:514],
                                                   scalar1=HM[:, 1:2], scalar2=None, op0=ALU.mult),
                  r=[CONST], w=[ZPr])
        wcb, wcbr = ring_load(w_in[:, COL['cb']:COL['cb'] + 1024])
        for fc in range(8):
            bank = 1 + fc % 2
            for kc in range(8):
                S.add('pe', lambda e, bank=bank, kc=kc, fc=fc: e.matmul(
                    PS[bank], lhsT=wcb[:, kc, fc * 128:(fc + 1) * 128], rhs=HTG[:, kc, 0:512],
                    start=(kc == 0), stop=(kc == 7)), r=[wcbr, HTGr], w=[PSr[bank]])
            S.add('dve', lambda e, fc=fc: e.tensor_scalar(out=CT, in0=ZP[:, fc, 0:512], scalar1=CW[:, 0, fc:fc + 1],
                                                          scalar2=None, op0=ALU.mult), r=[ZPr, CONST], w=[p4])
            S.add('dve', lambda e, fc=fc: e.scalar_tensor_tensor(out=CT, in0=ZP[:, fc, 1:513], scalar=CW[:, 1, fc:fc + 1],
                                                                 in1=CT, op0=ALU.mult, op1=ALU.add),
                  r=[ZPr, CONST, p4], w=[p4])
            S.add('dve', lambda e, fc=fc: e.scalar_tensor_tensor(out=CT, in0=ZP[:, fc, 2:514], scalar=CW[:, 2, fc:fc + 1],
                                                                 in1=CT, op0=ALU.mult, op1=ALU.add),
                  r=[ZPr, CONST, p4], w=[p4])
            S.add('dve', lambda e, fc=fc, bank=bank: e.tensor_tensor(out=U[:, fc, :], in0=PS[bank], in1=CT,
                                                                      op=ALU.mult), r=[PSr[bank], p4], w=[Ur])
        wco, wcor = ring_load(w_conv_o[:, :])
        wgc, wgcr = ring_load(w_in[:, COL['gc']:COL['gc'] + 1024])
        for dc in range(8):
            ba = 1 + 2 * (dc % 2); bb = ba + 1
            for kc in range(8):
                S.add('pe', lambda e, ba=ba, kc=kc, dc=dc: e.matmul(
                    PS[ba], lhsT=wco[:, kc, dc * 128:(dc + 1) * 128], rhs=U[:, kc, :],
                    start=(kc == 0), stop=(kc == 7)), r=[wcor, Ur], w=[PSr[ba]])
            for kc in range(8):
                S.add('pe', lambda e, bb=bb, kc=kc, dc=dc: e.matmul(
                    PS[bb], lhsT=wgc[:, kc, dc * 128:(dc + 1) * 128], rhs=HTG[:, kc, 0:512],
                    start=(kc == 0), stop=(kc == 7)), r=[wgcr, HTGr], w=[PSr[bb]])
            S.add('act', lambda e, bb=bb: e.activation(out=SG, in_=PS[bb], func=AF.Sigmoid), r=[PSr[bb]], w=[SGr])
            S.add('dve', lambda e, ba=ba, dc=dc: e.tensor_tensor(out=MC[:, dc, :], in0=PS[ba], in1=SG, op=ALU.mult),
                  r=[PSr[ba], SGr], w=[MCr])
        wao, waor = ring_load(w_attn_o[:, :])
        wga, wgar = ring_load(w_in[:, COL['ga']:COL['ga'] + 1024])
        for dc in range(8):
            ba = 1 + 2 * (dc % 2); bb = ba + 1
            for kc in range(8):
                S.add('pe', lambda e, ba=ba, kc=kc, dc=dc: e.matmul(
                    PS[ba], lhsT=wao[:, kc, dc * 128:(dc + 1) * 128], rhs=OT[:, kc, q0:q0 + 512],
                    start=(kc == 0), stop=(kc == 7)), r=[waor, OTr[g]], w=[PSr[ba]])
            for kc in range(8):
                S.add('pe', lambda e, bb=bb, kc=kc, dc=dc: e.matmul(
                    PS[bb], lhsT=wga[:, kc, dc * 128:(dc + 1) * 128], rhs=HTG[:, kc, 0:512],
                    start=(kc == 0), stop=(kc == 7)), r=[wgar, HTGr], w=[PSr[bb]])
            S.add('act', lambda e, bb=bb: e.activation(out=SG, in_=PS[bb], func=AF.Sigmoid), r=[PSr[bb]], w=[SGr])
            S.add('dve', lambda e, ba=ba: e.tensor_tensor(out=CT, in0=PS[ba], in1=SG, op=ALU.mult),
                  r=[PSr[ba], SGr], w=[p4])
            S.add('pool', lambda e, dc=dc: e.tensor_tensor(out=M[:, dc, :], in0=CT, in1=MC[:, dc, :], op=ALU.add),
                  r=[p4, MCr], w=[Mr])
        if DEBUG == 'p4' and g == 3:
            S.add('sp', lambda e: e.dma_start(out=dbg[:, 0, :], in_=ZP.rearrange("p a b -> p (a b)")), r=[ZPr], dma=misc)
            S.add('sp', lambda e: e.dma_start(out=dbg[:, 1, 0:4096], in_=MC.rearrange("p a b -> p (a b)")), r=[MCr], dma=misc)
            S.add('sp', lambda e: e.dma_start(out=dbg[:, 2, 0:4096], in_=M.rearrange("p a b -> p (a b)")), r=[Mr], dma=misc)
            S.add('sp', lambda e: e.dma_start(out=dbg[:, 3, :], in_=HTG.rearrange("p a b -> p (a b)")), r=[HTGr], dma=misc)
            S.wait_regs('sp', [misc])
            return finish(nc, S, stack)
        wo, wor = ring_load(w_out[:, :])
        for tt in range(4):
            tile_i = g * 4 + tt
            xt = XG[tt % 2]; xr = XGr[tt % 2]
            S.add('sp', lambda e, xt=xt, q0=q0, tt=tt: e.dma_start(
                out=xt, in_=xs[q0 + tt * 128:q0 + (tt + 1) * 128, :]), w=[xr], dma=xr)
            for half in range(2):
                bank = 5 + half
                for kc in range(8):
                    S.add('pe', lambda e, bank=bank, kc=kc, tt=tt, half=half: e.matmul(
                        PS[bank], lhsT=M[:, kc, tt * 128:(tt + 1) * 128], rhs=wo[:, kc, half * 512:(half + 1) * 512],
                        start=(kc == 0), stop=(kc == 7)), r=[Mr, wor], w=[PSr[bank]])
                S.add('dve', lambda e, bank=bank, half=half: e.tensor_tensor(
                    out=TMP2[:, half * 512:(half + 1) * 512], in0=PS[bank], in1=G1[:, half * 512:(half + 1) * 512],
                    op=ALU.mult), r=[PSr[bank], misc], w=[H2Tr])
            S.add('pool', lambda e, xt=xt: e.tensor_tensor(out=X1, in0=TMP2, in1=xt, op=ALU.add),
                  r=[H2Tr, xr], w=[X1r])
            x1dst = y if DEBUG == 'x1' else X1d
            S.add('sp', lambda e, tile_i=tile_i, x1dst=x1dst: e.dma_start(
                out=x1dst[tile_i * 128:(tile_i + 1) * 128, :], in_=X1), r=[X1r], dma=X1r)
            norm_tile(X1, X1r, W2, SH2, JUNK, SSQ, TMP, [H2, H2B], [H2r, H2Br], tag="n2")
            for kc in range(8):
                bank = 1 + kc // 4
                S.add('pe', lambda e, kc=kc, bank=bank: e.transpose(
                    out=PS[bank][:, (kc % 4) * 128:(kc % 4 + 1) * 128], in_=H2[:, kc * 128:(kc + 1) * 128],
                    identity=identF), r=[H2r, CONST], w=[PSr[bank]])
            for b2 in range(2):
                S.add('act', lambda e, b2=b2: e.copy(out=H2T32[:, b2 * 4:(b2 + 1) * 4, :].rearrange("p a b -> p (a b)"),
                                                     in_=PS[1 + b2]), r=[PSr[1 + b2]], w=[H2Tr])
            for kc in range(8):
                S.add('pe', lambda e, kc=kc: e.matmul(PS[3][:, 0:NE], lhsT=H2T32[:, kc, :], rhs=WRT[:, kc, :],
                                                      start=(kc == 0), stop=(kc == 7)),
                      r=[H2Tr, CONST], w=[PSr[3]])
            S.add('dve', lambda e: e.tensor_tensor(out=LG, in0=PS[3][:, 0:NE], in1=BR, op=ALU.add),
                  r=[PSr[3], CONST], w=[rr4])
            S.add('dve', lambda e: e.max(out=MX, in_=LG), r=[rr4], w=[rr4])
            S.add('dve', lambda e: e.tensor_scalar(out=MK, in0=LG, scalar1=MX[:, 3:4], scalar2=None, op0=ALU.is_ge),
                  r=[rr4], w=[rr4])
            if MOE_GATHER:
                S.add('sp', lambda e, tile_i=tile_i: e.dma_start(out=H2d[tile_i * 128:(tile_i + 1) * 128, :],
                                                                 in_=H2B), r=[H2Br], dma=H2Br)
            S.add('dve', lambda e: e.tensor_scalar(out=SM[:, 0:1], in0=MX[:, 0:1], scalar1=-1.0, scalar2=None,
                                                   op0=ALU.mult), r=[rr4], w=[rr4])
            S.add('act', lambda e: e.activation(out=EX, in_=LG, func=AF.Exp, bias=SM[:, 0:1]), r=[rr4], w=[rr4])
            S.add('dve', lambda e: e.tensor_tensor(out=EX, in0=EX, in1=MK, op=ALU.mult), r=[rr4], w=[rr4])
            S.add('dve', lambda e: e.tensor_reduce(out=SM[:, 1:2], in_=EX, axis=AX.X, op=ALU.add), r=[rr4], w=[rr4])
            S.add('dve', lambda e: e.reciprocal(out=SM[:, 2:3], in_=SM[:, 1:2]), r=[rr4], w=[rr4])
            S.add('dve', lambda e, tile_i=tile_i: e.tensor_scalar(out=WG[:, tile_i, :], in0=EX, scalar1=SM[:, 2:3],
                                                                   scalar2=None, op0=ALU.mult), r=[rr4], w=[WGr])
            for kc in range(8):
                S.add('pe', lambda e, kc=kc: e.transpose(out=PSB[7][:, kc * 128:(kc + 1) * 128],
                                                         in_=H2B[:, kc * 128:(kc + 1) * 128], identity=identB),
                      r=[H2Br, misc], w=[PSr[7]])
            S.add('act', lambda e, q0=q0, tt=tt: e.copy(
                out=OT[:, :, q0 + tt * 128:q0 + (tt + 1) * 128], in_=PSB[7].rearrange("p (a b) -> p a b", b=128)),
                r=[PSr[7]], w=[OTr[g]])

    if DEBUG == 'x1':
        barrier()
        S.add('sp', lambda e: e.dma_start(out=dbg.rearrange("(t p) e -> p t e", p=128), in_=WG), r=[WGr], dma=misc)
        S.wait_regs('sp', [misc])
        return finish(nc, S, stack)

    barrier()
    A.off = P5_MARK
    H2T = OT
    ACC = A.alloc(F32, 16, D); ACCr = [Reg("acc%d" % i) for i in range(16)]
    BGg = A.alloc(F32, 8, NE); BGu = A.alloc(F32, 8, NE)
    RK = A.alloc(F32, 16, NE); rkr = Reg("rk")
    IOTA = A.alloc(F32, CAP)
    H2TOK = OT.rearrange("p a b -> p (a b)").rearrange("p (t d) -> p t d", d=D)
    LOOP_MARK = A.off
    BGrow = A.alloc(F32, 2048)
    BD = A.alloc(F32, D)
    WGT = A.alloc(F32, 16, 128)
    p5 = Reg("p5")
    S.add('sp', lambda e: e.dma_start(out=BGrow[0:NE, :], in_=b_gate_up), w=[p5], dma=p5)
    S.add('sp', lambda e: e.dma_start(out=BD[0:NE, :], in_=b_down), dma=p5)
    p5.w = ('dma', p5, p5.dcnt)
    bg3 = BGrow.rearrange("p (f m t) -> p f m t", m=128, t=2)
    for fc in range(8):
        S.add('pe', lambda e, fc=fc: e.transpose(out=PS[0][:, fc * NE:(fc + 1) * NE],
                                                 in_=bg3[0:NE, fc, :, 0],
                                                 identity=identF[0:NE, 0:NE]), r=[p5, CONST], w=[PSr[0]])
        S.add('pe', lambda e, fc=fc: e.transpose(out=PS[0][:, 256 + fc * NE:256 + (fc + 1) * NE],
                                                 in_=bg3[0:NE, fc, :, 1],
                                                 identity=identF[0:NE, 0:NE]), r=[p5, CONST], w=[PSr[0]])
    bgr = Reg("bg")
    S.add('dve', lambda e: e.tensor_copy(out=BGg.rearrange("p a b -> p (a b)"), in_=PS[0][:, 0:256]),
          r=[PSr[0]], w=[bgr])
    S.add('dve', lambda e: e.tensor_scalar(out=BGu.rearrange("p a b -> p (a b)"), in0=PS[0][:, 256:512],
                                           scalar1=1.0, scalar2=None, op0=ALU.add), r=[PSr[0]], w=[bgr])
    for ti in range(16):
        S.add('pe', lambda e, ti=ti: e.transpose(out=PS[1][0:NE, 0:128], in_=WG[:, ti, :], identity=identF),
              r=[WGr, CONST], w=[PSr[1]])
        S.add('dve', lambda e, ti=ti: e.tensor_copy(out=WGT[0:NE, ti, :], in_=PS[1][0:NE, 0:128]),
              r=[PSr[1]], w=[bgr])
        for half in range(2):
            S.add('pe', lambda e, ti=ti, half=half: e.matmul(
                PS[2 + half], lhsT=WGT[0:NE, ti, :], rhs=BD[0:NE, half * 512:(half + 1) * 512],
                start=True, stop=True), r=[bgr, p5], w=[PSr[2 + half]])
            S.add('act', lambda e, ti=ti, half=half: e.copy(out=ACC[:, ti, half * 512:(half + 1) * 512],
                                                            in_=PS[2 + half]), r=[PSr[2 + half]], w=[ACCr[ti]])
    def moe_dense():
        barrier()
        A.off = LOOP_MARK
        RING5 = [A.alloc(BF, 8, 1024) for _ in range(3)]; RING5r = [Reg("r5_%d" % i) for i in range(3)]
        r5_i = [0]

        def ring5_load(src2d):
            i = r5_i[0] % 3; r5_i[0] += 1
            S.add('pool', lambda e, i=i: e.dma_start(out=RING5[i], in_=src2d.rearrange("(kc p) n -> p kc n", p=128)),
                  w=[RING5r[i]], dma=RING5r[i])
            return RING5[i], RING5r[i]

        AT = [A.alloc(BF, 8, 512) for _ in range(2)]; ATr = [Reg("at0"), Reg("at1")]
        GS = [A.alloc(F32, 512) for _ in range(2)]; GSr = [Reg("gs0"), Reg("gs1")]
        SGM = [A.alloc(F32, 512) for _ in range(2)]
        U1 = [A.alloc(F32, 512) for _ in range(2)]

        it5 = [0]
        for ex in range(NE):
            wgA, wgAr = ring5_load(w_gate_up[ex, :, 0:1024])
            wgB, wgBr = ring5_load(w_gate_up[ex, :, 1024:2048])
            wd, wdr = ring5_load(w_down[ex, :, :])
            for tg in range(4):
                at = AT[tg % 2]; atr = ATr[tg % 2]
                for fc in range(8):
                    wt, wr = (wgA, wgAr) if fc < 4 else (wgB, wgBr)
                    wv = wt.rearrange("p k (f m t) -> p k f m t", m=128, t=2)
                    k = it5[0] % 2; it5[0] += 1
                    bg_, bu_ = 4 * k, 4 * k + 1
                    for (bank, off) in ((bg_, 0), (bu_, 1)):
                        for kc in range(8):
                            S.add('pe', lambda e, bank=bank, off=off, wv=wv, kc=kc, fc=fc, tg=tg: e.matmul(
                                PS[bank], lhsT=wv[:, kc, fc % 4, :, off],
                                rhs=H2T[:, kc, tg * 512:(tg + 1) * 512], start=(kc == 0), stop=(kc == 7)),
                                r=[wr, OTr[tg]], w=[PSr[bank]])
                    gs = GS[k]; gsr = GSr[k]; sg = SGM[k]; u1 = U1[k]
                    S.add('dve', lambda e, gs=gs, bg_=bg_, fc=fc, ex=ex: e.tensor_scalar(
                        out=gs, in0=PS[bg_], scalar1=BGg[:, fc, ex:ex + 1], scalar2=7.0, op0=ALU.add, op1=ALU.min),
                        r=[PSr[bg_], bgr], w=[gsr])
                    S.add('act', lambda e, gs=gs, sg=sg: e.activation(out=sg, in_=gs, func=AF.Sigmoid, scale=1.702),
                          r=[gsr], w=[gsr])
                    S.add('dve', lambda e, u1=u1, bu_=bu_, fc=fc, ex=ex: e.tensor_scalar(
                        out=u1, in0=PS[bu_], scalar1=BGu[:, fc, ex:ex + 1], scalar2=8.0, op0=ALU.add, op1=ALU.min),
                        r=[PSr[bu_], bgr], w=[gsr])
                    S.add('dve', lambda e, gs=gs, sg=sg: e.tensor_tensor(out=gs, in0=gs, in1=sg, op=ALU.mult),
                          r=[gsr], w=[gsr])
                    S.add('dve', lambda e, u1=u1, gs=gs, at=at, fc=fc: e.scalar_tensor_tensor(
                        out=at[:, fc, :], in0=u1, scalar=-6.0, in1=gs, op0=ALU.max, op1=ALU.mult),
                        r=[gsr], w=[atr])
                for tt in range(4):
                    ti = tg * 4 + tt
                    for half in range(2):
                        bank = 2 + half
                        for fc in range(8):
                            S.add('pe', lambda e, bank=bank, fc=fc, at=at, tt=tt, half=half: e.matmul(
                                PS[bank], lhsT=at[:, fc, tt * 128:(tt + 1) * 128],
                                rhs=wd[:, fc, half * 512:(half + 1) * 512], start=(fc == 0), stop=(fc == 7)),
                                r=[atr, wdr], w=[PSr[bank]])
                        S.add('dve', lambda e, bank=bank, ti=ti, half=half, ex=ex: e.scalar_tensor_tensor(
                            out=ACC[:, ti, half * 512:(half + 1) * 512], in0=PS[bank], scalar=WG[:, ti, ex:ex + 1],
                            in1=ACC[:, ti, half * 512:(half + 1) * 512], op0=ALU.mult, op1=ALU.add),
                            r=[PSr[bank], WGr], w=[ACCr[ti]])


    if MOE_GATHER:
        TRIF = A.alloc(F32, 128); TRIB = A.alloc(BF, 128); ONESB = A.alloc(BF, 128)
        MKA = A.alloc(BF, 16, NE)
        S.add('dve', lambda e: e.tensor_scalar(out=MKA.rearrange("p a b -> p (a b)"),
                                               in0=WG.rearrange("p a b -> p (a b)"), scalar1=0.0, scalar2=None,
                                               op0=ALU.is_gt), r=[WGr], w=[bgr])
        S.add('sp', lambda e: e.dma_start(out=TRIF, in_=tri), w=[p5], dma=p5)
        S.add('sp', lambda e: e.dma_start(out=IOTA, in_=iota), w=[p5], dma=p5)
        S.add('sp', lambda e: e.dma_start(out=H2TOK, in_=H2d.rearrange("(t p) d -> p t d", p=128)),
              w=OTr + [p5], dma=p5)
        S.add('dve', lambda e: e.tensor_copy(out=TRIB, in_=TRIF), r=[p5], w=[bgr])
        S.add('pool', lambda e: e.memset(ONESB, 1.0), w=[bgr])
        for ti in range(16):
            b = 4 + ti % 2
            for tj in range(ti):
                S.add('pe', lambda e: e.matmul(PS[b][:, 0:NE], lhsT=ONESB, rhs=MKA[:, tj, :],
                                               start=(tj == 0), stop=False), r=[bgr, WGr], w=[PSr[b]])
            S.add('pe', lambda e: e.matmul(PS[b][:, 0:NE], lhsT=TRIB, rhs=MKA[:, ti, :],
                                           start=(ti == 0), stop=True), r=[bgr, WGr], w=[PSr[b]])
            S.add('dve', lambda e: e.scalar_tensor_tensor(out=RK[:, ti, :], in0=PS[b][:, 0:NE], scalar=1.0,
                                                          in1=MKA[:, ti, :], op0=ALU.add, op1=ALU.mult),
                  r=[PSr[b], WGr], w=[rkr])
            S.add('dve', lambda e: e.tensor_scalar(out=RK[:, ti, :], in0=RK[:, ti, :], scalar1=-1.0, scalar2=None,
                                                   op0=ALU.add), r=[rkr], w=[rkr])
        barrier()
        A.off = LOOP_MARK
        NR = 4
        RING5 = [A.alloc(BF, 8, 512) for _ in range(NR)]; RING5r = [Reg("r5_%d" % i) for i in range(NR)]
        r5_i = [0]

        def ring5_load(src2d):
            i = r5_i[0] % NR; r5_i[0] += 1
            S.add('pool', lambda e: e.dma_start(out=RING5[i], in_=src2d.rearrange("(kc p) n -> p kc n", p=128)),
                  w=[RING5r[i]], dma=RING5r[i])
            return RING5[i], RING5r[i]

        SEL = A.alloc(BF, 16, CAP); SELr = Reg("sel")
        XET = A.alloc(BF, 8, CAP); XEr = Reg("xet")
        YE = XET.rearrange("p a b -> p (a b)").rearrange("p (c d) -> p c d", d=D)
        AT = A.alloc(BF, 8, CAP); ATr = Reg("at")
        ST = [A.alloc(BF, CAP) for _ in range(2)]; STr = [Reg("st0"), Reg("st1")]
        GS = [A.alloc(F32, CAP) for _ in range(2)]; GSr = [Reg("gs0"), Reg("gs1")]
        SGM = [A.alloc(F32, CAP) for _ in range(2)]
        U1 = [A.alloc(F32, CAP) for _ in range(2)]
        NCC = CAP // 128
        for ex in range(NE):
            for ti in range(16):
                S.add('dve', lambda e: e.tensor_scalar(out=SEL[:, ti, :], in0=IOTA, scalar1=RK[:, ti, ex:ex + 1],
                                                       scalar2=None, op0=ALU.is_equal), r=[rkr, p5], w=[SELr])
            for fc in range(8):
                b = fc % 2
                for ti in range(16):
                    S.add('pe', lambda e: e.matmul(PS[b][:, 0:CAP], lhsT=H2TOK[:, ti, fc * 128:(fc + 1) * 128],
                                                   rhs=SEL[:, ti, :], start=(ti == 0), stop=(ti == 15)),
                          r=[p5, SELr], w=[PSr[b]])
                S.add('act', lambda e: e.copy(out=XET[:, fc, :], in_=PS[b][:, 0:CAP]), r=[PSr[b]], w=[XEr])
            for fc in range(8):
                if fc % 2 == 0:
                    wt, wr = ring5_load(w_gate_up[ex, :, fc * 256:fc * 256 + 512])
                    wv = wt.rearrange("p k (f m t) -> p k f m t", m=128, t=2)
                k = fc % 2
                bg_, bu_ = 2 + 2 * k, 3 + 2 * k
                for (bank, off) in ((bg_, 0), (bu_, 1)):
                    for kc in range(8):
                        S.add('pe', lambda e: e.matmul(PS[bank][:, 0:CAP], lhsT=wv[:, kc, fc % 2, :, off],
                                                       rhs=XET[:, kc, :], start=(kc == 0), stop=(kc == 7)),
                              r=[wr, XEr], w=[PSr[bank]])
                gs = GS[k]; gsr = GSr[k]; sg = SGM[k]; u1 = U1[k]
                S.add('dve', lambda e: e.tensor_scalar(out=gs, in0=PS[bg_][:, 0:CAP], scalar1=BGg[:, fc, ex:ex + 1],
                                                       scalar2=7.0, op0=ALU.add, op1=ALU.min),
                      r=[PSr[bg_], bgr], w=[gsr])
                S.add('act', lambda e: e.activation(out=sg, in_=gs, func=AF.Sigmoid, scale=1.702),
                      r=[gsr], w=[gsr])
                S.add('dve', lambda e: e.tensor_scalar(out=u1, in0=PS[bu_][:, 0:CAP], scalar1=BGu[:, fc, ex:ex + 1],
                                                       scalar2=8.0, op0=ALU.add, op1=ALU.min),
                      r=[PSr[bu_], bgr], w=[gsr])
                S.add('dve', lambda e: e.tensor_tensor(out=gs, in0=gs, in1=sg, op=ALU.mult), r=[gsr], w=[gsr])
                S.add('dve', lambda e: e.scalar_tensor_tensor(out=AT[:, fc, :], in0=u1, scalar=-6.0, in1=gs,
                                                              op0=ALU.max, op1=ALU.mult), r=[gsr], w=[ATr])
            for half in range(2):
                wd, wdr = ring5_load(w_down[ex, :, half * 512:(half + 1) * 512])
                for cc in range(NCC):
                    b = 6 + (half * NCC + cc) % 2
                    for fc in range(8):
                        S.add('pe', lambda e: e.matmul(PS[b], lhsT=AT[:, fc, cc * 128:(cc + 1) * 128],
                                                       rhs=wd[:, fc, :], start=(fc == 0), stop=(fc == 7)),
                              r=[ATr, wdr], w=[PSr[b]])
                    S.add('act', lambda e: e.copy(out=YE[:, cc, half * 512:(half + 1) * 512], in_=PS[b]),
                          r=[PSr[b]], w=[XEr])
            for ti in range(16):
                tb = ti % 2
                st = ST[tb]; str_ = STr[tb]
                for cc in range(NCC):
                    S.add('pe', lambda e: e.transpose(out=PSB[tb][:, cc * 128:(cc + 1) * 128],
                                                      in_=SEL[:, ti, cc * 128:(cc + 1) * 128], identity=identB),
                          r=[SELr, misc], w=[PSr[tb]])
                S.add('act', lambda e: e.copy(out=st, in_=PSB[tb][:, 0:CAP]), r=[PSr[tb]], w=[str_])
                for half in range(2):
                    bank = 2 + 2 * tb + half
                    for cc in range(NCC):
                        S.add('pe', lambda e: e.matmul(PS[bank], lhsT=st[:, cc * 128:(cc + 1) * 128],
                                                       rhs=YE[:, cc, half * 512:(half + 1) * 512],
                                                       start=(cc == 0), stop=(cc == NCC - 1)),
                              r=[str_, XEr], w=[PSr[bank]])
                    S.add('dve', lambda e: e.scalar_tensor_tensor(
                        out=ACC[:, ti, half * 512:(half + 1) * 512], in0=PS[bank], scalar=WG[:, ti, ex:ex + 1],
                        in1=ACC[:, ti, half * 512:(half + 1) * 512], op0=ALU.mult, op1=ALU.add),
                        r=[PSr[bank], WGr], w=[ACCr[ti]])
    else:
        moe_dense()

    barrier()
    A.off = LOOP_MARK
    XF = [A.alloc(F32, D) for _ in range(2)]; XFr = [Reg("xf0"), Reg("xf1")]
    for ti in range(16):
        xf = XF[ti % 2]; xfr = XFr[ti % 2]
        S.add('sp', lambda e, xf=xf, ti=ti: e.dma_start(out=xf, in_=X1d[ti * 128:(ti + 1) * 128, :]),
              w=[xfr], dma=xfr)
        S.add('dve', lambda e, ti=ti: e.tensor_tensor(out=ACC[:, ti, :], in0=ACC[:, ti, :], in1=G2, op=ALU.mult),
              r=[CONST, misc], w=[ACCr[ti]])
        S.add('pool', lambda e, ti=ti, xf=xf: e.tensor_tensor(out=xf, in0=ACC[:, ti, :], in1=xf, op=ALU.add),
              r=[ACCr[ti]], w=[xfr])
        S.add('sp', lambda e, xf=xf, ti=ti: e.dma_start(out=y[ti * 128:(ti + 1) * 128, :], in_=xf),
              r=[xfr], dma=xfr)
    S.wait_regs('sp', XFr)
    return finish(nc, S, stack)


def finish(nc, S, stack):
    S.prepare(nc, stack)
    with nc.Block() as block:
        @block.tensor
        def _(e):
            S.run('pe', e)

        @block.scalar
        def _(e):
            S.run('act', e)

        @block.vector
        def _(e):
            S.run('dve', e)

        @block.gpsimd
        def _(e):
            S.run('pool', e)

        @block.sync
        def _(e):
            S.run('sp', e)
    stack.close()
    return nc


def rope_table():
    inv = (np.float32(10000.0) ** (-(np.arange(0, 64, 2, dtype=np.float32) / np.float32(64)))).astype(np.float32)
    pos = np.arange(SEQ, dtype=np.float32)
    ang = (pos[:, None] * inv[None, :]).astype(np.float32)
    emb = np.concatenate([ang, ang], axis=-1)
    cos = np.cos(emb).astype(np.float32); sin = np.sin(emb).astype(np.float32)
    sin[:, 0:32] = -sin[:, 0:32]
    return np.concatenate([cos, sin], axis=1).astype(np.float32)


def make_in_maps(inp):
    x = np.asarray(inp['x'], dtype=np.float32)
    tab = rope_table()
    shared = {}
    for k in ('b_ada', 'norm1_w', 'q_norm_w', 'k_norm_w', 'lambda_q1', 'lambda_k1', 'lambda_q2', 'lambda_k2',
              'subln_w', 'norm2_w', 'b_router'):
        shared[k] = np.ascontiguousarray(np.asarray(inp[k], dtype=np.float32))
    for k in ('w_ada', 'w_in', 'w_attn_o', 'conv_w', 'w_conv_o', 'w_out', 'w_router', 'w_gate_up', 'b_gate_up',
              'w_down', 'b_down'):
        shared[k] = np.ascontiguousarray(np.asarray(inp[k], dtype=np.float32)[0])
    shared['ident'] = np.eye(128, dtype=np.float32)
    shared['tri'] = np.triu(np.ones((128, 128), np.float32), 1)
    shared['iota'] = np.ascontiguousarray(np.broadcast_to(np.arange(CAP, dtype=np.float32), (128, CAP)))
    maps = []
    for core in range(8):
        b = core // 4; t0 = (core % 4) * TOWN
        m = dict(shared)
        m['xs'] = np.ascontiguousarray(np.roll(x[b], -t0, axis=0))
        m['rope'] = np.ascontiguousarray(np.roll(tab, -t0, axis=0))
        xhal = np.zeros((2, D), np.float32); msk = np.zeros((128, 2), np.float32)
        if t0 > 0:
            xhal[0] = x[b, t0 - 1]; msk[:, 0] = 1.0
        if t0 + TOWN < SEQ:
            xhal[1] = x[b, t0 + TOWN]; msk[:, 1] = 1.0
        m['xh'] = xhal; m['hm'] = msk
        m['cvec'] = np.ascontiguousarray(np.asarray(inp['c'], dtype=np.float32)[b:b + 1])
        maps.append(m)
    return maps


_NC = None


def kernel(**inp):
    global _NC
    if _NC is None:
        _NC = build()
    maps = make_in_maps(inp)
    res = run_bass_kernel_spmd(_NC, maps, core_ids=list(range(8)))
    out = np.zeros((2, SEQ, D), np.float32)
    for core in range(8):
        b = core // 4; t0 = (core % 4) * TOWN
        out[b, t0:t0 + TOWN] = res.results[core]['y']
    return out
```

```python
import types
import numpy as np
from contextlib import ExitStack
import concourse.bass as bass
import concourse.mybir as mybir
from concourse.bass_utils import run_bass_kernel_spmd

F32 = mybir.dt.float32
BF = mybir.dt.bfloat16
ALU = mybir.AluOpType
AF = mybir.ActivationFunctionType
AX = mybir.AxisListType

SEQ = 8192
D = 1024
TOWN = 2048
NE = 32
ENGS = ['pe', 'act', 'dve', 'pool', 'sp']
DEBUG = None
MOE_GATHER = False
CAP = 384


class Reg:
    __slots__ = ('name', 'w', 'rs', 'dsem', 'dcnt', 'excl')
    ALL = []

    def __init__(s, name, excl=False):
        s.name = name; s.w = None; s.rs = {}; s.dsem = None; s.dcnt = 0; s.excl = excl
        Reg.ALL.append(s)


class Op:
    __slots__ = ('eng', 'fn', 'deps', 'needs_inc', 'sigval', 'dreg')


def freeze(fn):
    if fn is None or fn.__closure__ is None:
        return fn
    cells = []
    for c in fn.__closure__:
        try:
            cells.append(types.CellType(c.cell_contents))
        except ValueError:
            cells.append(c)
    return types.FunctionType(fn.__code__, fn.__globals__, fn.__name__, fn.__defaults__, tuple(cells))


class Sched:
    def __init__(s):
        s.q = {e: [] for e in ENGS}
        s.dregs = []

    def add(s, eng, fn, r=(), w=(), dma=None):
        op = Op(); op.eng = eng; op.fn = freeze(fn); op.needs_inc = False; op.sigval = 0; op.dreg = dma
        w = list(w) + [g for g in r if g.excl]
        r = [g for g in r if not g.excl]
        deps = []
        for g in r:
            if g.w is not None:
                deps.append(g.w)
        for g in w:
            if g.w is not None:
                deps.append(g.w)
            deps.extend(g.rs.values())
        op.deps = [d for d in deps if not (d[0] == 'op' and d[1].eng == eng and eng == 'pe')]
        if dma is not None:
            if dma.dcnt == 0 and dma not in s.dregs:
                s.dregs.append(dma)
            dma.dcnt += 16
            tok = ('dma', dma, dma.dcnt); key = ('d', id(dma))
        else:
            tok = ('op', op); key = eng
        for g in r:
            g.rs[key] = tok
        for g in w:
            g.w = tok; g.rs = {}
        s.q[eng].append(op)
        return op

    def wait_regs(s, eng, regs):
        op = Op(); op.eng = eng; op.fn = None; op.needs_inc = False; op.sigval = 0; op.dreg = None
        deps = []
        for g in regs:
            if g.w is not None:
                deps.append(g.w)
            deps.extend(g.rs.values())
        op.deps = deps
        s.q[eng].append(op)

    def barrier(s):
        deps = []
        for e in ENGS:
            for op in reversed(s.q[e]):
                if op.fn is not None and op.dreg is None:
                    deps.append(('op', op))
                    break
        for g in s.dregs:
            if g.dcnt > 0:
                deps.append(('dma', g, g.dcnt))
        for e in ENGS:
            op = Op(); op.eng = e; op.fn = None; op.needs_inc = False; op.sigval = 0; op.dreg = None
            op.deps = [d for d in deps if not (d[0] == 'op' and d[1].eng == e and e == 'pe')]
            s.q[e].append(op)
        for g in Reg.ALL:
            g.w = None; g.rs = {}

    def prepare(s, nc, stack):
        for e in ENGS:
            for op in s.q[e]:
                for d in op.deps:
                    if d[0] == 'op':
                        d[1].needs_inc = True
        for e in ENGS:
            c = 0
            for op in s.q[e]:
                if op.needs_inc:
                    c += 1
                    op.sigval = c
        s.esem = {e: stack.enter_context(nc.semaphore("sem_" + e)) for e in ENGS}
        for i, g in enumerate(s.dregs):
            g.dsem = stack.enter_context(nc.semaphore("dsem%d_%s" % (i, g.name)))

    def run(s, e, eng):
        known = {}
        for op in s.q[e]:
            for d in op.deps:
                if d[0] == 'op':
                    sem = s.esem[d[1].eng]; val = d[1].sigval; k = d[1].eng
                else:
                    sem = d[1].dsem; val = d[2]; k = id(d[1])
                if known.get(k, 0) < val:
                    eng.wait_ge(sem, val)
                    known[k] = val
            if op.fn is None:
                continue
            ins = op.fn(eng)
            if op.dreg is not None:
                ins.then_inc(op.dreg.dsem, 16)
            elif op.needs_inc:
                ins.then_inc(s.esem[e], 1)


class Arena:
    def __init__(s, t, nbytes):
        s.t = t; s.off = 0; s.cap = nbytes

    def alloc(s, dtype, *free):
        n = 1
        for f in free:
            n *= f
        esz = 2 if dtype == BF else 4
        nbytes = (n * esz + 63) // 64 * 64
        assert s.off + nbytes <= s.cap, ("arena overflow", s.off, nbytes, s.cap)
        a = s.t[:, s.off // 4:(s.off + nbytes) // 4]
        s.off += nbytes
        if dtype != F32:
            a = a.bitcast(dtype)
        a = a[:, 0:n]
        if len(free) == 2:
            a = a.rearrange("p (a b) -> p a b", b=free[1])
        elif len(free) == 3:
            a = a.rearrange("p (a b c) -> p a b c", b=free[1], c=free[2])
        return a


def build():
    nc = bass.Bass("TRN2", target_bir_lowering=False)

    def din(name, shape, dt=F32):
        return nc.dram_tensor(name, list(shape), dt, kind="ExternalInput").ap()

    xs = din("xs", [SEQ, D]); xh = din("xh", [2, D]); hm = din("hm", [128, 2])
    rope = din("rope", [SEQ, 128]); cvec = din("cvec", [1, D]); ident = din("ident", [128, 128])
    w_ada = din("w_ada", [D, 6 * D]); b_ada = din("b_ada", [1, 6 * D])
    norm1_w = din("norm1_w", [1, D]); w_in = din("w_in", [D, 8 * D])
    q_norm_w = din("q_norm_w", [1, 64]); k_norm_w = din("k_norm_w", [1, 64])
    lq1 = din("lambda_q1", [1, 64]); lk1 = din("lambda_k1", [1, 64])
    lq2 = din("lambda_q2", [1, 64]); lk2 = din("lambda_k2", [1, 64])
    subln_w = din("subln_w", [1, 128]); w_attn_o = din("w_attn_o", [D, D])
    conv_w = din("conv_w", [3, D]); w_conv_o = din("w_conv_o", [D, D]); w_out = din("w_out", [D, D])
    norm2_w = din("norm2_w", [1, D]); w_router = din("w_router", [D, NE]); b_router = din("b_router", [1, NE])
    w_gate_up = din("w_gate_up", [NE, D, 2 * D]); b_gate_up = din("b_gate_up", [NE, 2 * D])
    w_down = din("w_down", [NE, D, D]); b_down = din("b_down", [NE, D])
    y = nc.dram_tensor("y", [TOWN, D], F32, kind="ExternalOutput").ap()
    KTd = nc.dram_tensor("ktd", [128, 8, SEQ], BF, kind="Internal").ap()
    Vd = nc.dram_tensor("vd", [8, SEQ, 128], BF, kind="Internal").ap()
    X1d = nc.dram_tensor("x1d", [TOWN, D], F32, kind="Internal").ap()
    H2d = nc.dram_tensor("h2d", [TOWN, D], BF, kind="Internal").ap()
    tri = din("tri", [128, 128]); iota = din("iota", [128, CAP])
    dbg = None
    if DEBUG == 'attn':
        dbg = nc.dram_tensor("dbg", [128, 8 * TOWN], BF, kind="ExternalOutput").ap()
    elif DEBUG == 'x1':
        dbg = nc.dram_tensor("dbg", [TOWN, 32], F32, kind="ExternalOutput").ap()
    elif DEBUG == 'p4':
        dbg = nc.dram_tensor("dbg", [128, 4, 8 * 514], BF, kind="ExternalOutput").ap()

    S = Sched()
    stack = ExitStack()
    ARENA_BYTES = 188 * 1024
    arena_t = stack.enter_context(nc.sbuf_tensor("arena", [128, ARENA_BYTES // 4], F32))
    A = Arena(arena_t, ARENA_BYTES)
    PS = []
    PSr = []
    SXYs = []
    for i in range(2):
        sxy = stack.enter_context(nc.psum_tensor("sxy%d" % i, [128, 1024], F32))
        SXYs.append(sxy[:, :])
        PS.append(sxy[:, 0:512]); PS.append(sxy[:, 512:1024])
    for i in range(4, 8):
        t = stack.enter_context(nc.psum_tensor("ps%d" % i, [128, 512], F32))
        PS.append(t[:, :])
    for i in range(8):
        PSr.append(Reg("ps%d" % i, excl=True))
    PSB = [p.bitcast(BF) for p in PS]

    identF = A.alloc(F32, 128); identB = A.alloc(BF, 128)
    OT = A.alloc(BF, 8, TOWN)
    G2 = A.alloc(F32, D)
    WG = A.alloc(F32, 16, NE)
    P5_MARK = A.off
    SH1 = A.alloc(F32, D); G1 = A.alloc(F32, D); SH2 = A.alloc(F32, D)
    W1 = A.alloc(F32, D); W2 = A.alloc(F32, D)
    KNW = A.alloc(F32, 64); KNWS = A.alloc(F32, 64); QNW = A.alloc(F32, 64); QNWS = A.alloc(F32, 64)
    SUBW = A.alloc(F32, 128); NLAM = A.alloc(F32, 2)
    CW = A.alloc(F32, 3, 8); HM = A.alloc(F32, 2); BR = A.alloc(F32, NE); WRT = A.alloc(F32, 8, NE)
    PASS_MARK = A.off

    CONST = Reg("const")
    OTr = [Reg("ot%d" % g) for g in range(4)]
    WGr = Reg("wg")
    misc = Reg("misc")

    def cload(out, in_, **kw):
        S.add('sp', lambda e: e.dma_start(out=out, in_=in_, **kw), dma=CONST)

    def row_b(ap1d):
        return ap1d.partition_broadcast(128)

    SC1 = A.alloc(F32, D); SC2 = A.alloc(F32, D)
    cT = A.alloc(F32, 8); scT = A.alloc(F32, 8); scB = A.alloc(F32, 8, 128)
    LQ = [A.alloc(F32, 64) for _ in range(4)]
    LT = A.alloc(F32, 8)
    WA = [A.alloc(F32, 8, 512) for _ in range(2)]
    WAr = [Reg("wa0"), Reg("wa1")]

    cload(identF, ident)
    mod_tiles = [SH1, SC1, G1, SH2, SC2, G2]
    for i, t in enumerate(mod_tiles):
        cload(t, row_b(b_ada[0, i * D:(i + 1) * D]))
    cload(W1, row_b(norm1_w[0, :])); cload(W2, row_b(norm2_w[0, :]))
    cload(KNW, row_b(k_norm_w[0, :])); cload(QNW, row_b(q_norm_w[0, :]))
    cload(KNWS[:, 0:32], row_b(k_norm_w[0, 32:64])); cload(KNWS[:, 32:64], row_b(k_norm_w[0, 0:32]))
    cload(QNWS[:, 0:32], row_b(q_norm_w[0, 32:64])); cload(QNWS[:, 32:64], row_b(q_norm_w[0, 0:32]))
    cload(SUBW, row_b(subln_w[0, :]))
    for t, src in zip(LQ, (lq1, lk1, lq2, lk2)):
        cload(t, row_b(src[0, :]))
    cload(HM, hm); cload(BR, row_b(b_router[0, :]))
    cload(WRT, w_router.rearrange("(kc p) e -> p kc e", p=128))
    for k3 in range(3):
        cload(CW[:, k3, :], conv_w[k3, :].rearrange("(j p) -> p j", p=128), allow_slow_non_contiguous=True)
    cload(cT, cvec.rearrange("o (j p) -> p (o j)", p=128), allow_slow_non_contiguous=True)
    CONST.w = ('dma', CONST, CONST.dcnt)

    S.add('dve', lambda e: e.tensor_copy(out=identB, in_=identF), r=[CONST], w=[misc])
    S.add('pool', lambda e: e.memset(NLAM[:, 1:2], -0.5), w=[misc])
    S.add('act', lambda e: e.activation(out=scT, in_=cT, func=AF.Sigmoid), r=[CONST], w=[misc])
    S.add('dve', lambda e: e.tensor_tensor(out=scT, in0=scT, in1=cT, op=ALU.mult), r=[misc, CONST], w=[misc])
    S.add('dve', lambda e: e.tensor_copy(out=scB, in_=scT.unsqueeze(2).broadcast_to([128, 8, 128])),
          r=[misc], w=[misc])
    for i in range(2):
        S.add('dve', lambda e, i=i: e.tensor_tensor(out=LQ[2 * i], in0=LQ[2 * i], in1=LQ[2 * i + 1], op=ALU.mult),
              r=[CONST, misc], w=[misc])
        S.add('dve', lambda e, i=i: e.tensor_reduce(out=LT[:, i:i + 1], in_=LQ[2 * i], axis=AX.X, op=ALU.add),
              r=[misc], w=[misc])
    S.add('act', lambda e: e.activation(out=LT[:, 2:4], in_=LT[:, 0:2], func=AF.Exp), r=[misc], w=[misc])
    S.add('dve', lambda e: e.tensor_tensor(out=LT[:, 4:5], in0=LT[:, 2:3], in1=LT[:, 3:4], op=ALU.subtract),
          r=[misc], w=[misc])
    S.add('dve', lambda e: e.tensor_scalar(out=NLAM[:, 0:1], in0=LT[:, 4:5], scalar1=0.2, scalar2=-1.0,
                                           op0=ALU.add, op1=ALU.mult), r=[misc], w=[misc])
    S.add('dve', lambda e: e.tensor_scalar(out=SUBW, in0=SUBW, scalar1=0.8, scalar2=None, op0=ALU.mult),
          r=[CONST, misc], w=[misc])

    for cg in range(12):
        wa = WA[cg % 2]; war = WAr[cg % 2]
        S.add('sp', lambda e, wa=wa, cg=cg: e.dma_start(
            out=wa, in_=w_ada[:, cg * 512:(cg + 1) * 512].rearrange("(kc p) n -> p kc n", p=128)),
            w=[war], dma=war)
        bank = cg % 2
        for kc in range(8):
            S.add('pe', lambda e, wa=wa, kc=kc, bank=bank: e.matmul(
                PS[bank], lhsT=scB[:, kc, :], rhs=wa[:, kc, :], start=(kc == 0), stop=(kc == 7)),
                r=[misc, war], w=[PSr[bank]])
        dst = mod_tiles[cg // 2][:, (cg % 2) * 512:(cg % 2 + 1) * 512]
        S.add('dve', lambda e, dst=dst, bank=bank: e.tensor_tensor(out=dst, in0=PS[bank], in1=dst, op=ALU.add),
              r=[PSr[bank], CONST, misc], w=[misc])
    S.add('dve', lambda e: e.scalar_tensor_tensor(out=W1, in0=SC1, scalar=1.0, in1=W1, op0=ALU.add, op1=ALU.mult),
          r=[misc, CONST], w=[misc])
    S.add('dve', lambda e: e.scalar_tensor_tensor(out=W2, in0=SC2, scalar=1.0, in1=W2, op0=ALU.add, op1=ALU.mult),
          r=[misc, CONST], w=[misc])

    barrier = S.barrier

    nrm_r = Reg("nrm")

    def norm_tile(xt, xr, Wt, SHt, junk, ssq, tmp, outs, regs_out, npart=128, tag="n"):
        tr = nrm_r
        S.add('act', lambda e: e.activation(out=junk[0:npart], in_=xt[0:npart], func=AF.Square,
                                            accum_out=ssq[0:npart, 0:1]), r=[xr], w=[tr])
        S.add('act', lambda e: e.activation(out=ssq[0:npart, 1:2], in_=ssq[0:npart, 0:1], func=AF.Sqrt,
                                            scale=1.0 / D, bias=1e-6), r=[tr], w=[tr])
        S.add('dve', lambda e: e.reciprocal(out=ssq[0:npart, 2:3], in_=ssq[0:npart, 1:2]), r=[tr], w=[tr])
        S.add('dve', lambda e: e.scalar_tensor_tensor(out=tmp[0:npart], in0=xt[0:npart], scalar=ssq[0:npart, 2:3],
                                                      in1=Wt[0:npart], op0=ALU.mult, op1=ALU.mult),
              r=[tr, xr, misc], w=[tr])
        for o, ro in zip(outs, regs_out):
            S.add('dve', lambda e, o=o: e.tensor_tensor(out=o[0:npart], in0=tmp[0:npart], in1=SHt[0:npart],
                                                        op=ALU.add), r=[tr, misc, xr], w=[ro])

    barrier()
    A.off = PASS_MARK
    QT = A.alloc(BF, 8, TOWN); QTr = Reg("qt")
    WKV = A.alloc(BF, 8, 2048); WKVr = Reg("wkv")
    WQ = A.alloc(BF, 8, 1024); WQr = Reg("wq")
    XT = [A.alloc(F32, D) for _ in range(2)]; XTr = [Reg("xt0"), Reg("xt1")]
    RT = [A.alloc(F32, 128) for _ in range(2)]; RTr = [Reg("rt0"), Reg("rt1")]
    Hb2 = [A.alloc(BF, D) for _ in range(2)]; Hr2 = [Reg("h0"), Reg("h1")]
    HT2 = [A.alloc(BF, 8, 128) for _ in range(2)]; HTr2 = [Reg("ht0"), Reg("ht1")]
    JUNK = A.alloc(BF, D)
    SSQ = A.alloc(F32, 4)
    SQ = A.alloc(F32, D)
    T1 = A.alloc(F32, D); T2 = A.alloc(F32, D)
    S16 = A.alloc(F32, 48)
    AB = A.alloc(F32, 256)
    KB2 = [A.alloc(BF, D) for _ in range(2)]; KBr2 = [Reg("kb0"), Reg("kb1")]
    QB = A.alloc(BF, D); QBr = Reg("qb")
    KTt = [A.alloc(BF, 8, 128) for _ in range(2)]; KTtr = [Reg("ktt0"), Reg("ktt1")]
    Vt = [A.alloc(BF, D) for _ in range(2)]; Vtr = [Reg("vt0"), Reg("vt1")]
    p1r = Reg("p1")

    for half in range(2):
        S.add('pool', lambda e, half=half: e.dma_start(
            out=WKV[:, :, half * 1024:(half + 1) * 1024],
            in_=w_in[:, 1024 + half * 1024:2048 + half * 1024].rearrange("(kc p) n -> p kc n", p=128)),
            dma=WKVr)
    WKVr.w = ('dma', WKVr, WKVr.dcnt)
    S.add('pool', lambda e: e.dma_start(out=WQ, in_=w_in[:, 0:1024].rearrange("(kc p) n -> p kc n", p=128)),
          w=[WQr], dma=WQr)

    kpost_r = Reg("kpost")

    def qk_post(banks, A_, B_, outbf, outr, tag):
        tr = kpost_r
        for hh in range(2):
            S.add('act', lambda e, hh=hh: e.activation(out=SQ[:, hh * 512:(hh + 1) * 512], in_=PS[banks[hh]],
                                                       func=AF.Square), r=[PSr[banks[hh]]], w=[tr])
        S.add('dve', lambda e: e.tensor_reduce(out=S16[:, 0:16], in_=SQ.rearrange("p (g d) -> p g d", d=64),
                                               axis=AX.X, op=ALU.add), r=[tr], w=[tr])
        S.add('act', lambda e: e.activation(out=S16[:, 16:32], in_=S16[:, 0:16], func=AF.Sqrt,
                                            scale=1.0 / 64, bias=1e-6), r=[tr], w=[tr])
        S.add('dve', lambda e: e.reciprocal(out=S16[:, 32:48], in_=S16[:, 16:32]), r=[tr], w=[tr])
        for hh in range(2):
            pv = PS[banks[hh]].rearrange("p (g d) -> p g d", d=64)
            t1 = T1[:, hh * 512:(hh + 1) * 512].rearrange("p (g d) -> p g d", d=64)
            t2 = T2[:, hh * 512:(hh + 1) * 512].rearrange("p (g d) -> p g d", d=64)
            S.add('dve', lambda e, pv=pv, t1=t1: e.tensor_tensor(
                out=t1, in0=pv, in1=A_.unsqueeze(1).broadcast_to([128, 8, 64]), op=ALU.mult),
                r=[PSr[banks[hh]], p1r], w=[tr])
            S.add('dve', lambda e, pv=pv, t2=t2: e.tensor_tensor(
                out=t2[:, :, 0:32], in0=pv[:, :, 32:64], in1=B_[:, 0:32].unsqueeze(1).broadcast_to([128, 8, 32]),
                op=ALU.mult), r=[PSr[banks[hh]], p1r], w=[tr])
            S.add('dve', lambda e, pv=pv, t2=t2: e.tensor_tensor(
                out=t2[:, :, 32:64], in0=pv[:, :, 0:32], in1=B_[:, 32:64].unsqueeze(1).broadcast_to([128, 8, 32]),
                op=ALU.mult), r=[PSr[banks[hh]], p1r], w=[tr])
        S.add('pool', lambda e: e.tensor_tensor(out=T1, in0=T1, in1=T2, op=ALU.add), r=[tr], w=[tr])
        S.add('dve', lambda e: e.tensor_tensor(
            out=outbf.rearrange("p (g d) -> p g d", d=64), in0=T1.rearrange("p (g d) -> p g d", d=64),
            in1=S16[:, 32:48].unsqueeze(2).broadcast_to([128, 16, 64]), op=ALU.mult), r=[tr], w=[outr])

    NT = SEQ // 128
    NOWN = TOWN // 128

    def kbanks(j):
        return [1, 2] if (j < NOWN or j % 2 == 0) else [6, 7]

    def st_N(j):
        xt = XT[j % 2]; xr = XTr[j % 2]; rt = RT[j % 2]; rr = RTr[j % 2]
        S.add('sp', lambda e: e.dma_start(out=xt, in_=xs[j * 128:(j + 1) * 128, :]), w=[xr], dma=xr)
        S.add('sp', lambda e: e.dma_start(out=rt, in_=rope[j * 128:(j + 1) * 128, :]), w=[rr], dma=rr)
        norm_tile(xt, xr, W1, SH1, JUNK, SSQ, xt, [Hb2[j % 2]], [Hr2[j % 2]], tag="n1")

    def st_T(j):
        hb = Hb2[j % 2]; hr = Hr2[j % 2]; ht = HT2[j % 2]; htr = HTr2[j % 2]
        for kc in range(8):
            S.add('pe', lambda e: e.transpose(out=PSB[0][:, kc * 128:(kc + 1) * 128],
                                              in_=hb[:, kc * 128:(kc + 1) * 128], identity=identB),
                  r=[hr, misc], w=[PSr[0]])
        S.add('act', lambda e: e.copy(out=ht.rearrange("p a b -> p (a b)"), in_=PSB[0]), r=[PSr[0]], w=[htr])

    def st_M(j):
        ht = HT2[j % 2]; htr = HTr2[j % 2]
        kb_ = kbanks(j)
        banks = [kb_[0], kb_[1], 3, 4]
        for n in range(4):
            for kc in range(8):
                S.add('pe', lambda e: e.matmul(
                    PS[banks[n]], lhsT=ht[:, kc, :], rhs=WKV[:, kc, n * 512:(n + 1) * 512],
                    start=(kc == 0), stop=(kc == 7)), r=[htr, WKVr], w=[PSr[banks[n]]])
        if j < NOWN:
            for n in range(2):
                for kc in range(8):
                    S.add('pe', lambda e: e.matmul(
                        PS[6 + n], lhsT=ht[:, kc, :], rhs=WQ[:, kc, n * 512:(n + 1) * 512],
                        start=(kc == 0), stop=(kc == 7)), r=[htr, WQr], w=[PSr[6 + n]])

    def st_E(j):
        own = j < NOWN
        rt = RT[j % 2]; rr = RTr[j % 2]
        vt = Vt[j % 2]; vr = Vtr[j % 2]
        for n in range(2):
            S.add('act', lambda e: e.copy(out=vt[:, n * 512:(n + 1) * 512], in_=PS[3 + n]),
                  r=[PSr[3 + n]], w=[vr])
        S.add('sp', lambda e: e.dma_start(
            out=Vd[:, j * 128:(j + 1) * 128, :].rearrange("h t d -> t h d"),
            in_=vt.rearrange("p (h d) -> p h d", d=128)), r=[vr], dma=vr)
        S.add('dve', lambda e: e.tensor_tensor(out=AB[:, 0:64], in0=rt[:, 0:64], in1=KNW, op=ALU.mult),
              r=[rr, CONST], w=[p1r])
        S.add('dve', lambda e: e.tensor_tensor(out=AB[:, 64:128], in0=rt[:, 64:128], in1=KNWS, op=ALU.mult),
              r=[rr, CONST], w=[p1r])
        if own:
            S.add('dve', lambda e: e.tensor_tensor(out=AB[:, 128:192], in0=rt[:, 0:64], in1=QNW, op=ALU.mult),
                  r=[rr, CONST], w=[p1r])
            S.add('dve', lambda e: e.tensor_tensor(out=AB[:, 192:256], in0=rt[:, 64:128], in1=QNWS,
                                                   op=ALU.mult), r=[rr, CONST], w=[p1r])
        qk_post(kbanks(j), AB[:, 0:64], AB[:, 64:128], KB2[j % 2], KBr2[j % 2], "kpost")
        if own:
            qk_post([6, 7], AB[:, 128:192], AB[:, 192:256], QB, QBr, "qpost")

    def st_K(j):
        own = j < NOWN
        kb = KB2[j % 2]; kbr = KBr2[j % 2]
        ktt = KTt[j % 2]; ktr = KTtr[j % 2]
        for h in range(8):
            S.add('pe', lambda e: e.transpose(out=PSB[5][:, h * 128:(h + 1) * 128],
                                              in_=kb[:, h * 128:(h + 1) * 128], identity=identB),
                  r=[kbr, misc], w=[PSr[5]])
        S.add('act', lambda e: e.copy(out=ktt.rearrange("p a b -> p (a b)"), in_=PSB[5]),
              r=[PSr[5]], w=[ktr])
        S.add('sp', lambda e: e.dma_start(out=KTd[:, :, j * 128:(j + 1) * 128], in_=ktt),
              r=[ktr], dma=ktr)
        if own:
            qb_ = QB; qbr = QBr
            for h in range(8):
                S.add('pe', lambda e: e.transpose(out=PSB[5][:, h * 128:(h + 1) * 128],
                                                  in_=qb_[:, h * 128:(h + 1) * 128], identity=identB),
                      r=[qbr, misc], w=[PSr[5]])
            S.add('act', lambda e: e.copy(out=QT[:, :, j * 128:(j + 1) * 128],
                                          in_=PSB[5].rearrange("p (a b) -> p a b", b=128)),
                  r=[PSr[5]], w=[QTr])

    st_N(0); st_N(1); st_T(0); st_M(0)
    for j in range(NT):
        if j + 1 < NT:
            st_T(j + 1)
        st_E(j)
        if j + 1 < NT:
            st_M(j + 1)
        if j + 2 < NT:
            st_N(j + 2)
        st_K(j)

    barrier()
    A.off = PASS_MARK + 2 * 8 * TOWN
    KTb = [A.alloc(BF, SEQ) for _ in range(2)]; KTbr = [Reg("ktb0"), Reg("ktb1")]
    Vb = [A.alloc(BF, 64, 130) for _ in range(2)]; Vbr = [Reg("vb0"), Reg("vb1")]
    PT = [A.alloc(BF, 512) for _ in range(3)]; PTr = [Reg("pt%d" % i) for i in range(3)]
    RC = A.alloc(F32, 8)
    OS = [A.alloc(F32, 128) for _ in range(2)]
    ONB = [A.alloc(BF, 128) for _ in range(2)]
    JKF2 = [A.alloc(F32, 128) for _ in range(2)]
    ppr = [Reg("pp0"), Reg("pp1")]
    Sr = [Reg("s0", excl=True), Reg("s1", excl=True)]
    for b in range(2):
        S.add('pool', lambda e, b=b: e.memset(Vb[b][:, :, 128:129], 1.0), w=[Vbr[b]])

    for h in range(8):
        kt = KTb[h % 2]; ktr = KTbr[h % 2]; vb = Vb[h % 2]; vbr = Vbr[h % 2]
        S.add('sp', lambda e, kt=kt, h=h: e.dma_start(out=kt, in_=KTd[:, h, :]), w=[ktr], dma=ktr)
        S.add('sp', lambda e, vb=vb, h=h: e.dma_start(
            out=vb[:, :, 0:128], in_=Vd[h].rearrange("(kc p) d -> p kc d", p=128)), w=[vbr], dma=vbr)
        def emit_qk(i, kt=kt, ktr=ktr, h=h):
            qt, kc = divmod(i, 64); q0 = qt * 256; sb = i % 2
            for c in range(2):
                S.add('pe', lambda e: e.matmul(
                    PS[2 * sb + c][:, 0:256], lhsT=kt[c * 64:(c + 1) * 64, kc * 128:(kc + 1) * 128],
                    rhs=QT[c * 64:(c + 1) * 64, h, q0:q0 + 256], start=True, stop=True),
                    r=[ktr, QTr], w=[Sr[sb]])

        emit_qk(0)
        emit_qk(1)
        pending = []
        for it in range(512):
            qt, kc = divmod(it, 64); q0 = qt * 256
            sb = it % 2; pt = PT[it % 3]; ptr = PTr[it % 3]
            S.add('act', lambda e: e.activation(
                out=pt.rearrange("p (c n) -> p c n", n=256),
                in_=SXYs[sb].rearrange("p (c n) -> p c n", n=512)[:, :, 0:256],
                func=AF.Exp, scale=0.125), r=[Sr[sb]], w=[ptr])
            if it + 2 < 512:
                emit_qk(it + 2)
            for qb in range(2):
                for c in range(2):
                    bank = 4 + 2 * (qt % 2) + qb
                    S.add('pe', lambda e: e.matmul(
                        PS[bank][:, c * 256:c * 256 + 129], lhsT=pt[:, c * 256 + qb * 128:c * 256 + qb * 128 + 128],
                        rhs=vb[:, kc, 0:129], start=(kc == 0 and c == 0), stop=(kc == 63),
                        skip_group_check=True),
                        r=[ptr, vbr], w=[PSr[bank]])
            if pending and (kc == 12 or it == 511):
                for fn_ in pending:
                    fn_()
                pending = []
            if kc != 63:
                continue
            for qb in range(2):
                bk = 4 + 2 * (qt % 2) + qb
                pr = ppr[qb]; os_ = OS[qb]; onb = ONB[qb]
                rc = RC[:, qb * 4:(qb + 1) * 4]
                S.add('dve', lambda e, bk=bk, rc=rc: e.reciprocal(out=rc[:, 0:1], in_=PS[bk][:, 128:129]),
                      r=[PSr[bk]], w=[pr])
                S.add('dve', lambda e, bk=bk, rc=rc: e.reciprocal(out=rc[:, 1:2], in_=PS[bk][:, 384:385]),
                      r=[PSr[bk]], w=[pr])
                S.add('dve', lambda e, rc=rc: e.tensor_tensor(out=rc[:, 1:2], in0=rc[:, 1:2], in1=NLAM[:, 0:1],
                                                              op=ALU.mult), r=[pr, misc], w=[pr])
                S.add('dve', lambda e, bk=bk, rc=rc, os_=os_: e.tensor_scalar(
                    out=os_, in0=PS[bk][:, 0:128], scalar1=rc[:, 0:1], scalar2=None, op0=ALU.mult),
                    r=[PSr[bk], pr], w=[pr])
                S.add('dve', lambda e, bk=bk, rc=rc, os_=os_: e.scalar_tensor_tensor(
                    out=os_, in0=PS[bk][:, 256:384], scalar=rc[:, 1:2], in1=os_, op0=ALU.mult, op1=ALU.add),
                    r=[PSr[bk], pr], w=[pr])
            for qb in range(2):
                bk = 4 + 2 * (qt % 2) + qb
                pr = ppr[qb]; os_ = OS[qb]; onb = ONB[qb]
                rc = RC[:, qb * 4:(qb + 1) * 4]
                S.add('dve', lambda e, os_=os_: e.tensor_tensor(out=JKF2[qb], in0=os_, in1=os_, op=ALU.mult),
                      r=[pr], w=[pr])
                S.add('dve', lambda e, rc=rc: e.tensor_reduce(out=rc[:, 2:3], in_=JKF2[qb], axis=AX.X, op=ALU.add),
                      r=[pr], w=[pr])
                S.add('dve', lambda e, rc=rc: e.tensor_scalar(out=rc[:, 2:3], in0=rc[:, 2:3], scalar1=1.0 / 128,
                                                              scalar2=1e-5, op0=ALU.mult, op1=ALU.add),
                      r=[pr], w=[pr])
                S.add('pool', lambda e, rc=rc: e.tensor_tensor(out=rc[:, 3:4], in0=rc[:, 2:3],
                                                               in1=NLAM[:, 1:2], op=ALU.pow), r=[pr, misc], w=[pr])
                S.add('dve', lambda e, os_=os_, rc=rc, onb=onb: e.scalar_tensor_tensor(
                    out=onb, in0=os_, scalar=rc[:, 3:4], in1=SUBW, op0=ALU.mult, op1=ALU.mult),
                    r=[pr, misc], w=[pr])
                def fin_post(onb=onb, pr=pr, h=h, q0=q0, qb=qb, tbk=4 + 2 * (qt % 2)):
                    S.add('pe', lambda e: e.transpose(out=PSB[tbk][:, 0:128], in_=onb, identity=identB),
                          r=[pr, misc], w=[PSr[tbk]])
                    g = (q0 + qb * 128) // 512
                    S.add('dve', lambda e: e.tensor_copy(
                        out=OT[:, h, q0 + qb * 128:q0 + qb * 128 + 128], in_=PSB[tbk][:, 0:128]),
                        r=[PSr[tbk]], w=[OTr[g]])
                pending.append(fin_post)
            if it == 511:
                for fn_ in pending:
                    fn_()
                pending = []

    if DEBUG == 'attn':
        barrier()
        S.add('sp', lambda e: e.dma_start(out=dbg, in_=OT.rearrange("p a b -> p (a b)")), r=OTr, dma=misc)
        S.wait_regs('sp', [misc])
        return finish(nc, S, stack)

    barrier()
    A.off = PASS_MARK
    RING = [A.alloc(BF, 8, 1024) for _ in range(3)]; RINGr = [Reg("ring%d" % i) for i in range(3)]
    ring_i = [0]

    def ring_load(src2d):
        i = ring_i[0] % len(RING); ring_i[0] += 1
        S.add('pool', lambda e, i=i: e.dma_start(out=RING[i], in_=src2d.rearrange("(kc p) n -> p kc n", p=128)),
              w=[RINGr[i]], dma=RINGr[i])
        return RING[i], RINGr[i]

    XG = [A.alloc(F32, D) for _ in range(2)]; XGr = [Reg("xg0"), Reg("xg1")]
    Hb = A.alloc(BF, D); Hr = Reg("h4")
    HTG = A.alloc(BF, 8, 514); HTGr = Reg("htg")
    JUNK = A.alloc(BF, D); SSQ = A.alloc(F32, 4); TMP = A.alloc(F32, D)
    CCs = A.alloc(F32, 512); CCr = Reg("ccs")
    ZP = A.alloc(BF, 8, 514); ZPr = Reg("zp")
    ZH = A.alloc(F32, 32)
    CT = A.alloc(F32, 512)
    U = A.alloc(BF, 8, 512); Ur = Reg("u")
    SG = A.alloc(BF, 512); SGr = Reg("sg")
    MC = A.alloc(BF, 8, 512); MCr = Reg("mc")
    M = U; Mr = Ur
    TMP2 = A.alloc(F32, D)
    X1 = A.alloc(F32, D); X1r = Reg("x1")
    H2 = A.alloc(F32, D); H2r = Reg("h2")
    H2B = A.alloc(BF, D); H2Br = Reg("h2b")
    H2T32 = TMP2.rearrange("p (a b) -> p a b", b=128); H2Tr = Reg("h2t32")
    LG = A.alloc(F32, NE); MX = A.alloc(F32, 8); EX = A.alloc(F32, NE); MK = A.alloc(F32, NE)
    SM = A.alloc(F32, 4)
    rr4 = Reg("r4")
    p4 = Reg("p4")

    COL = dict(cb=3072, cc=4096, cx=5120, ga=6144, gc=7168)
    for g in range(4):
        q0 = g * 512
        for tt in range(5):
            xt = XG[tt % 2]; xr = XGr[tt % 2]
            if tt < 4:
                npart = 128
                S.add('sp', lambda e, xt=xt, q0=q0, tt=tt: e.dma_start(
                    out=xt, in_=xs[q0 + tt * 128:q0 + (tt + 1) * 128, :]), w=[xr], dma=xr)
            else:
                npart = 2
                S.add('sp', lambda e, xt=xt, g=g, q0=q0: e.dma_start(
                    out=xt[0:1, :], in_=(xs[q0 - 1:q0, :] if g > 0 else xh[0:1, :])), w=[xr], dma=xr)
                S.add('sp', lambda e, xt=xt, g=g, q0=q0: e.dma_start(
                    out=xt[1:2, :], in_=(xs[q0 + 512:q0 + 513, :] if g < 3 else xh[1:2, :])), w=[xr], dma=xr)
            norm_tile(xt, xr, W1, SH1, JUNK, SSQ, TMP, [Hb], [Hr], npart=npart, tag="n4")
            for kc in range(8):
                S.add('pe', lambda e, kc=kc, npart=npart: e.transpose(
                    out=PSB[0][:, kc * 128:kc * 128 + npart], in_=Hb[0:npart, kc * 128:(kc + 1) * 128],
                    identity=identB[0:npart, 0:npart]), r=[Hr, misc], w=[PSr[0]])
            c0 = tt * 128
            S.add('act', lambda e, c0=c0, npart=npart: e.copy(
                out=HTG[:, :, c0:c0 + npart], in_=PSB[0].rearrange("p (a b) -> p a b", b=128)[:, :, 0:npart]),
                r=[PSr[0]], w=[HTGr])
        wcc, wccr = ring_load(w_in[:, COL['cc']:COL['cc'] + 1024])
        wcx, wcxr = ring_load(w_in[:, COL['cx']:COL['cx'] + 1024])
        for fc in range(8):
            for (wt, wr, bank) in ((wcc, wccr, 1), (wcx, wcxr, 2)):
                for kc in range(8):
                    S.add('pe', lambda e, wt=wt, bank=bank, kc=kc, fc=fc: e.matmul(
                        PS[bank], lhsT=wt[:, kc, fc * 128:(fc + 1) * 128], rhs=HTG[:, kc, 0:512],
                        start=(kc == 0), stop=(kc == 7)), r=[wr, HTGr], w=[PSr[bank]])
            for wi, (wt, wr) in enumerate(((wcc, wccr), (wcx, wcxr))):
                for kc in range(8):
                    S.add('pe', lambda e, wt=wt, kc=kc, fc=fc, wi=wi: e.matmul(
                        PS[3][:, (fc * 2 + wi) * 2:(fc * 2 + wi) * 2 + 2], lhsT=wt[:, kc, fc * 128:(fc + 1) * 128],
                        rhs=HTG[:, kc, 512:514], start=(kc == 0), stop=(kc == 7)), r=[wr, HTGr], w=[PSr[3]])
            S.add('act', lambda e: e.copy(out=CCs, in_=PS[1]), r=[PSr[1]], w=[CCr])
            S.add('dve', lambda e, fc=fc: e.tensor_tensor(out=ZP[:, fc, 1:513], in0=PS[2], in1=CCs, op=ALU.mult),
                  r=[PSr[2], CCr], w=[ZPr])
        S.add('act', lambda e: e.copy(out=ZH, in_=PS[3][:, 0:32]), r=[PSr[3]], w=[p4])
        zh4 = ZH.rearrange("p (f w c) -> p f w c", w=2, c=2)
        for ci, col in ((0, 0), (1, 513)):
            S.add('dve', lambda e, ci=ci, col=col: e.tensor_tensor(
                out=ZP[:, :, col:col + 1], in0=zh4[:, :, 0, ci:ci + 1], in1=zh4[:, :, 1, ci:ci + 1], op=ALU.mult),
                r=[p4], w=[ZPr])
        if g == 0:
            S.add('dve', lambda e: e.tensor_scalar(out=ZP[:, :, 0:1], in0=ZP[:, :, 0:1], scalar1=HM[:, 0:1],
                                                   scalar2=None, op0=ALU.mult), r=[CONST], w=[ZPr])
        if g == 3:
            S.add('dve', lambda e: e.tensor_scalar(out=ZP[:, :, 513:514], in0=ZP[:, :, 513:514],
                                                   scalar1=HM[:, 1:2], scalar2=None, op0=ALU.mult),
                  r=[CONST], w=[ZPr])
        wcb, wcbr = ring_load(w_in[:, COL['cb']:COL['cb'] + 1024])
        for fc in range(8):
            bank = 1 + fc % 2
            for kc in range(8):
                S.add('pe', lambda e, bank=bank, kc=kc, fc=fc: e.matmul(
                    PS[bank], lhsT=wcb[:, kc, fc * 128:(fc + 1) * 128], rhs=HTG[:, kc, 0:512],
                    start=(kc == 0), stop=(kc == 7)), r=[wcbr, HTGr], w=[PSr[bank]])
            S.add('dve', lambda e, fc=fc: e.tensor_scalar(out=CT, in0=ZP[:, fc, 0:512], scalar1=CW[:, 0, fc:fc + 1],
                                                          scalar2=None, op0=ALU.mult), r=[ZPr, CONST], w=[p4])
            S.add('dve', lambda e, fc=fc: e.scalar_tensor_tensor(out=CT, in0=ZP[:, fc, 1:513], scalar=CW[:, 1, fc:fc + 1],
                                                                 in1=CT, op0=ALU.mult, op1=ALU.add),
                  r=[ZPr, CONST, p4], w=[p4])
            S.add('dve', lambda e, fc=fc: e.scalar_tensor_tensor(out=CT, in0=ZP[:, fc, 2:514], scalar=CW[:, 2, fc:fc + 1],
                                                                 in1=CT, op0=ALU.mult, op1=ALU.add),
                  r=[ZPr, CONST, p4], w=[p4])
            S.add('dve', lambda e, fc=fc, bank=bank: e.tensor_tensor(out=U[:, fc, :], in0=PS[bank], in1=CT,
                                                                      op=ALU.mult), r=[PSr[bank], p4], w=[Ur])
        wco, wcor = ring_load(w_conv_o[:, :])
        wgc, wgcr = ring_load(w_in[:, COL['gc']:COL['gc'] + 1024])
        for dc in range(8):
            ba = 1 + 2 * (dc % 2); bb = ba + 1
            for kc in range(8):
                S.add('pe', lambda e, ba=ba, kc=kc, dc=dc: e.matmul(
                    PS[ba], lhsT=wco[:, kc, dc * 128:(dc + 1) * 128], rhs=U[:, kc, :],
                    start=(kc == 0), stop=(kc == 7)), r=[wcor, Ur], w=[PSr[ba]])
            for kc in range(8):
                S.add('pe', lambda e, bb=bb, kc=kc, dc=dc: e.matmul(
                    PS[bb], lhsT=wgc[:, kc, dc * 128:(dc + 1) * 128], rhs=HTG[:, kc, 0:512],
                    start=(kc == 0), stop=(kc == 7)), r=[wgcr, HTGr], w=[PSr[bb]])
            S.add('act', lambda e, bb=bb: e.activation(out=SG, in_=PS[bb], func=AF.Sigmoid), r=[PSr[bb]], w=[SGr])
            S.add('dve', lambda e, ba=ba, dc=dc: e.tensor_tensor(out=MC[:, dc, :], in0=PS[ba], in1=SG, op=ALU.mult),
                  r=[PSr[ba], SGr], w=[MCr])
        wao, waor = ring_load(w_attn_o[:, :])
        wga, wgar = ring_load(w_in[:, COL['ga']:COL['ga'] + 1024])
        for dc in range(8):
            ba = 1 + 2 * (dc % 2); bb = ba + 1
            for kc in range(8):
                S.add('pe', lambda e, ba=ba, kc=kc, dc=dc: e.matmul(
                    PS[ba], lhsT=wao[:, kc, dc * 128:(dc + 1) * 128], rhs=OT[:, kc, q0:q0 + 512],
                    start=(kc == 0), stop=(kc == 7)), r=[waor, OTr[g]], w=[PSr[ba]])
            for kc in range(8):
                S.add('pe', lambda e, bb=bb, kc=kc, dc=dc: e.matmul(
                    PS[bb], lhsT=wga[:, kc, dc * 128:(dc + 1) * 128], rhs=HTG[:, kc, 0:512],
                    start=(kc == 0), stop=(kc == 7)), r=[wgar, HTGr], w=[PSr[bb]])
            S.add('act', lambda e, bb=bb: e.activation(out=SG, in_=PS[bb], func=AF.Sigmoid), r=[PSr[bb]], w=[SGr])
            S.add('dve', lambda e, ba=ba: e.tensor_tensor(out=CT, in0=PS[ba], in1=SG, op=ALU.mult),
                  r=[PSr[ba], SGr], w=[p4])
            S.add('pool', lambda e, dc=dc: e.tensor_tensor(out=M[:, dc, :], in0=CT, in1=MC[:, dc, :], op=ALU.add),
                  r=[p4, MCr], w=[Mr])
        if DEBUG == 'p4' and g == 3:
            S.add('sp', lambda e: e.dma_start(out=dbg[:, 0, :], in_=ZP.rearrange("p a b -> p (a b)")), r=[ZPr], dma=misc)
            S.add('sp', lambda e: e.dma_start(out=dbg[:, 1, 0:4096], in_=MC.rearrange("p a b -> p (a b)")), r=[MCr], dma=misc)
            S.add('sp', lambda e: e.dma_start(out=dbg[:, 2, 0:4096], in_=M.rearrange("p a b -> p (a b)")), r=[Mr], dma=misc)
            S.add('sp', lambda e: e.dma_start(out=dbg[:, 3, :], in_=HTG.rearrange("p a b -> p (a b)")), r=[HTGr], dma=misc)
            S.wait_regs('sp', [misc])
            return finish(nc, S, stack)
        wo, wor = ring_load(w_out[:, :])
        for tt in range(4):
            tile_i = g * 4 + tt
            xt = XG[tt % 2]; xr = XGr[tt % 2]
            S.add('sp', lambda e, xt=xt, q0=q0, tt=tt: e.dma_start(
                out=xt, in_=xs[q0 + tt * 128:q0 + (tt + 1) * 128, :]), w=[xr], dma=xr)
            for half in range(2):
                bank = 5 + half
                for kc in range(8):
                    S.add('pe', lambda e, bank=bank, kc=kc, tt=tt, half=half: e.matmul(
                        PS[bank], lhsT=M[:, kc, tt * 128:(tt + 1) * 128], rhs=wo[:, kc, half * 512:(half + 1) * 512],
                        start=(kc == 0), stop=(kc == 7)), r=[Mr, wor], w=[PSr[bank]])
                S.add('dve', lambda e, bank=bank, half=half: e.tensor_tensor(
                    out=TMP2[:, half * 512:(half + 1) * 512], in0=PS[bank], in1=G1[:, half * 512:(half + 1) * 512],
                    op=ALU.mult), r=[PSr[bank], misc], w=[H2Tr])
            S.add('pool', lambda e, xt=xt: e.tensor_tensor(out=X1, in0=TMP2, in1=xt, op=ALU.add),
                  r=[H2Tr, xr], w=[X1r])
            x1dst = y if DEBUG == 'x1' else X1d
            S.add('sp', lambda e, tile_i=tile_i, x1dst=x1dst: e.dma_start(
                out=x1dst[tile_i * 128:(tile_i + 1) * 128, :], in_=X1), r=[X1r], dma=X1r)
            norm_tile(X1, X1r, W2, SH2, JUNK, SSQ, TMP, [H2, H2B], [H2r, H2Br], tag="n2")
            for kc in range(8):
                bank = 1 + kc // 4
                S.add('pe', lambda e, kc=kc, bank=bank: e.transpose(
                    out=PS[bank][:, (kc % 4) * 128:(kc % 4 + 1) * 128], in_=H2[:, kc * 128:(kc + 1) * 128],
                    identity=identF), r=[H2r, CONST], w=[PSr[bank]])
            for b2 in range(2):
                S.add('act', lambda e, b2=b2: e.copy(out=H2T32[:, b2 * 4:(b2 + 1) * 4, :].rearrange("p a b -> p (a b)"),
                                                     in_=PS[1 + b2]), r=[PSr[1 + b2]], w=[H2Tr])
            for kc in range(8):
                S.add('pe', lambda e, kc=kc: e.matmul(PS[3][:, 0:NE], lhsT=H2T32[:, kc, :], rhs=WRT[:, kc, :],
                                                      start=(kc == 0), stop=(kc == 7)),
                      r=[H2Tr, CONST], w=[PSr[3]])
            S.add('dve', lambda e: e.tensor_tensor(out=LG, in0=PS[3][:, 0:NE], in1=BR, op=ALU.add),
                  r=[PSr[3], CONST], w=[rr4])
            S.add('dve', lambda e: e.max(out=MX, in_=LG), r=[rr4], w=[rr4])
            S.add('dve', lambda e: e.tensor_scalar(out=MK, in0=LG, scalar1=MX[:, 3:4], scalar2=None, op0=ALU.is_ge),
                  r=[rr4], w=[rr4])
            if MOE_GATHER:
                S.add('sp', lambda e, tile_i=tile_i: e.dma_start(out=H2d[tile_i * 128:(tile_i + 1) * 128, :],
                                                                 in_=H2B), r=[H2Br], dma=H2Br)
            S.add('dve', lambda e: e.tensor_scalar(out=SM[:, 0:1], in0=MX[:, 0:1], scalar1=-1.0, scalar2=None,
                                                   op0=ALU.mult), r=[rr4], w=[rr4])
            S.add('act', lambda e: e.activation(out=EX, in_=LG, func=AF.Exp, bias=SM[:, 0:1]), r=[rr4], w=[rr4])
            S.add('dve', lambda e: e.tensor_tensor(out=EX, in0=EX, in1=MK, op=ALU.mult), r=[rr4], w=[rr4])
            S.add('dve', lambda e: e.tensor_reduce(out=SM[:, 1:2], in_=EX, axis=AX.X, op=ALU.add), r=[rr4], w=[rr4])
            S.add('dve', lambda e: e.reciprocal(out=SM[:, 2:3], in_=SM[:, 1:2]), r=[rr4], w=[rr4])
            S.add('dve', lambda e, tile_i=tile_i: e.tensor_scalar(out=WG[:, tile_i, :], in0=EX, scalar1=SM[:, 2:3],
                                                                   scalar2=None, op0=ALU.mult), r=[rr4], w=[WGr])
            for kc in range(8):
                S.add('pe', lambda e, kc=kc: e.transpose(out=PSB[7][:, kc * 128:(kc + 1) * 128],
                                                         in_=H2B[:, kc * 128:(kc + 1) * 128], identity=identB),
                      r=[H2Br, misc], w=[PSr[7]])
            S.add('act', lambda e, q0=q0, tt=tt: e.copy(
                out=OT[:, :, q0 + tt * 128:q0 + (tt + 1) * 128], in_=PSB[7].rearrange("p (a b) -> p a b", b=128)),
                r=[PSr[7]], w=[OTr[g]])

    if DEBUG == 'x1':
        barrier()
        S.add('sp', lambda e: e.dma_start(out=dbg.rearrange("(t p) e -> p t e", p=128), in_=WG), r=[WGr], dma=misc)
        S.wait_regs('sp', [misc])
        return finish(nc, S, stack)

    barrier()
    A.off = P5_MARK
    H2T = OT
    ACC = A.alloc(F32, 16, D); ACCr = [Reg("acc%d" % i) for i in range(16)]
    BGg = A.alloc(F32, 8, NE); BGu = A.alloc(F32, 8, NE)
    RK = A.alloc(F32, 16, NE); rkr = Reg("rk")
    IOTA = A.alloc(F32, CAP)
    H2TOK = OT.rearrange("p a b -> p (a b)").rearrange("p (t d) -> p t d", d=D)
    LOOP_MARK = A.off
    BGrow = A.alloc(F32, 2048)
    BD = A.alloc(F32, D)
    WGT = A.alloc(F32, 16, 128)
    p5 = Reg("p5")
    S.add('sp', lambda e: e.dma_start(out=BGrow[0:NE, :], in_=b_gate_up), w=[p5], dma=p5)
    S.add('sp', lambda e: e.dma_start(out=BD[0:NE, :], in_=b_down), dma=p5)
    p5.w = ('dma', p5, p5.dcnt)
    bg3 = BGrow.rearrange("p (f m t) -> p f m t", m=128, t=2)
    for fc in range(8):
        S.add('pe', lambda e, fc=fc: e.transpose(out=PS[0][:, fc * NE:(fc + 1) * NE],
                                                 in_=bg3[0:NE, fc, :, 0],
                                                 identity=identF[0:NE, 0:NE]), r=[p5, CONST], w=[PSr[0]])
        S.add('pe', lambda e, fc=fc: e.transpose(out=PS[0][:, 256 + fc * NE:256 + (fc + 1) * NE],
                                                 in_=bg3[0:NE, fc, :, 1],
                                                 identity=identF[0:NE, 0:NE]), r=[p5, CONST], w=[PSr[0]])
    bgr = Reg("bg")
    S.add('dve', lambda e: e.tensor_copy(out=BGg.rearrange("p a b -> p (a b)"), in_=PS[0][:, 0:256]),
          r=[PSr[0]], w=[bgr])
    S.add('dve', lambda e: e.tensor_scalar(out=BGu.rearrange("p a b -> p (a b)"), in0=PS[0][:, 256:512],
                                           scalar1=1.0, scalar2=None, op0=ALU.add), r=[PSr[0]], w=[bgr])
    for ti in range(16):
        S.add('pe', lambda e, ti=ti: e.transpose(out=PS[1][0:NE, 0:128], in_=WG[:, ti, :], identity=identF),
              r=[WGr, CONST], w=[PSr[1]])
        S.add('dve', lambda e, ti=ti: e.tensor_copy(out=WGT[0:NE, ti, :], in_=PS[1][0:NE, 0:128]),
              r=[PSr[1]], w=[bgr])
        for half in range(2):
            S.add('pe', lambda e, ti=ti, half=half: e.matmul(
                PS[2 + half], lhsT=WGT[0:NE, ti, :], rhs=BD[0:NE, half * 512:(half + 1) * 512],
                start=True, stop=True), r=[bgr, p5], w=[PSr[2 + half]])
            S.add('act', lambda e, ti=ti, half=half: e.copy(out=ACC[:, ti, half * 512:(half + 1) * 512],
                                                            in_=PS[2 + half]), r=[PSr[2 + half]], w=[ACCr[ti]])
    def moe_dense():
        barrier()
        A.off = LOOP_MARK
        RING5 = [A.alloc(BF, 8, 1024) for _ in range(3)]; RING5r = [Reg("r5_%d" % i) for i in range(3)]
        r5_i = [0]

        def ring5_load(src2d):
            i = r5_i[0] % 3; r5_i[0] += 1
            S.add('pool', lambda e, i=i: e.dma_start(out=RING5[i], in_=src2d.rearrange("(kc p) n -> p kc n", p=128)),
                  w=[RING5r[i]], dma=RING5r[i])
            return RING5[i], RING5r[i]

        AT = [A.alloc(BF, 8, 512) for _ in range(2)]; ATr = [Reg("at0"), Reg("at1")]
        GS = [A.alloc(F32, 512) for _ in range(2)]; GSr = [Reg("gs0"), Reg("gs1")]
        SGM = [A.alloc(F32, 512) for _ in range(2)]
        U1 = [A.alloc(F32, 512) for _ in range(2)]

        it5 = [0]
        for ex in range(NE):
            wgA, wgAr = ring5_load(w_gate_up[ex, :, 0:1024])
            wgB, wgBr = ring5_load(w_gate_up[ex, :, 1024:2048])
            wd, wdr = ring5_load(w_down[ex, :, :])
            for tg in range(4):
                at = AT[tg % 2]; atr = ATr[tg % 2]
                for fc in range(8):
                    wt, wr = (wgA, wgAr) if fc < 4 else (wgB, wgBr)
                    wv = wt.rearrange("p k (f m t) -> p k f m t", m=128, t=2)
                    k = it5[0] % 2; it5[0] += 1
                    bg_, bu_ = 4 * k, 4 * k + 1
                    for (bank, off) in ((bg_, 0), (bu_, 1)):
                        for kc in range(8):
                            S.add('pe', lambda e, bank=bank, off=off, wv=wv, kc=kc, fc=fc, tg=tg: e.matmul(
                                PS[bank], lhsT=wv[:, kc, fc % 4, :, off],
                                rhs=H2T[:, kc, tg * 512:(tg + 1) * 512], start=(kc == 0), stop=(kc == 7)),
                                r=[wr, OTr[tg]], w=[PSr[bank]])
                    gs = GS[k]; gsr = GSr[k]; sg = SGM[k]; u1 = U1[k]
                    S.add('dve', lambda e, gs=gs, bg_=bg_, fc=fc, ex=ex: e.tensor_scalar(
                        out=gs, in0=PS[bg_], scalar1=BGg[:, fc, ex:ex + 1], scalar2=7.0, op0=ALU.add, op1=ALU.min),
                        r=[PSr[bg_], bgr], w=[gsr])
                    S.add('act', lambda e, gs=gs, sg=sg: e.activation(out=sg, in_=gs, func=AF.Sigmoid, scale=1.702),
                          r=[gsr], w=[gsr])
                    S.add('dve', lambda e, u1=u1, bu_=bu_, fc=fc, ex=ex: e.tensor_scalar(
                        out=u1, in0=PS[bu_], scalar1=BGu[:, fc, ex:ex + 1], scalar2=8.0, op0=ALU.add, op1=ALU.min),
                        r=[PSr[bu_], bgr], w=[gsr])
                    S.add('dve', lambda e, gs=gs, sg=sg: e.tensor_tensor(out=gs, in0=gs, in1=sg, op=ALU.mult),
                          r=[gsr], w=[gsr])
                    S.add('dve', lambda e, u1=u1, gs=gs, at=at, fc=fc: e.scalar_tensor_tensor(
                        out=at[:, fc, :], in0=u1, scalar=-6.0, in1=gs, op0=ALU.max, op1=ALU.mult),
                        r=[gsr], w=[atr])
                for tt in range(4):
                    ti = tg * 4 + tt
                    for half in range(2):
                        bank = 2 + half
                        for fc in range(8):
                            S.add('pe', lambda e, bank=bank, fc=fc, at=at, tt=tt, half=half: e.matmul(
                                PS[bank], lhsT=at[:, fc, tt * 128:(tt + 1) * 128],
                                rhs=wd[:, fc, half * 512:(half + 1) * 512], start=(fc == 0), stop=(fc == 7)),
                                r=[atr, wdr], w=[PSr[bank]])
                        S.add('dve', lambda e, bank=bank, ti=ti, half=half, ex=ex: e.scalar_tensor_tensor(
                            out=ACC[:, ti, half * 512:(half + 1) * 512], in0=PS[bank], scalar=WG[:, ti, ex:ex + 1],
                            in1=ACC[:, ti, half * 512:(half + 1) * 512], op0=ALU.mult, op1=ALU.add),
                            r=[PSr[bank], WGr], w=[ACCr[ti]])


    if MOE_GATHER:
        TRIF = A.alloc(F32, 128); TRIB = A.alloc(BF, 128); ONESB = A.alloc(BF, 128)
        MKA = A.alloc(BF, 16, NE)
        S.add('dve', lambda e: e.tensor_scalar(out=MKA.rearrange("p a b -> p (a b)"),
                                               in0=WG.rearrange("p a b -> p (a b)"), scalar1=0.0, scalar2=None,
                                               op0=ALU.is_gt), r=[WGr], w=[bgr])
        S.add('sp', lambda e: e.dma_start(out=TRIF, in_=tri), w=[p5], dma=p5)
        S.add('sp', lambda e: e.dma_start(out=IOTA, in_=iota), w=[p5], dma=p5)
        S.add('sp', lambda e: e.dma_start(out=H2TOK, in_=H2d.rearrange("(t p) d -> p t d", p=128)),
              w=OTr + [p5], dma=p5)
        S.add('dve', lambda e: e.tensor_copy(out=TRIB, in_=TRIF), r=[p5], w=[bgr])
        S.add('pool', lambda e: e.memset(ONESB, 1.0), w=[bgr])
        for ti in range(16):
            b = 4 + ti % 2
            for tj in range(ti):
                S.add('pe', lambda e: e.matmul(PS[b][:, 0:NE], lhsT=ONESB, rhs=MKA[:, tj, :],
                                               start=(tj == 0), stop=False), r=[bgr, WGr], w=[PSr[b]])
            S.add('pe', lambda e: e.matmul(PS[b][:, 0:NE], lhsT=TRIB, rhs=MKA[:, ti, :],
                                           start=(ti == 0), stop=True), r=[bgr, WGr], w=[PSr[b]])
            S.add('dve', lambda e: e.scalar_tensor_tensor(out=RK[:, ti, :], in0=PS[b][:, 0:NE], scalar=1.0,
                                                          in1=MKA[:, ti, :], op0=ALU.add, op1=ALU.mult),
                  r=[PSr[b], WGr], w=[rkr])
            S.add('dve', lambda e: e.tensor_scalar(out=RK[:, ti, :], in0=RK[:, ti, :], scalar1=-1.0, scalar2=None,
                                                   op0=ALU.add), r=[rkr], w=[rkr])
        barrier()
        A.off = LOOP_MARK
        NR = 4
        RING5 = [A.alloc(BF, 8, 512) for _ in range(NR)]; RING5r = [Reg("r5_%d" % i) for i in range(NR)]
        r5_i = [0]

        def ring5_load(src2d):
            i = r5_i[0] % NR; r5_i[0] += 1
            S.add('pool', lambda e: e.dma_start(out=RING5[i], in_=src2d.rearrange("(kc p) n -> p kc n", p=128)),
                  w=[RING5r[i]], dma=RING5r[i])
            return RING5[i], RING5r[i]

        SEL = A.alloc(BF, 16, CAP); SELr = Reg("sel")
        XET = A.alloc(BF, 8, CAP); XEr = Reg("xet")
        YE = XET.rearrange("p a b -> p (a b)").rearrange("p (c d) -> p c d", d=D)
        AT = A.alloc(BF, 8, CAP); ATr = Reg("at")
        ST = [A.alloc(BF, CAP) for _ in range(2)]; STr = [Reg("st0"), Reg("st1")]
        GS = [A.alloc(F32, CAP) for _ in range(2)]; GSr = [Reg("gs0"), Reg("gs1")]
        SGM = [A.alloc(F32, CAP) for _ in range(2)]
        U1 = [A.alloc(F32, CAP) for _ in range(2)]
        NCC = CAP // 128
        for ex in range(NE):
            for ti in range(16):
                S.add('dve', lambda e: e.tensor_scalar(out=SEL[:, ti, :], in0=IOTA, scalar1=RK[:, ti, ex:ex + 1],
                                                       scalar2=None, op0=ALU.is_equal), r=[rkr, p5], w=[SELr])
            for fc in range(8):
                b = fc % 2
                for ti in range(16):
                    S.add('pe', lambda e: e.matmul(PS[b][:, 0:CAP], lhsT=H2TOK[:, ti, fc * 128:(fc + 1) * 128],
                                                   rhs=SEL[:, ti, :], start=(ti == 0), stop=(ti == 15)),
                          r=[p5, SELr], w=[PSr[b]])
                S.add('act', lambda e: e.copy(out=XET[:, fc, :], in_=PS[b][:, 0:CAP]), r=[PSr[b]], w=[XEr])
            for fc in range(8):
                if fc % 2 == 0:
                    wt, wr = ring5_load(w_gate_up[ex, :, fc * 256:fc * 256 + 512])
                    wv = wt.rearrange("p k (f m t) -> p k f m t", m=128, t=2)
                k = fc % 2
                bg_, bu_ = 2 + 2 * k, 3 + 2 * k
                for (bank, off) in ((bg_, 0), (bu_, 1)):
                    for kc in range(8):
                        S.add('pe', lambda e: e.matmul(PS[bank][:, 0:CAP], lhsT=wv[:, kc, fc % 2, :, off],
                                                       rhs=XET[:, kc, :], start=(kc == 0), stop=(kc == 7)),
                              r=[wr, XEr], w=[PSr[bank]])
                gs = GS[k]; gsr = GSr[k]; sg = SGM[k]; u1 = U1[k]
                S.add('dve', lambda e: e.tensor_scalar(out=gs, in0=PS[bg_][:, 0:CAP], scalar1=BGg[:, fc, ex:ex + 1],
                                                       scalar2=7.0, op0=ALU.add, op1=ALU.min),
                      r=[PSr[bg_], bgr], w=[gsr])
                S.add('act', lambda e: e.activation(out=sg, in_=gs, func=AF.Sigmoid, scale=1.702),
                      r=[gsr], w=[gsr])
                S.add('dve', lambda e: e.tensor_scalar(out=u1, in0=PS[bu_][:, 0:CAP], scalar1=BGu[:, fc, ex:ex + 1],
                                                       scalar2=8.0, op0=ALU.add, op1=ALU.min),
                      r=[PSr[bu_], bgr], w=[gsr])
                S.add('dve', lambda e: e.tensor_tensor(out=gs, in0=gs, in1=sg, op=ALU.mult), r=[gsr], w=[gsr])
                S.add('dve', lambda e: e.scalar_tensor_tensor(out=AT[:, fc, :], in0=u1, scalar=-6.0, in1=gs,
                                                              op0=ALU.max, op1=ALU.mult), r=[gsr], w=[ATr])
            for half in range(2):
                wd, wdr = ring5_load(w_down[ex, :, half * 512:(half + 1) * 512])
                for cc in range(NCC):
                    b = 6 + (half * NCC + cc) % 2
                    for fc in range(8):
                        S.add('pe', lambda e: e.matmul(PS[b], lhsT=AT[:, fc, cc * 128:(cc + 1) * 128],
                                                       rhs=wd[:, fc, :], start=(fc == 0), stop=(fc == 7)),
                              r=[ATr, wdr], w=[PSr[b]])
                    S.add('act', lambda e: e.copy(out=YE[:, cc, half * 512:(half + 1) * 512], in_=PS[b]),
                          r=[PSr[b]], w=[XEr])
            for ti in range(16):
                tb = ti % 2
                st = ST[tb]; str_ = STr[tb]
                for cc in range(NCC):
                    S.add('pe', lambda e: e.transpose(out=PSB[tb][:, cc * 128:(cc + 1) * 128],
                                                      in_=SEL[:, ti, cc * 128:(cc + 1) * 128], identity=identB),
                          r=[SELr, misc], w=[PSr[tb]])
                S.add('act', lambda e: e.copy(out=st, in_=PSB[tb][:, 0:CAP]), r=[PSr[tb]], w=[str_])
                for half in range(2):
                    bank = 2 + 2 * tb + half
                    for cc in range(NCC):
                        S.add('pe', lambda e: e.matmul(PS[bank], lhsT=st[:, cc * 128:(cc + 1) * 128],
                                                       rhs=YE[:, cc, half * 512:(half + 1) * 512],
                                                       start=(cc == 0), stop=(cc == NCC - 1)),
                              r=[str_, XEr], w=[PSr[bank]])
                    S.add('dve', lambda e: e.scalar_tensor_tensor(
                        out=ACC[:, ti, half * 512:(half + 1) * 512], in0=PS[bank], scalar=WG[:, ti, ex:ex + 1],
                        in1=ACC[:, ti, half * 512:(half + 1) * 512], op0=ALU.mult, op1=ALU.add),
                        r=[PSr[bank], WGr], w=[ACCr[ti]])
    else:
        moe_dense()

    barrier()
    A.off = LOOP_MARK
    XF = [A.alloc(F32, D) for _ in range(2)]; XFr = [Reg("xf0"), Reg("xf1")]
    for ti in range(16):
        xf = XF[ti % 2]; xfr = XFr[ti % 2]
        S.add('sp', lambda e, xf=xf, ti=ti: e.dma_start(out=xf, in_=X1d[ti * 128:(ti + 1) * 128, :]),
              w=[xfr], dma=xfr)
        S.add('dve', lambda e, ti=ti: e.tensor_tensor(out=ACC[:, ti, :], in0=ACC[:, ti, :], in1=G2, op=ALU.mult),
              r=[CONST, misc], w=[ACCr[ti]])
        S.add('pool', lambda e, ti=ti, xf=xf: e.tensor_tensor(out=xf, in0=ACC[:, ti, :], in1=xf, op=ALU.add),
              r=[ACCr[ti]], w=[xfr])
        S.add('sp', lambda e, xf=xf, ti=ti: e.dma_start(out=y[ti * 128:(ti + 1) * 128, :], in_=xf),
              r=[xfr], dma=xfr)
    S.wait_regs('sp', XFr)
    return finish(nc, S, stack)


def finish(nc, S, stack):
    S.prepare(nc, stack)
    with nc.Block() as block:
        @block.tensor
        def _(e):
            S.run('pe', e)

        @block.scalar
        def _(e):
            S.run('act', e)

        @block.vector
        def _(e):
            S.run('dve', e)

        @block.gpsimd
        def _(e):
            S.run('pool', e)

        @block.sync
        def _(e):
            S.run('sp', e)
    stack.close()
    return nc


def rope_table():
    inv = (np.float32(10000.0) ** (-(np.arange(0, 64, 2, dtype=np.float32) / np.float32(64)))).astype(np.float32)
    pos = np.arange(SEQ, dtype=np.float32)
    ang = (pos[:, None] * inv[None, :]).astype(np.float32)
    emb = np.concatenate([ang, ang], axis=-1)
    cos = np.cos(emb).astype(np.float32); sin = np.sin(emb).astype(np.float32)
    sin[:, 0:32] = -sin[:, 0:32]
    return np.concatenate([cos, sin], axis=1).astype(np.float32)


def make_in_maps(inp):
    x = np.asarray(inp['x'], dtype=np.float32)
    tab = rope_table()
    shared = {}
    for k in ('b_ada', 'norm1_w', 'q_norm_w', 'k_norm_w', 'lambda_q1', 'lambda_k1', 'lambda_q2', 'lambda_k2',
              'subln_w', 'norm2_w', 'b_router'):
        shared[k] = np.ascontiguousarray(np.asarray(inp[k], dtype=np.float32))
    for k in ('w_ada', 'w_in', 'w_attn_o', 'conv_w', 'w_conv_o', 'w_out', 'w_router', 'w_gate_up', 'b_gate_up',
              'w_down', 'b_down'):
        shared[k] = np.ascontiguousarray(np.asarray(inp[k], dtype=np.float32)[0])
    shared['ident'] = np.eye(128, dtype=np.float32)
    shared['tri'] = np.triu(np.ones((128, 128), np.float32), 1)
    shared['iota'] = np.ascontiguousarray(np.broadcast_to(np.arange(CAP, dtype=np.float32), (128, CAP)))
    maps = []
    for core in range(8):
        b = core // 4; t0 = (core % 4) * TOWN
        m = dict(shared)
        m['xs'] = np.ascontiguousarray(np.roll(x[b], -t0, axis=0))
        m['rope'] = np.ascontiguousarray(np.roll(tab, -t0, axis=0))
        xhal = np.zeros((2, D), np.float32); msk = np.zeros((128, 2), np.float32)
        if t0 > 0:
            xhal[0] = x[b, t0 - 1]; msk[:, 0] = 1.0
        if t0 + TOWN < SEQ:
            xhal[1] = x[b, t0 + TOWN]; msk[:, 1] = 1.0
        m['xh'] = xhal; m['hm'] = msk
        m['cvec'] = np.ascontiguousarray(np.asarray(inp['c'], dtype=np.float32)[b:b + 1])
        maps.append(m)
    return maps


_NC = None


def kernel(**inp):
    global _NC
    if _NC is None:
        _NC = build()
    maps = make_in_maps(inp)
    res = run_bass_kernel_spmd(_NC, maps, core_ids=list(range(8)))
    out = np.zeros((2, SEQ, D), np.float32)
    for core in range(8):
        b = core // 4; t0 = (core % 4) * TOWN
        out[b, t0:t0 + TOWN] = res.results[core]['y']
    return out
```
